# Optimizing a Trainium2 kernel written in Bass

```python
import math
import jax, jax.numpy as jnp
from jax import lax
import numpy as np

D_MODEL = 1024
BATCH = 8
SEQ = 4096
DEPTH = 1

D_MIX = D_MODEL
D_POOL = D_MIX // 2
D_CONV = D_MIX - D_POOL
POOL_WINDOWS = (2, 4, 8, 16)
N_POOL_GROUPS = len(POOL_WINDOWS)
POOL_GROUP_DIM = D_POOL // N_POOL_GROUPS
N_CONV_HEADS = 8
CONV_WIDTH = 3
D_IN_PROJ = D_POOL + 3 * D_CONV

N_EXPERTS = 32
TOP_K = 4
D_FF = D_MODEL
SWIGLU_LIMIT = 7.0
SWIGLU_ALPHA = 1.702
EXPERT_BLOCK = 128

PLE_DIM = 256

DEEPNORM_ALPHA = (2.0 * DEPTH) ** 0.25
DEEPNORM_BETA = (8.0 * DEPTH) ** -0.25
LN_EPS = 1e-5

kernel_name = "hybrid_pool_shortconv_moe_deepnorm"


def layer_norm(x, g, b):
    xf = x.astype(jnp.float32)
    mu = jnp.mean(xf, axis=-1, keepdims=True)
    var = jnp.mean(jnp.square(xf - mu), axis=-1, keepdims=True)
    y = (xf - mu) * lax.rsqrt(var + LN_EPS)
    return (y * g.astype(jnp.float32) + b.astype(jnp.float32)).astype(x.dtype)


def pool_mixer(v, pool_mix, pool_scale):
    B, S, _ = v.shape
    vf = v.astype(jnp.float32)
    t = jnp.arange(S)
    outs = []
    for g, w in enumerate(POOL_WINDOWS):
        vg = vf[..., g * POOL_GROUP_DIM:(g + 1) * POOL_GROUP_DIM]
        cs = jnp.cumsum(vg, axis=1)
        lagged = jnp.pad(cs, ((0, 0), (w, 0), (0, 0)))[:, :S]
        cnt = jnp.minimum(t + 1, w).astype(jnp.float32)[None, :, None]
        outs.append((cs - lagged) / cnt - vg)
    d = jnp.stack(outs, axis=2).astype(v.dtype)
    y = jnp.einsum('bsgc,gcd->bsgd', d, pool_mix).reshape(B, S, D_POOL)
    return y * pool_scale


def conv_mixer(b_gate, c_gate, v, conv_w):
    S = v.shape[1]
    u = c_gate * v
    up = jnp.pad(u, ((0, 0), (CONV_WIDTH - 1, 0), (0, 0)))
    y = conv_w[0] * up[:, 0:S]
    for k in range(1, CONV_WIDTH):
        y = y + conv_w[k] * up[:, k:k + S]
    return b_gate * y


def clamped_swiglu_expert(xb, w_gu, b_gu, w_dn, b_dn):
    gu = xb @ w_gu + b_gu
    gate = jnp.minimum(gu[:, :D_FF], SWIGLU_LIMIT)
    up = jnp.clip(gu[:, D_FF:], -SWIGLU_LIMIT, SWIGLU_LIMIT)
    glu = gate * jax.nn.sigmoid(SWIGLU_ALPHA * gate)
    return ((up + 1.0) * glu) @ w_dn + b_dn


def moe(h, router_w, router_b, w_gate_up, b_gate_up, w_down, b_down):
    B, S, D = h.shape
    N = B * S
    hf = h.reshape(N, D)
    logits = (hf @ router_w + router_b).astype(jnp.float32)
    top_v, top_i = lax.top_k(logits, TOP_K)
    gates = jax.nn.softmax(top_v, axis=-1)
    A = N * TOP_K
    flat_e = top_i.reshape(A).astype(jnp.int32)
    flat_tok = jnp.repeat(jnp.arange(N, dtype=jnp.int32), TOP_K)
    flat_g = gates.reshape(A)
    order = jnp.argsort(flat_e)
    e_sorted = flat_e[order]
    counts = jax.ops.segment_sum(jnp.ones((A,), jnp.int32), flat_e, num_segments=N_EXPERTS)
    offsets = jnp.cumsum(counts) - counts
    padded = ((counts + EXPERT_BLOCK - 1) // EXPERT_BLOCK) * EXPERT_BLOCK
    pad_end = jnp.cumsum(padded)
    pad_start = pad_end - padded
    dest = pad_start[e_sorted] + (jnp.arange(A, dtype=jnp.int32) - offsets[e_sorted])
    P = A + N_EXPERTS * EXPERT_BLOCK
    NB = P // EXPERT_BLOCK
    row_tok = jnp.zeros((P,), jnp.int32).at[dest].set(flat_tok[order])
    row_gate = jnp.zeros((P,), jnp.float32).at[dest].set(flat_g[order])
    blk_start = jnp.arange(NB, dtype=jnp.int32) * EXPERT_BLOCK
    blk_e = jnp.minimum(jnp.sum(blk_start[:, None] >= pad_end[None, :], axis=1),
                        N_EXPERTS - 1).astype(jnp.int32)
    xb = hf[row_tok].reshape(NB, EXPERT_BLOCK, D)

    def run_block(args):
        xblk, e = args
        return clamped_swiglu_expert(xblk, w_gate_up[e], b_gate_up[e], w_down[e], b_down[e])

    yb = lax.map(run_block, (xb, blk_e)).reshape(P, D)
    y = jax.ops.segment_sum(yb * row_gate[:, None].astype(yb.dtype), row_tok, num_segments=N)
    return y.reshape(B, S, D)


def setup_inputs(seed: int = 0) -> dict:
    key = jax.random.key(seed)
    ks = jax.random.split(key, 24)
    L, D, E, F = DEPTH, D_MODEL, N_EXPERTS, D_FF
    nrm = lambda k, shape, s: jax.random.normal(k, shape, jnp.float32) * s
    return {
        "x": nrm(ks[0], (BATCH, SEQ, D), 1.0),
        "p": nrm(ks[1], (L, BATCH, SEQ, PLE_DIM), 1.0),
        "w_in": nrm(ks[2], (L, D, D_IN_PROJ), D ** -0.5),
        "pool_mix": nrm(ks[3], (L, N_POOL_GROUPS, POOL_GROUP_DIM, POOL_GROUP_DIM), POOL_GROUP_DIM ** -0.5),
        "pool_scale": 1.0 + nrm(ks[4], (L, D_POOL), 0.1),
        "conv_w": nrm(ks[5], (L, CONV_WIDTH, D_CONV), CONV_WIDTH ** -0.5),
        "w_out": nrm(ks[6], (L, D_MIX, D), D_MIX ** -0.5 * DEEPNORM_BETA),
        "ln1_g": 1.0 + nrm(ks[7], (L, D), 0.02),
        "ln1_b": nrm(ks[8], (L, D), 0.02),
        "router_w": nrm(ks[9], (L, D, E), D ** -0.5),
        "router_b": nrm(ks[10], (L, E), 0.01),
        "w_gate_up": nrm(ks[11], (L, E, D, 2 * F), D ** -0.5),
        "b_gate_up": nrm(ks[12], (L, E, 2 * F), 0.02),
        "w_down": nrm(ks[13], (L, E, F, D), F ** -0.5 * DEEPNORM_BETA),
        "b_down": nrm(ks[14], (L, E, D), 0.02),
        "ln2_g": 1.0 + nrm(ks[15], (L, D), 0.02),
        "ln2_b": nrm(ks[16], (L, D), 0.02),
        "ple_proj": nrm(ks[17], (L, PLE_DIM, D), PLE_DIM ** -0.5 * DEEPNORM_BETA),
        "ple_gate_w": nrm(ks[18], (L, D, D), D ** -0.5),
        "ple_gate_b": nrm(ks[19], (L, D), 0.02),
        "ln3_g": 1.0 + nrm(ks[20], (L, D), 0.02),
        "ln3_b": nrm(ks[21], (L, D), 0.02),
    }


def reference(x, p, w_in, pool_mix, pool_scale, conv_w, w_out, ln1_g, ln1_b,
              router_w, router_b, w_gate_up, b_gate_up, w_down, b_down,
              ln2_g, ln2_b, ple_proj, ple_gate_w, ple_gate_b, ln3_g, ln3_b):
    for i in range(DEPTH):
        proj = x @ w_in[i]
        v_pool = proj[..., :D_POOL]
        b_gate = proj[..., D_POOL:D_POOL + D_CONV]
        c_gate = proj[..., D_POOL + D_CONV:D_POOL + 2 * D_CONV]
        v_conv = proj[..., D_POOL + 2 * D_CONV:]
        y_pool = pool_mixer(v_pool, pool_mix[i], pool_scale[i])
        y_conv = conv_mixer(b_gate, c_gate, v_conv, conv_w[i])
        mix = jnp.concatenate([y_pool, y_conv], axis=-1) @ w_out[i]
        x = layer_norm(DEEPNORM_ALPHA * x + mix, ln1_g[i], ln1_b[i])
        ffn = moe(x, router_w[i], router_b[i], w_gate_up[i], b_gate_up[i], w_down[i], b_down[i])
        x = layer_norm(DEEPNORM_ALPHA * x + ffn, ln2_g[i], ln2_b[i])
        gate = jax.nn.sigmoid((x @ ple_gate_w[i] + ple_gate_b[i]).astype(jnp.float32)).astype(x.dtype)
        ple = p[i] @ ple_proj[i]
        x = layer_norm(DEEPNORM_ALPHA * x + gate * ple, ln3_g[i], ln3_b[i])
    return x
```

```python
import numpy as np
import concourse.bass as bass
import concourse.mybir as mybir
from concourse.bass_utils import run_bass_kernel_spmd

F32 = mybir.dt.float32
BF16 = mybir.dt.bfloat16
I32 = mybir.dt.int32
ALU = mybir.AluOpType
AF = mybir.ActivationFunctionType
AX = mybir.AxisListType

S = 4096
D = 1024
NT = S // 128
NM = S // 512
E = 32
CAP = 640
NBLK = CAP // 128
XR = E * CAP
ALPHA = float(2.0 ** 0.25)
EPS = 1e-5
WINS = (2, 4, 8, 16)
SBUF_BYTES = 206 * 1024


class T:
    def __init__(self, ap):
        self.ap = ap
        self.w = None
        self.r = []
        self.dsem = None
        self.dcnt = 0

    def __getitem__(self, k):
        return self.ap[k]


class Eng:
    def __init__(self, name, sem):
        self.name = name
        self.sem = sem
        self.cnt = 0
        self.seen = {}
        self.prog = []


class K:
    def __init__(self, nc, stack):
        self.nc = nc
        self.stack = stack
        self.eng = {}
        for n in ("tensor", "vector", "scalar", "gpsimd", "sync"):
            self.eng[n] = Eng(n, stack.enter_context(nc.semaphore("c_" + n)))
        self.bar = stack.enter_context(nc.semaphore("bar"))
        self.nbar = 0
        self.dtiles = []
        self.off = 0
        self.nalloc = 0
        self.semid = {}

    def sb(self, shape, dt, name=None):
        self.nalloc += 1
        nb = int(np.prod(shape[1:])) * (2 if dt == BF16 else 4)
        self.off = (self.off + 31) // 32 * 32
        assert self.off + nb <= SBUF_BYTES, (self.off, nb, name)
        h = self.nc.alloc_sbuf_tensor_at("t%d_%s" % (self.nalloc, name or ""), list(shape), dt, offset=self.base + self.off)
        self.off += nb
        return h

    def tile(self, shape, dt, name=None):
        h = self.sb(shape, dt, name)
        return T(h.ap())

    def view(self, ap):
        return T(ap)

    def _sid(self, sem):
        k = id(sem)
        if k not in self.semid:
            self.semid[k] = sem
        return k

    def _deps(self, e, reads, writes, extra):
        need = {}

        def add(ev):
            if ev is None:
                return
            k = self._sid(ev[0])
            if need.get(k, 0) < ev[1]:
                need[k] = ev[1]

        for t in reads:
            add(t.w)
        for t in writes:
            add(t.w)
            for ev in t.r:
                add(ev)
        for ev in extra:
            add(ev)
        waits = []
        own = id(e.sem)
        for k, v in need.items():
            if k == own and v > e.cnt:
                continue
            if e.seen.get(k, 0) < v:
                e.seen[k] = v
                waits.append((self.semid[k], v))
        return waits

    @staticmethod
    def _mark(ev, reads, writes):
        for t in reads:
            t.r.append(ev)
        for t in writes:
            t.w = ev
            t.r = []

    def op(self, en, fn, reads=(), writes=(), signal=True, extra=()):
        e = self.eng[en]
        waits = self._deps(e, reads, writes, extra)
        sem = e.sem
        if signal:
            e.cnt += 1
            ev = (sem, e.cnt)
        else:
            ev = (sem, e.cnt + 1)

        def run(eng, waits=waits, fn=fn, sem=sem, signal=signal):
            for s_, v_ in waits:
                eng.wait_ge(s_, v_)
            ins = fn(eng)
            if signal:
                ins.then_inc(sem, 1)

        e.prog.append(run)
        self._mark(ev, reads, writes)
        return ev

    def dma(self, qn, fn, owner, reads=(), writes=(), extra=()):
        e = self.eng[qn]
        waits = self._deps(e, reads, writes, extra)
        if owner.dsem is None:
            owner.dsem = self.stack.enter_context(self.nc.semaphore("d%d" % len(self.dtiles)))
            self.dtiles.append(owner)
        owner.dcnt += 16
        sem = owner.dsem
        ev = (sem, owner.dcnt)

        def run(eng, waits=waits, fn=fn, sem=sem):
            for s_, v_ in waits:
                eng.wait_ge(s_, v_)
            fn(eng).then_inc(sem, 16)

        e.prog.append(run)
        self._mark(ev, reads, writes)
        return ev

    def barrier(self):
        self.nbar += 1
        target = 5 * self.nbar
        bar = self.bar
        for n, e in self.eng.items():
            waits = []
            if n != "sync" and e.cnt > 0:
                waits.append((e.sem, e.cnt))
            if n == "sync":
                for t in self.dtiles:
                    waits.append((t.dsem, t.dcnt))

            def run(eng, waits=waits, target=target):
                for s_, v_ in waits:
                    eng.wait_ge(s_, v_)
                eng.sem_inc(bar, 1)
                eng.wait_ge(bar, target)

            e.prog.append(run)

    def finish(self, final_events):
        for n, e in self.eng.items():
            waits = []
            if n != "sync" and e.cnt > 0:
                waits.append((e.sem, e.cnt))
            if n == "sync":
                for t in self.dtiles:
                    waits.append((t.dsem, t.dcnt))

            def run(eng, waits=waits):
                for s_, v_ in waits:
                    eng.wait_ge(s_, v_)

            e.prog.append(run)


def build(debug=False, upto="D"):
    from contextlib import ExitStack

    nc = bass.Bass("TRN2", target_bir_lowering=False)

    def din(name, shape, dt=F32):
        return nc.dram_tensor(name, list(shape), dt, kind="ExternalInput")

    x_d = din("x", [S, D])
    xT_d = din("xT", [D, S])
    pT_d = din("pT", [256, S])
    w_in_d = din("w_in", [D, 2048])
    pmix_d = din("pool_mix", [4, 128, 128])
    pscale_d = din("pool_scale_t", [128, 4])
    convw_d = din("conv_w_t", [128, 12])
    w_out_d = din("w_out", [D, D])
    ln_d = din("ln_all", [6, D])
    rw_d = din("router_w", [D, E])
    rb_d = din("router_b", [1, E])
    wgu_d = din("w_gate_up", [E, D, 2048])
    bgu_d = din("b_gu_t", [128, E * 16])
    wdn_d = din("w_down", [E, D, D])
    bdn_d = din("b_down", [E, D])
    pproj_d = din("ple_proj", [256, D])
    pgw_d = din("ple_gate_w", [D, D])
    pgb_d = din("ple_gate_b", [1, D])
    ident_d = din("c_ident", [128, 128])
    tri_d = din("c_tri", [128, 128])
    ecol_d = din("c_ecol", [128, E])
    pfix_d = din("c_poolfix", [128, 64])
    out_d = nc.dram_tensor("out", [S, D], F32, kind="ExternalOutput")
    skind = "ExternalOutput" if debug else "Internal"
    XE = nc.dram_tensor("XE", [XR, D], BF16, kind=skind)
    YE = nc.dram_tensor("YE", [XR, D], F32, kind=skind)
    H1F = nc.dram_tensor("H1F", [S, D], F32, kind=skind)
    if debug:
        DBG_ROW = nc.dram_tensor("DBG_ROW", [128, NT * 4], I32, kind="ExternalOutput")
        DBG_GATE = nc.dram_tensor("DBG_GATE", [128, NT * 4], F32, kind="ExternalOutput")

    with ExitStack() as stack:
        abase = (nc._sbuf_addr_for_side("left") + 31) // 32 * 32
        arena = stack.enter_context(nc.sbuf_tensor("arena", [128, SBUF_BYTES], mybir.dt.uint8))
        assert nc._sbuf_addr_for_side("left") == abase + SBUF_BYTES, (abase, nc._sbuf_addr_for_side("left"))
        k = K(nc, stack)
        k.base = abase

        regs = {}

        def mkreg(eng):
            r = eng.alloc_register("bc")
            eng.reg_mov(r, XR - 1)
            regs["bc"] = r

        k.eng["gpsimd"].prog.append(mkreg)

        pb = [T(nc.alloc_psum_tensor("pb%d" % i, [128, 512], F32).ap()) for i in range(4)]
        pw = T(nc.alloc_psum_tensor("pw", [128, 1024], F32).ap())
        pt = T(nc.alloc_psum_tensor("pt", [128, 1024], BF16).ap())
        pl = T(nc.alloc_psum_tensor("pl", [128, 512], F32).ap())

        ident = k.tile([128, 128], BF16, "ident")
        tri = k.tile([128, 128], BF16, "tri")
        ones = k.tile([128, 128], BF16, "ones")
        ecol = k.tile([128, 1, E], F32, "ecol")
        ROWI = k.tile([128, NT, 4], I32, "rowi")
        GATE = k.tile([128, NT, 4], F32, "gate")
        base_run = k.tile([128, E], F32, "base")
        cstage = k.tile([128, 128], F32, "cstage")
        cstage2 = k.tile([128, 128], F32, "cstage2")
        persist_end = k.off

        def load_cast(dst_t, src_ap, stage_t, q="sync", ce="vector"):
            k.dma(q, lambda g: g.dma_start(out=stage_t.ap, in_=src_ap), stage_t, writes=[stage_t])
            k.op(ce, lambda g: g.tensor_copy(out=dst_t.ap, in_=stage_t.ap), reads=[stage_t], writes=[dst_t])

        load_cast(ident, ident_d.ap(), cstage)
        load_cast(tri, tri_d.ap(), cstage2)
        k.op("vector", lambda g: g.memset(ones.ap, 1.0), writes=[ones])
        k.dma("sync", lambda g: g.dma_start(out=ecol.ap, in_=ecol_d.ap().rearrange("p (o e) -> p o e", o=1)), ecol, writes=[ecol])
        k.op("vector", lambda g: g.memset(base_run.ap, 0.0), writes=[base_run])
        k.op("vector", lambda g: g.memset(ROWI.ap, 0), writes=[ROWI])
        k.op("vector", lambda g: g.memset(GATE.ap, 0.0), writes=[GATE])

        def layer_norm(z, G, Bv, out, st, mv, rs, nm, mul_eng="gpsimd"):
            k.op("vector", lambda g: g.bn_stats(out=st.ap[:, 0:6], in_=z.ap[:, 0:512]), reads=[z], writes=[st])
            k.op("vector", lambda g: g.bn_stats(out=st.ap[:, 6:12], in_=z.ap[:, 512:1024]), reads=[z, st], writes=[st])
            k.op("vector", lambda g: g.bn_aggr(out=mv.ap, in_=st.ap), reads=[st], writes=[mv])
            k.op("scalar", lambda g: g.activation(out=rs.ap, in_=mv.ap[:, 1:2], func=AF.Sqrt, bias=EPS, scale=1.0), reads=[mv], writes=[rs])
            k.op("vector", lambda g: g.reciprocal(out=rs.ap, in_=rs.ap), reads=[rs], writes=[rs])
            k.op("vector", lambda g: g.tensor_scalar(out=nm.ap, in0=mv.ap[:, 0:1], scalar1=rs.ap, scalar2=-1.0,
                                                      op0=ALU.mult, op1=ALU.mult), reads=[mv, rs], writes=[nm])
            k.op("scalar", lambda g: g.activation(out=z.ap, in_=z.ap, func=AF.Identity, bias=nm.ap, scale=rs.ap),
                 reads=[z, nm, rs], writes=[z])
            k.op(mul_eng, lambda g: g.tensor_tensor(out=z.ap, in0=z.ap, in1=G.ap, op=ALU.mult), reads=[z, G], writes=[z])
            k.op("vector", lambda g: g.tensor_tensor(out=out.ap, in0=z.ap, in1=Bv.ap, op=ALU.add), reads=[z, Bv], writes=[out])

        def transpose8(src_bf, dstT, copy_eng, out_ap=None):
            if out_ap is None:
                out_ap = dstT.ap
            pt3 = pt.ap.rearrange("p (k t) -> p k t", k=8)
            for kk in range(8):
                k.op("tensor", lambda g, kk=kk: g.transpose(out=pt.ap[:, kk * 128:(kk + 1) * 128],
                                                            in_=src_bf.ap[:, kk * 128:(kk + 1) * 128], identity=ident.ap),
                     reads=[src_bf, ident], writes=[pt], signal=(kk == 7))
            if copy_eng == "scalar":
                k.op("scalar", lambda g: g.copy(out=out_ap, in_=pt3), reads=[pt], writes=[dstT])
            else:
                k.op(copy_eng, lambda g: g.tensor_copy(out=out_ap, in_=pt3), reads=[pt], writes=[dstT])

        k.off = persist_end
        w_in = [k.tile([128, 2048], BF16, "w_in%d" % i) for i in range(8)]
        w_out = [k.tile([128, 1024], BF16, "w_out%d" % i) for i in range(8)]
        pmix = k.tile([128, 4, 128], BF16, "pmix")
        rw = k.tile([128, 8, E], BF16, "rw")
        rb = k.tile([128, E], F32, "rb")
        pscale = k.tile([128, 4], F32, "pscale")
        convw = k.tile([128, 12], F32, "convw")
        pfix = k.tile([128, 64], F32, "pfix")
        G1 = k.tile([128, D], F32, "G1")
        B1 = k.tile([128, D], F32, "B1")
        wst = [k.tile([128, 1024], F32, "wst%d" % i) for i in range(3)]
        zero = k.tile([128, 2048], BF16, "zero")
        xs = k.tile([128, 8, 512], F32, "xs")
        xb = [k.tile([128, 8, 512], BF16, "xb0")] * 2
        vp = [[k.tile([128, 528], F32, "vp%d" % g_)] * 2 for g_ in range(4)]
        ptmp = [k.tile([128, 528], F32, "ptmp%d" % i) for i in range(2)]
        db = [k.tile([128, 512], BF16, "db%d" % i) for i in range(2)]
        ub = [[k.tile([128, 514], F32, "ub%d" % j)] * 2 for j in range(4)]
        cu = k.tile([128, 512], F32, "cu")
        ctmp = [k.tile([128, 512], F32, "ctmp%d" % i) for i in range(2)]
        mixT = [[k.tile([128, 512], BF16, "mixT%d" % c) for c in range(8)]] * 2
        xt = [k.tile([128, D], F32, "xt%d" % i) for i in range(2)]
        zt = [k.tile([128, D], F32, "zt%d" % i) for i in range(2)]
        h1 = [k.tile([128, D], F32, "h1_%d" % i) for i in range(2)]
        h1b = [[k.tile([128, D], BF16, "h1b%d_%d" % (i, j)) for j in range(4)] for i in range(2)]
        h1T = [k.tile([128, 8, 128], BF16, "h1T%d" % i) for i in range(2)]
        st = k.tile([128, 12], F32, "st")
        mv = k.tile([128, 2], F32, "mv")
        rs = k.tile([128, 1], F32, "rs")
        nm = k.tile([128, 1], F32, "nm")
        Lm = [k.tile([128, 128], F32, "Lm%d" % i) for i in range(2)]
        m8 = k.tile([128, 4, 8], F32, "m8")
        maskb = k.tile([128, 128], BF16, "maskb")
        d4 = k.tile([128, 4, 4], F32, "d4")
        e4 = k.tile([128, 4, 4], F32, "e4")
        s4 = k.tile([128, 4, 1], F32, "s4")
        r4 = k.tile([128, 4, 1], F32, "r4")
        Rk = k.tile([128, 128], F32, "Rk")
        pen = k.tile([128, 128], F32, "pen")
        oh = k.tile([128, 128], F32, "oh")
        rowf = k.tile([128, 4, 4], F32, "rowf")
        print("phase A sbuf bytes", k.off)

        k.op("gpsimd", lambda g: g.memset(zero.ap, 0.0), writes=[zero])
        zf_ev = None
        for n in range(XR // 256):
            zf_ev = k.dma("scalar", lambda g, n=n: g.dma_start(
                out=XE[n * 256:(n + 1) * 256, :].rearrange("(p r) d -> p (r d)", p=128), in_=zero.ap), zero, reads=[zero])

        k.dma("sync", lambda g: g.dma_start(out=pscale.ap, in_=pscale_d.ap()), pscale, writes=[pscale])
        k.dma("sync", lambda g: g.dma_start(out=convw.ap, in_=convw_d.ap()), convw, writes=[convw])
        k.dma("sync", lambda g: g.dma_start(out=pfix.ap, in_=pfix_d.ap()), pfix, writes=[pfix])
        k.dma("sync", lambda g: g.dma_start(out=rb.ap, in_=rb_d.ap().partition_broadcast(128)), rb, writes=[rb])
        k.dma("sync", lambda g: g.dma_start(out=G1.ap, in_=ln_d[0:1, :].partition_broadcast(128)), G1, writes=[G1])
        k.dma("sync", lambda g: g.dma_start(out=B1.ap, in_=ln_d[1:2, :].partition_broadcast(128)), B1, writes=[B1])
        ci = 0
        ces = ["vector", "gpsimd", "scalar"]

        def cast(eng, dst_ap, src_t, dst_t):
            if eng == "scalar":
                k.op("scalar", lambda g: g.copy(out=dst_ap, in_=src_t.ap), reads=[src_t], writes=[dst_t])
            else:
                k.op(eng, lambda g: g.tensor_copy(out=dst_ap, in_=src_t.ap), reads=[src_t], writes=[dst_t])

        for kk in range(8):
            for hf in range(2):
                s_ = wst[ci % 3]
                k.dma("sync", lambda g, kk=kk, hf=hf, s_=s_: g.dma_start(
                    out=s_.ap, in_=w_in_d[kk * 128:(kk + 1) * 128, hf * 1024:(hf + 1) * 1024]), s_, writes=[s_])
                cast(ces[ci % 3], w_in[kk].ap[:, hf * 1024:(hf + 1) * 1024], s_, w_in[kk])
                ci += 1
        for kk in range(8):
            s_ = wst[ci % 3]
            k.dma("sync", lambda g, kk=kk, s_=s_: g.dma_start(out=s_.ap, in_=w_out_d[kk * 128:(kk + 1) * 128, :]), s_, writes=[s_])
            cast(ces[ci % 3], w_out[kk].ap, s_, w_out[kk])
            ci += 1
        s_ = wst[ci % 3]
        k.dma("sync", lambda g, s_=s_: g.dma_start(out=s_.ap[:, 0:512].rearrange("p (g c) -> p g c", g=4),
                                                  in_=pmix_d.ap().rearrange("g p c -> p g c")), s_, writes=[s_])
        k.op("vector", lambda g, s_=s_: g.tensor_copy(out=pmix.ap.rearrange("p g c -> p (g c)"), in_=s_.ap[:, 0:512]), reads=[s_], writes=[pmix])
        ci += 1
        s_ = wst[ci % 3]
        k.dma("sync", lambda g, s_=s_: g.dma_start(out=s_.ap[:, 0:256].rearrange("p (k e) -> p k e", k=8),
                                                  in_=rw_d.ap().rearrange("(k p) e -> p k e", p=128)), s_, writes=[s_])
        k.op("vector", lambda g, s_=s_: g.tensor_copy(out=rw.ap.rearrange("p k e -> p (k e)"), in_=s_.ap[:, 0:256]), reads=[s_], writes=[rw])
        ci += 1
        for g_ in range(4):
            k.op("vector", lambda g, g_=g_: g.memset(vp[g_][0].ap[:, 0:16], 0.0), writes=[vp[g_][0]])
            k.op("vector", lambda g, g_=g_: g.memset(ub[g_][0].ap[:, 0:2], 0.0), writes=[ub[g_][0]])

        xT_v = xT_d.ap().rearrange("(k p) t -> p k t", p=128)
        pbi = [0]

        def next_pb():
            t_ = pb[pbi[0] % 3]
            pbi[0] += 1
            return t_

        def proj_chunk(xbm, fc, pbt):
            for kk in range(8):
                k.op("tensor", lambda g, kk=kk: g.matmul(pbt.ap, lhsT=w_in[kk].ap[:, fc * 128:(fc + 1) * 128],
                                                         rhs=xbm.ap[:, kk, :], start=(kk == 0), stop=(kk == 7)),
                     reads=[w_in[kk], xbm], writes=[pbt], signal=(kk == 7))

        def stage1(m):
            par = m % 2
            nxt = (m + 1) % 2
            t0 = m * 512
            xbm = xb[par]
            k.dma("sync", lambda g: g.dma_start(out=xs.ap, in_=xT_v[:, :, t0:t0 + 512]), xs, writes=[xs])
            for q in range(4):
                eng = ["vector", "scalar", "gpsimd", "vector"][q]
                if eng == "scalar":
                    k.op("scalar", lambda g, q=q: g.copy(out=xbm.ap[:, 2 * q:2 * q + 2, :], in_=xs.ap[:, 2 * q:2 * q + 2, :]), reads=[xs], writes=[xbm])
                else:
                    k.op(eng, lambda g, q=q: g.tensor_copy(out=xbm.ap[:, 2 * q:2 * q + 2, :], in_=xs.ap[:, 2 * q:2 * q + 2, :]), reads=[xs], writes=[xbm])
            mx = mixT[par]
            for g_ in range(4):
                w = WINS[g_]
                v = vp[g_][par]
                vn = vp[g_][nxt]
                pbt = next_pb()
                proj_chunk(xbm, g_, pbt)
                k.op("scalar", lambda g, v=v, pbt=pbt: g.copy(out=v.ap[:, 16:528], in_=pbt.ap), reads=[pbt], writes=[v])
                cur = v
                sh = 1
                lo = 0
                idx = 0
                while sh < w:
                    lo = lo + sh
                    dst = ptmp[idx % 2]
                    k.op("vector", lambda g, cur=cur, dst=dst, lo=lo, sh=sh: g.tensor_tensor(
                        out=dst.ap[:, lo:528], in0=cur.ap[:, lo:528], in1=cur.ap[:, lo - sh:528 - sh], op=ALU.add),
                        reads=[cur], writes=[dst])
                    cur = dst
                    sh *= 2
                    idx += 1
                if m == 0:
                    k.op("vector", lambda g, cur=cur, g_=g_: g.tensor_tensor(
                        out=cur.ap[:, 16:32], in0=cur.ap[:, 16:32], in1=pfix.ap[:, g_ * 16:(g_ + 1) * 16], op=ALU.mult),
                        reads=[cur, pfix], writes=[cur])
                dbt = db[g_ % 2]
                k.op("vector", lambda g, cur=cur, v=v, dbt=dbt, w=w: g.scalar_tensor_tensor(
                    out=dbt.ap, in0=cur.ap[:, 16:528], scalar=1.0 / w, in1=v.ap[:, 16:528], op0=ALU.mult, op1=ALU.subtract),
                    reads=[cur, v], writes=[dbt])
                k.op("gpsimd", lambda g, v=v, vn=vn: g.tensor_copy(out=vn.ap[:, 0:16], in_=v.ap[:, 512:528]), reads=[v], writes=[vn])
                k.op("tensor", lambda g, g_=g_, dbt=dbt: g.matmul(pb[3].ap, lhsT=pmix.ap[:, g_, :], rhs=dbt.ap, start=True, stop=True),
                     reads=[pmix, dbt], writes=[pb[3]])
                k.op("scalar", lambda g, g_=g_: g.activation(out=mx[g_].ap, in_=pb[3].ap, func=AF.Identity, scale=pscale.ap[:, g_:g_ + 1]),
                     reads=[pb[3], pscale], writes=[mx[g_]])
            for j in range(4):
                u = ub[j][par]
                un = ub[j][nxt]
                pB = next_pb()
                proj_chunk(xbm, 4 + j, pB)
                pC = next_pb()
                proj_chunk(xbm, 8 + j, pC)
                pV = next_pb()
                proj_chunk(xbm, 12 + j, pV)
                k.op("scalar", lambda g, pC=pC: g.copy(out=cu.ap, in_=pC.ap), reads=[pC], writes=[cu])
                k.op("vector", lambda g, u=u, pV=pV: g.tensor_tensor(out=u.ap[:, 2:514], in0=cu.ap, in1=pV.ap, op=ALU.mult),
                     reads=[cu, pV], writes=[u])
                c0, c1 = ctmp
                k.op("gpsimd", lambda g, u=u, j=j: g.tensor_scalar(out=c0.ap, in0=u.ap[:, 0:512], scalar1=convw.ap[:, 3 * j:3 * j + 1],
                                                                     scalar2=None, op0=ALU.mult), reads=[u, convw], writes=[c0])
                k.op("vector", lambda g, u=u, j=j: g.scalar_tensor_tensor(out=c1.ap, in0=u.ap[:, 1:513], scalar=convw.ap[:, 3 * j + 1:3 * j + 2],
                                                                            in1=c0.ap, op0=ALU.mult, op1=ALU.add), reads=[u, convw, c0], writes=[c1])
                k.op("vector", lambda g, u=u, j=j: g.scalar_tensor_tensor(out=c0.ap, in0=u.ap[:, 2:514], scalar=convw.ap[:, 3 * j + 2:3 * j + 3],
                                                                            in1=c1.ap, op0=ALU.mult, op1=ALU.add), reads=[u, convw, c1], writes=[c0])
                k.op("gpsimd", lambda g, u=u, un=un: g.tensor_copy(out=un.ap[:, 0:2], in_=u.ap[:, 512:514]), reads=[u], writes=[un])
                k.op("vector", lambda g, j=j, pB=pB: g.tensor_tensor(out=mx[4 + j].ap, in0=c0.ap, in1=pB.ap, op=ALU.mult),
                     reads=[c0, pB], writes=[mx[4 + j]])
            for sub in range(4):
                tl = 4 * m + sub
                xtt = xt[sub % 2]
                ztt = zt[sub % 2]
                h1t = h1[sub % 2]
                k.dma("sync", lambda g, tl=tl, xtt=xtt: g.dma_start(out=xtt.ap, in_=x_d[tl * 128:(tl + 1) * 128, :]), xtt, writes=[xtt])
                for hf in range(2):
                    for kk in range(8):
                        k.op("tensor", lambda g, kk=kk, hf=hf, sub=sub: g.matmul(
                            pw.ap[:, hf * 512:(hf + 1) * 512], lhsT=mx[kk].ap[:, sub * 128:(sub + 1) * 128],
                            rhs=w_out[kk].ap[:, hf * 512:(hf + 1) * 512], start=(kk == 0), stop=(kk == 7)),
                            reads=[mx[kk], w_out[kk]], writes=[pw], signal=(kk == 7))
                k.op("vector", lambda g, xtt=xtt, ztt=ztt: g.scalar_tensor_tensor(out=ztt.ap, in0=xtt.ap, scalar=ALPHA, in1=pw.ap,
                                                                                    op0=ALU.mult, op1=ALU.add), reads=[xtt, pw], writes=[ztt])
                layer_norm(ztt, G1, B1, h1t, st, mv, rs, nm)
                k.dma("gpsimd", lambda g, tl=tl, h1t=h1t: g.dma_start(out=H1F[tl * 128:(tl + 1) * 128, :], in_=h1t.ap), h1t, reads=[h1t])
                hb = h1b[par][sub]
                k.op("scalar", lambda g, h1t=h1t, hb=hb: g.copy(out=hb.ap, in_=h1t.ap), reads=[h1t], writes=[hb])

        def stage2(m):
            par = m % 2
            L = Lm[par]
            L3 = L.ap.rearrange("p (j e) -> p j e", e=E)
            for sub in range(4):
                hb = h1b[par][sub]
                hT = h1T[sub % 2]
                transpose8(hb, hT, "scalar" if sub % 2 else "vector")
                for kk in range(8):
                    k.op("tensor", lambda g, kk=kk, hT=hT: g.matmul(pl.ap[:, 0:E], lhsT=hT.ap[:, kk, :], rhs=rw.ap[:, kk, :],
                                                                      start=(kk == 0), stop=(kk == 7)),
                         reads=[hT, rw], writes=[pl], signal=(kk == 7))
                k.op("vector", lambda g, sub=sub: g.tensor_tensor(out=L.ap[:, sub * E:(sub + 1) * E], in0=pl.ap[:, 0:E], in1=rb.ap, op=ALU.add),
                     reads=[pl, rb], writes=[L])
            for j in range(4):
                k.op("vector", lambda g, j=j: g.max(out=m8.ap[:, j, :], in_=L.ap[:, j * E:(j + 1) * E]), reads=[L], writes=[m8])
            k.op("vector", lambda g: g.tensor_tensor(out=maskb.ap.rearrange("p (j e) -> p j e", e=E), in0=L3,
                                                      in1=m8.ap[:, :, 3:4].to_broadcast([128, 4, E]), op=ALU.is_ge),
                 reads=[L, m8], writes=[maskb])
            k.op("vector", lambda g: g.tensor_tensor(out=d4.ap, in0=m8.ap[:, :, 0:4], in1=m8.ap[:, :, 0:1].to_broadcast([128, 4, 4]),
                                                      op=ALU.subtract), reads=[m8], writes=[d4])
            k.op("scalar", lambda g: g.activation(out=e4.ap, in_=d4.ap, func=AF.Exp), reads=[d4], writes=[e4])
            k.op("vector", lambda g: g.tensor_reduce(out=s4.ap.rearrange("p j o -> p (j o)"), in_=e4.ap, axis=AX.X, op=ALU.add),
                 reads=[e4], writes=[s4])
            k.op("vector", lambda g: g.reciprocal(out=r4.ap, in_=s4.ap), reads=[s4], writes=[r4])
            k.op("vector", lambda g: g.tensor_tensor(out=GATE.ap[:, 4 * m:4 * m + 4, :], in0=e4.ap, in1=r4.ap.to_broadcast([128, 4, 4]),
                                                      op=ALU.mult), reads=[e4, r4, GATE], writes=[GATE])
            k.op("tensor", lambda g: g.matmul(pl.ap[:, 128:256], lhsT=tri.ap, rhs=maskb.ap, start=True, stop=True),
                 reads=[tri, maskb], writes=[pl])
            k.op("tensor", lambda g: g.matmul(pl.ap[:, 256:384], lhsT=ones.ap, rhs=maskb.ap, start=True, stop=True),
                 reads=[ones, maskb], writes=[pl])
            for j in range(4):
                k.op("vector", lambda g, j=j: g.tensor_tensor(out=Rk.ap[:, j * E:(j + 1) * E], in0=pl.ap[:, 128 + j * E:128 + (j + 1) * E],
                                                               in1=base_run.ap, op=ALU.add), reads=[pl, base_run, Rk], writes=[Rk])
                k.op("vector", lambda g, j=j: g.tensor_tensor(out=base_run.ap, in0=base_run.ap, in1=pl.ap[:, 256 + j * E:256 + (j + 1) * E],
                                                               op=ALU.add), reads=[pl, base_run], writes=[base_run])
            k.op("vector", lambda g: g.tensor_scalar(out=pen.ap, in0=Rk.ap, scalar1=CAP + 0.5, scalar2=1.0e6, op0=ALU.is_gt, op1=ALU.mult),
                 reads=[Rk], writes=[pen])
            Rk3 = Rk.ap.rearrange("p (j e) -> p j e", e=E)
            k.op("vector", lambda g: g.tensor_tensor(out=Rk3, in0=Rk3, in1=ecol.ap.to_broadcast([128, 4, E]), op=ALU.add),
                 reads=[Rk, ecol], writes=[Rk])
            k.op("vector", lambda g: g.tensor_tensor(out=Rk.ap, in0=Rk.ap, in1=pen.ap, op=ALU.add), reads=[Rk, pen], writes=[Rk])
            oh3 = oh.ap.rearrange("p (j e) -> p j e", e=E)
            for kq in range(4):
                k.op("vector", lambda g, kq=kq: g.tensor_tensor(out=oh3, in0=L3, in1=m8.ap[:, :, kq:kq + 1].to_broadcast([128, 4, E]),
                                                                 op=ALU.is_equal), reads=[L, m8], writes=[oh])
                k.op("vector", lambda g: g.tensor_tensor(out=oh.ap, in0=oh.ap, in1=Rk.ap, op=ALU.mult), reads=[oh, Rk], writes=[oh])
                k.op("vector", lambda g, kq=kq: g.tensor_reduce(out=rowf.ap[:, :, kq], in_=oh3, axis=AX.X, op=ALU.add),
                     reads=[oh, rowf], writes=[rowf])
            k.op("vector", lambda g: g.tensor_copy(out=ROWI.ap[:, 4 * m:4 * m + 4, :], in_=rowf.ap), reads=[rowf, ROWI], writes=[ROWI])
            for j in range(4):
                hb = h1b[par][j]
                for kq in range(4):
                    k.dma("gpsimd", lambda g, j=j, kq=kq, hb=hb: g.indirect_dma_start(
                        out=XE[:, :], out_offset=bass.IndirectOffsetOnAxis(ap=ROWI.ap[:, 4 * m + j, kq:kq + 1], axis=0),
                        in_=hb.ap, in_offset=None, bounds_check=regs["bc"], oob_is_err=False),
                        hb, reads=[hb, ROWI], extra=[zf_ev])

        import os as _os
        NMR = int(_os.environ.get("K_NM", NM))
        stage1(0)
        for m in range(1, NMR):
            stage1(m)
            stage2(m - 1)
        stage2(NMR - 1)
        if debug:
            k.dma("sync", lambda g: g.dma_start(out=DBG_ROW.ap(), in_=ROWI.ap.rearrange("p t k -> p (t k)")), ROWI, reads=[ROWI])
            k.dma("sync", lambda g: g.dma_start(out=DBG_GATE.ap(), in_=GATE.ap.rearrange("p t k -> p (t k)")), GATE, reads=[GATE])
        k.barrier()

        k.off = persist_end
        import os as _os
        upto = _os.environ.get("K_UPTO", upto)
        wgu = [[k.tile([128, 2048], BF16, "wgu%d_%d" % (i, kk)) for kk in range(8)] for i in range(2)]
        wdn = [[k.tile([128, 1024], BF16, "wdn%d_%d" % (i, kk)) for kk in range(8)] for i in range(2)]
        cst = [k.tile([128, 1024], F32, "cst%d" % i) for i in range(4)]
        bgu = k.tile([128, E * 16], F32, "bgu")
        bdn = [k.tile([128, D], F32, "bdn%d" % i) for i in range(2)]
        xblk = [k.tile([128, D], BF16, "xblk%d" % i) for i in range(3)]
        XT = k.tile([128, 8, CAP], BF16, "XT")
        actT = [k.tile([128, CAP], BF16, "actT%d" % i) for i in range(8)]
        g1 = [k.tile([128, 512], F32, "g1_%d" % i) for i in range(2)]
        sg = [k.tile([128, 512], F32, "sg_%d" % i) for i in range(2)]
        u0 = [k.tile([128, 512], F32, "u0_%d" % i) for i in range(2)]
        tg = [k.tile([128, 512], F32, "tg_%d" % i) for i in range(2)]
        yb = [k.tile([128, D], F32, "yb%d" % i) for i in range(2)]
        print("phase C sbuf bytes", k.off)

        k.dma("sync", lambda g: g.dma_start(out=bgu.ap, in_=bgu_d.ap()), bgu, writes=[bgu])
        cctr = [0]
        cast_engs = ["scalar", "vector", "scalar", "gpsimd", "scalar", "vector"]

        def load_expert_weights(e):
            b = e % 2
            for kk in range(8):
                for hf in range(2):
                    s_ = cst[cctr[0] % 4]
                    k.dma("sync", lambda g, kk=kk, hf=hf, s_=s_: g.dma_start(
                        out=s_.ap, in_=wgu_d[e, kk * 128:(kk + 1) * 128, hf * 1024:(hf + 1) * 1024]), s_, writes=[s_])
                    cast(cast_engs[cctr[0] % 6], wgu[b][kk].ap[:, hf * 1024:(hf + 1) * 1024], s_, wgu[b][kk])
                    cctr[0] += 1
            for kk in range(8):
                s_ = cst[cctr[0] % 4]
                k.dma("sync", lambda g, kk=kk, s_=s_: g.dma_start(out=s_.ap, in_=wdn_d[e, kk * 128:(kk + 1) * 128, :]), s_, writes=[s_])
                cast(cast_engs[cctr[0] % 6], wdn[b][kk].ap, s_, wdn[b][kk])
                cctr[0] += 1
            k.dma("sync", lambda g: g.dma_start(out=bdn[b].ap, in_=bdn_d[e:e + 1, :].partition_broadcast(128)), bdn[b], writes=[bdn[b]])

        xctr = [0]

        def do_T(e):
            for blk in range(NBLK):
                xbk = xblk[xctr[0] % 3]
                xctr[0] += 1
                r0 = e * CAP + blk * 128
                k.dma("gpsimd", lambda g, xbk=xbk, r0=r0: g.dma_start(out=xbk.ap, in_=XE[r0:r0 + 128, :]), xbk, writes=[xbk])
                transpose8(xbk, XT, "scalar" if blk % 2 else "vector", out_ap=XT.ap[:, :, blk * 128:(blk + 1) * 128])

        gctr = [0]

        def do_GU(e):
            b = e % 2
            for pc in range(8):
                for unit in range(2):
                    if unit == 0:
                        pg, pu = (pb[0], pb[1]) if gctr[0] % 2 == 0 else (pb[2], pb[3])
                        pg_ap, pu_ap = pg.ap, pu.ap
                        n0, nn = 0, 512
                        blks = [0, 1, 2, 3]
                    else:
                        pg = pu = pl
                        pg_ap, pu_ap = pl.ap[:, 0:128], pl.ap[:, 128:256]
                        n0, nn = 512, 128
                        blks = [4]
                    bi = gctr[0] % 2
                    gctr[0] += 1
                    for which, (pp_t, pp_ap, cbase) in enumerate(((pg, pg_ap, pc * 128), (pu, pu_ap, 1024 + pc * 128))):
                        for kk in range(8):
                            k.op("tensor", lambda g, kk=kk, pp_ap=pp_ap, cbase=cbase, n0=n0, nn=nn: g.matmul(
                                pp_ap, lhsT=wgu[b][kk].ap[:, cbase:cbase + 128], rhs=XT.ap[:, kk, n0:n0 + nn],
                                start=(kk == 0), stop=(kk == 7)),
                                reads=[wgu[b][kk], XT], writes=[pp_t], signal=(kk == 7))
                    a_g1, a_sg, a_u0, a_tg = g1[bi], sg[bi], u0[bi], tg[bi]
                    bg_ap = bgu.ap[:, e * 16 + pc:e * 16 + pc + 1]
                    bu_ap = bgu.ap[:, e * 16 + 8 + pc:e * 16 + 8 + pc + 1]
                    k.op("vector", lambda g, pg_ap=pg_ap, a_g1=a_g1, bg_ap=bg_ap, nn=nn: g.tensor_scalar(
                        out=a_g1.ap[:, 0:nn], in0=pg_ap, scalar1=bg_ap, scalar2=7.0, op0=ALU.add, op1=ALU.min),
                        reads=[pg, bgu], writes=[a_g1])
                    k.op("scalar", lambda g, a_g1=a_g1, a_sg=a_sg, nn=nn: g.activation(out=a_sg.ap[:, 0:nn], in_=a_g1.ap[:, 0:nn], func=AF.Sigmoid, scale=1.702),
                         reads=[a_g1], writes=[a_sg])
                    k.op("scalar", lambda g, pu_ap=pu_ap, a_u0=a_u0, bu_ap=bu_ap, nn=nn: g.activation(out=a_u0.ap[:, 0:nn], in_=pu_ap, func=AF.Identity, bias=bu_ap),
                         reads=[pu, bgu], writes=[a_u0])
                    k.op("vector", lambda g, a_u0=a_u0, nn=nn: g.tensor_scalar(out=a_u0.ap[:, 0:nn], in0=a_u0.ap[:, 0:nn], scalar1=7.0, scalar2=-7.0,
                                                                              op0=ALU.min, op1=ALU.max), reads=[a_u0], writes=[a_u0])
                    k.op("gpsimd", lambda g, a_g1=a_g1, a_sg=a_sg, a_tg=a_tg, nn=nn: g.tensor_tensor(out=a_tg.ap[:, 0:nn], in0=a_g1.ap[:, 0:nn], in1=a_sg.ap[:, 0:nn], op=ALU.mult),
                         reads=[a_g1, a_sg], writes=[a_tg])
                    k.op("vector", lambda g, a_u0=a_u0, a_tg=a_tg, pc=pc, n0=n0, nn=nn: g.scalar_tensor_tensor(
                        out=actT[pc].ap[:, n0:n0 + nn], in0=a_u0.ap[:, 0:nn], scalar=1.0, in1=a_tg.ap[:, 0:nn], op0=ALU.add, op1=ALU.mult),
                        reads=[a_u0, a_tg, actT[pc]], writes=[actT[pc]])

        yctr = [0]

        def do_DN(e):
            b = e % 2
            for blk in range(NBLK):
                for hf in range(2):
                    for fc in range(8):
                        k.op("tensor", lambda g, fc=fc, hf=hf, blk=blk: g.matmul(
                            pw.ap[:, hf * 512:(hf + 1) * 512], lhsT=actT[fc].ap[:, blk * 128:(blk + 1) * 128],
                            rhs=wdn[b][fc].ap[:, hf * 512:(hf + 1) * 512], start=(fc == 0), stop=(fc == 7)),
                            reads=[actT[fc], wdn[b][fc]], writes=[pw], signal=(fc == 7))
                ybt = yb[yctr[0] % 2]
                yctr[0] += 1
                k.op("vector", lambda g, ybt=ybt: g.tensor_tensor(out=ybt.ap, in0=pw.ap, in1=bdn[b].ap, op=ALU.add),
                     reads=[pw, bdn[b]], writes=[ybt])
                r0 = e * CAP + blk * 128
                k.dma("gpsimd", lambda g, ybt=ybt, r0=r0: g.dma_start(out=YE[r0:r0 + 128, :], in_=ybt.ap), ybt, reads=[ybt])

        if upto != "A":
            load_expert_weights(0)
            do_T(0)
        for e in range(E if upto != "A" else 0):
            if e + 1 < E:
                load_expert_weights(e + 1)
            do_GU(e)
            if e + 1 < E:
                do_T(e + 1)
            do_DN(e)
        k.barrier()

        k.off = persist_end
        pgw = [k.tile([128, D], BF16, "pgw%d" % i) for i in range(8)]
        pproj = [k.tile([128, D], BF16, "pproj%d" % i) for i in range(2)]
        dst_ = [k.tile([128, 1024], F32, "dst%d" % i) for i in range(3)]
        G2 = k.tile([128, D], F32, "G2")
        B2 = k.tile([128, D], F32, "B2")
        G3 = k.tile([128, D], F32, "G3")
        B3 = k.tile([128, D], F32, "B3")
        pgb = k.tile([128, D], F32, "pgb")
        yk = [[k.tile([128, D], F32, "yk%d_%d" % (i, q)) for q in range(4)] for i in range(2)]
        h1r = [k.tile([128, D], F32, "h1r%d" % i) for i in range(2)]
        acc = [k.tile([128, D], F32, "acc%d" % i) for i in range(2)]
        h2 = [k.tile([128, D], F32, "h2_%d" % i) for i in range(3)]
        h2b = [k.tile([128, D], BF16, "h2b%d" % i) for i in range(2)]
        h2T = [k.tile([128, 8, 128], BF16, "h2T%d" % i) for i in range(2)]
        pts = [k.tile([128, 2, 128], F32, "pts%d" % i) for i in range(2)]
        ptb = [k.tile([128, 2, 128], BF16, "ptb%d" % i) for i in range(2)]
        gs = [k.tile([128, D], F32, "gs%d" % i) for i in range(2)]
        z3 = [k.tile([128, D], F32, "z3_%d" % i) for i in range(2)]
        ot = [k.tile([128, D], F32, "ot%d" % i) for i in range(2)]
        st2 = k.tile([128, 12], F32, "st2")
        mv2 = k.tile([128, 2], F32, "mv2")
        rs2 = k.tile([128, 1], F32, "rs2")
        nm2 = k.tile([128, 1], F32, "nm2")
        st3 = k.tile([128, 12], F32, "st3")
        mv3 = k.tile([128, 2], F32, "mv3")
        rs3 = k.tile([128, 1], F32, "rs3")
        nm3 = k.tile([128, 1], F32, "nm3")
        print("phase D sbuf bytes", k.off)

        for i, (tt, row) in enumerate(((G2, 2), (B2, 3), (G3, 4), (B3, 5))):
            k.dma("sync", lambda g, tt=tt, row=row: g.dma_start(out=tt.ap, in_=ln_d[row:row + 1, :].partition_broadcast(128)), tt, writes=[tt])
        k.dma("sync", lambda g: g.dma_start(out=pgb.ap, in_=pgb_d.ap().partition_broadcast(128)), pgb, writes=[pgb])
        ci = 0
        for kk in range(8):
            s_ = dst_[ci % 3]
            k.dma("sync", lambda g, kk=kk, s_=s_: g.dma_start(out=s_.ap, in_=pgw_d[kk * 128:(kk + 1) * 128, :]), s_, writes=[s_])
            cast(ces[ci % 3], pgw[kk].ap, s_, pgw[kk])
            ci += 1
        for c in range(2):
            s_ = dst_[ci % 3]
            k.dma("sync", lambda g, c=c, s_=s_: g.dma_start(out=s_.ap, in_=pproj_d[c * 128:(c + 1) * 128, :]), s_, writes=[s_])
            cast(ces[ci % 3], pproj[c].ap, s_, pproj[c])
            ci += 1
        pT_v = pT_d.ap().rearrange("(c p) t -> p c t", p=128)

        def stageD1(t):
            b = t % 2
            for q in range(4):
                k.dma("gpsimd", lambda g, q=q: g.indirect_dma_start(
                    out=yk[b][q].ap, out_offset=None, in_=YE[:, :],
                    in_offset=bass.IndirectOffsetOnAxis(ap=ROWI.ap[:, t, q:q + 1], axis=0),
                    bounds_check=regs["bc"], oob_is_err=False), yk[b][q], reads=[ROWI], writes=[yk[b][q]])
            k.dma("sync", lambda g: g.dma_start(out=h1r[b].ap, in_=H1F[t * 128:(t + 1) * 128, :]), h1r[b], writes=[h1r[b]])
            k.dma("sync", lambda g: g.dma_start(out=pts[b].ap, in_=pT_v[:, :, t * 128:(t + 1) * 128]), pts[b], writes=[pts[b]])
            k.op("gpsimd", lambda g: g.tensor_copy(out=ptb[b].ap, in_=pts[b].ap), reads=[pts[b]], writes=[ptb[b]])
            a = acc[b]
            k.op("gpsimd", lambda g: g.tensor_scalar(out=a.ap, in0=yk[b][0].ap, scalar1=GATE.ap[:, t, 0:1], scalar2=None, op0=ALU.mult),
                 reads=[yk[b][0], GATE], writes=[a])
            for q in range(1, 4):
                k.op("vector", lambda g, q=q: g.scalar_tensor_tensor(out=a.ap, in0=yk[b][q].ap, scalar=GATE.ap[:, t, q:q + 1], in1=a.ap,
                                                                      op0=ALU.mult, op1=ALU.add), reads=[yk[b][q], GATE, a], writes=[a])
            k.op("vector", lambda g: g.scalar_tensor_tensor(out=a.ap, in0=h1r[b].ap, scalar=ALPHA, in1=a.ap, op0=ALU.mult, op1=ALU.add),
                 reads=[h1r[b], a], writes=[a])
            h2t = h2[t % 3]
            layer_norm(a, G2, B2, h2t, st2, mv2, rs2, nm2)
            k.op("scalar", lambda g: g.copy(out=h2b[b].ap, in_=h2t.ap), reads=[h2t], writes=[h2b[b]])

        def stageD2(t):
            b = t % 2
            h2t = h2[t % 3]
            transpose8(h2b[b], h2T[b], "scalar")
            for hf in range(2):
                for kk in range(8):
                    k.op("tensor", lambda g, kk=kk, hf=hf: g.matmul(pw.ap[:, hf * 512:(hf + 1) * 512], lhsT=h2T[b].ap[:, kk, :],
                                                                      rhs=pgw[kk].ap[:, hf * 512:(hf + 1) * 512], start=(kk == 0), stop=(kk == 7)),
                         reads=[h2T[b], pgw[kk]], writes=[pw], signal=(kk == 7))
            pp = (pb[0], pb[1]) if b == 0 else (pb[2], pb[3])
            for hf in range(2):
                for c in range(2):
                    k.op("tensor", lambda g, c=c, hf=hf: g.matmul(pp[hf].ap, lhsT=ptb[b].ap[:, c, :], rhs=pproj[c].ap[:, hf * 512:(hf + 1) * 512],
                                                                    start=(c == 0), stop=(c == 1)),
                         reads=[ptb[b], pproj[c]], writes=[pp[hf]], signal=(c == 1))
            g_ = gs[b]
            k.op("vector", lambda g: g.tensor_tensor(out=g_.ap, in0=pw.ap, in1=pgb.ap, op=ALU.add), reads=[pw, pgb], writes=[g_])
            k.op("scalar", lambda g: g.activation(out=g_.ap, in_=g_.ap, func=AF.Sigmoid), reads=[g_], writes=[g_])
            for hf in range(2):
                k.op("vector", lambda g, hf=hf: g.tensor_tensor(out=g_.ap[:, hf * 512:(hf + 1) * 512], in0=g_.ap[:, hf * 512:(hf + 1) * 512],
                                                                 in1=pp[hf].ap, op=ALU.mult), reads=[g_, pp[hf]], writes=[g_])
            z = z3[b]
            k.op("vector", lambda g: g.scalar_tensor_tensor(out=z.ap, in0=h2t.ap, scalar=ALPHA, in1=g_.ap, op0=ALU.mult, op1=ALU.add),
                 reads=[h2t, g_], writes=[z])
            o = ot[b]
            layer_norm(z, G3, B3, o, st3, mv3, rs3, nm3)
            return k.dma("sync", lambda g: g.dma_start(out=out_d[t * 128:(t + 1) * 128, :], in_=o.ap), o, reads=[o])

        if upto == "D":
            stageD1(0)
            for t in range(1, NT):
                stageD1(t)
                stageD2(t - 1)
            stageD2(NT - 1)
        k.finish(None)

        with nc.Block() as block:
            @block.sync
            def _(eng):
                for f in k.eng["sync"].prog:
                    f(eng)

            @block.scalar
            def _(eng):
                for f in k.eng["scalar"].prog:
                    f(eng)

            @block.vector
            def _(eng):
                for f in k.eng["vector"].prog:
                    f(eng)

            @block.gpsimd
            def _(eng):
                for f in k.eng["gpsimd"].prog:
                    f(eng)

            @block.tensor
            def _(eng):
                for f in k.eng["tensor"].prog:
                    f(eng)
    return nc


def make_consts():
    ident = np.eye(128, dtype=np.float32)
    tri = np.triu(np.ones((128, 128), dtype=np.float32))
    ecol = np.tile((np.arange(E, dtype=np.float32) * CAP - 1.0)[None, :], (128, 1))
    pfix = np.ones((128, 4, 16), dtype=np.float32)
    for g_, w in enumerate(WINS):
        for t in range(16):
            pfix[:, g_, t] = w / min(t + 1, w)
    return ident, tri, ecol, pfix.reshape(128, 64)


def make_in_maps(inp):
    f = lambda a: np.ascontiguousarray(np.asarray(a, dtype=np.float32))
    x = f(inp["x"])
    p = f(inp["p"])[0]
    ident, tri, ecol, pfix = make_consts()
    shared = {
        "w_in": f(inp["w_in"][0]),
        "pool_mix": f(inp["pool_mix"][0]),
        "pool_scale_t": f(np.asarray(inp["pool_scale"][0]).reshape(4, 128).T),
        "conv_w_t": f(np.asarray(inp["conv_w"][0]).reshape(3, 4, 128).transpose(2, 1, 0).reshape(128, 12)),
        "w_out": f(inp["w_out"][0]),
        "ln_all": f(np.stack([np.asarray(inp[n][0]) for n in ("ln1_g", "ln1_b", "ln2_g", "ln2_b", "ln3_g", "ln3_b")], 0)),
        "router_w": f(inp["router_w"][0]),
        "router_b": f(np.asarray(inp["router_b"][0]).reshape(1, E)),
        "w_gate_up": f(inp["w_gate_up"][0]),
        "b_gu_t": f(np.asarray(inp["b_gate_up"][0]).reshape(E, 16, 128).transpose(2, 0, 1).reshape(128, E * 16)),
        "w_down": f(inp["w_down"][0]),
        "b_down": f(inp["b_down"][0]),
        "ple_proj": f(inp["ple_proj"][0]),
        "ple_gate_w": f(inp["ple_gate_w"][0]),
        "ple_gate_b": f(np.asarray(inp["ple_gate_b"][0]).reshape(1, D)),
        "c_ident": ident, "c_tri": tri, "c_ecol": ecol, "c_poolfix": pfix,
    }
    maps = []
    for b in range(8):
        m = dict(shared)
        m["x"] = f(x[b])
        m["xT"] = f(x[b].T)
        m["pT"] = f(p[b].T)
        maps.append(m)
    return maps


def kernel(**inputs):
    nc = build(debug=False)
    in_maps = make_in_maps(inputs)
    res = run_bass_kernel_spmd(nc, in_maps, core_ids=list(range(8)))
    out = np.stack([np.asarray(r["out"], dtype=np.float32).reshape(S, D) for r in res.results], 0)
    return out
```

```python
import numpy as np
import concourse.bass as bass
import concourse.mybir as mybir
from concourse.bass_utils import run_bass_kernel_spmd

F32 = mybir.dt.float32
BF16 = mybir.dt.bfloat16
I32 = mybir.dt.int32
ALU = mybir.AluOpType
AF = mybir.ActivationFunctionType
AX = mybir.AxisListType

S = 4096
D = 1024
NT = S // 128
NM = S // 512
E = 32
CAP = 640
NBLK = CAP // 128
XR = E * CAP
ALPHA = float(2.0 ** 0.25)
EPS = 1e-5
WINS = (2, 4, 8, 16)
SBUF_BYTES = 206 * 1024


class T:
    def __init__(self, ap):
        self.ap = ap
        self.w = None
        self.r = []
        self.dsem = None
        self.dcnt = 0

    def __getitem__(self, k):
        return self.ap[k]


class Eng:
    def __init__(self, name, sem):
        self.name = name
        self.sem = sem
        self.cnt = 0
        self.seen = {}
        self.prog = []


class K:
    def __init__(self, nc, stack):
        self.nc = nc
        self.stack = stack
        self.eng = {}
        for n in ("tensor", "vector", "scalar", "gpsimd", "sync"):
            self.eng[n] = Eng(n, stack.enter_context(nc.semaphore("c_" + n)))
        self.bar = stack.enter_context(nc.semaphore("bar"))
        self.nbar = 0
        self.dtiles = []
        self.off = 0
        self.nalloc = 0
        self.semid = {}

    def sb(self, shape, dt, name=None):
        self.nalloc += 1
        nb = int(np.prod(shape[1:])) * (2 if dt == BF16 else 4)
        self.off = (self.off + 31) // 32 * 32
        assert self.off + nb <= SBUF_BYTES, (self.off, nb, name)
        h = self.nc.alloc_sbuf_tensor_at("t%d_%s" % (self.nalloc, name or ""), list(shape), dt, offset=self.base + self.off)
        self.off += nb
        return h

    def tile(self, shape, dt, name=None):
        h = self.sb(shape, dt, name)
        return T(h.ap())

    def view(self, ap):
        return T(ap)

    def _sid(self, sem):
        k = id(sem)
        if k not in self.semid:
            self.semid[k] = sem
        return k

    def _deps(self, e, reads, writes, extra):
        need = {}

        def add(ev):
            if ev is None:
                return
            k = self._sid(ev[0])
            if need.get(k, 0) < ev[1]:
                need[k] = ev[1]

        for t in reads:
            add(t.w)
        for t in writes:
            add(t.w)
            for ev in t.r:
                add(ev)
        for ev in extra:
            add(ev)
        waits = []
        own = id(e.sem)
        for k, v in need.items():
            if k == own and v > e.cnt:
                continue
            if e.seen.get(k, 0) < v:
                e.seen[k] = v
                waits.append((self.semid[k], v))
        return waits

    @staticmethod
    def _mark(ev, reads, writes):
        for t in reads:
            t.r.append(ev)
        for t in writes:
            t.w = ev
            t.r = []

    def op(self, en, fn, reads=(), writes=(), signal=True, extra=()):
        e = self.eng[en]
        waits = self._deps(e, reads, writes, extra)
        sem = e.sem
        if signal:
            e.cnt += 1
            ev = (sem, e.cnt)
        else:
            ev = (sem, e.cnt + 1)

        def run(eng, waits=waits, fn=fn, sem=sem, signal=signal):
            for s_, v_ in waits:
                eng.wait_ge(s_, v_)
            ins = fn(eng)
            if signal:
                ins.then_inc(sem, 1)

        e.prog.append(run)
        self._mark(ev, reads, writes)
        return ev

    def dma(self, qn, fn, owner, reads=(), writes=(), extra=()):
        e = self.eng[qn]
        waits = self._deps(e, reads, writes, extra)
        if owner.dsem is None:
            owner.dsem = self.stack.enter_context(self.nc.semaphore("d%d" % len(self.dtiles)))
            self.dtiles.append(owner)
        owner.dcnt += 16
        sem = owner.dsem
        ev = (sem, owner.dcnt)

        def run(eng, waits=waits, fn=fn, sem=sem):
            for s_, v_ in waits:
                eng.wait_ge(s_, v_)
            fn(eng).then_inc(sem, 16)

        e.prog.append(run)
        self._mark(ev, reads, writes)
        return ev

    def barrier(self):
        self.nbar += 1
        target = 5 * self.nbar
        bar = self.bar
        for n, e in self.eng.items():
            waits = []
            if n != "sync" and e.cnt > 0:
                waits.append((e.sem, e.cnt))
            if n == "sync":
                for t in self.dtiles:
                    waits.append((t.dsem, t.dcnt))

            def run(eng, waits=waits, target=target):
                for s_, v_ in waits:
                    eng.wait_ge(s_, v_)
                eng.sem_inc(bar, 1)
                eng.wait_ge(bar, target)

            e.prog.append(run)

    def finish(self, final_events):
        for n, e in self.eng.items():
            waits = []
            if n != "sync" and e.cnt > 0:
                waits.append((e.sem, e.cnt))
            if n == "sync":
                for t in self.dtiles:
                    waits.append((t.dsem, t.dcnt))

            def run(eng, waits=waits):
                for s_, v_ in waits:
                    eng.wait_ge(s_, v_)

            e.prog.append(run)


def build(debug=False, upto="D"):
    from contextlib import ExitStack

    nc = bass.Bass("TRN2", target_bir_lowering=False)

    def din(name, shape, dt=F32):
        return nc.dram_tensor(name, list(shape), dt, kind="ExternalInput")

    x_d = din("x", [S, D])
    xT_d = din("xT", [D, S])
    pT_d = din("pT", [256, S])
    w_in_d = din("w_in", [D, 2048])
    pmix_d = din("pool_mix", [4, 128, 128])
    pscale_d = din("pool_scale_t", [128, 4])
    convw_d = din("conv_w_t", [128, 12])
    w_out_d = din("w_out", [D, D])
    ln_d = din("ln_all", [6, D])
    rw_d = din("router_w", [D, E])
    rb_d = din("router_b", [1, E])
    wgu_d = din("w_gate_up", [E, D, 2048])
    bgu_d = din("b_gu_t", [128, E * 16])
    wdn_d = din("w_down", [E, D, D])
    bdn_d = din("b_down", [E, D])
    pproj_d = din("ple_proj", [256, D])
    pgw_d = din("ple_gate_w", [D, D])
    pgb_d = din("ple_gate_b", [1, D])
    ident_d = din("c_ident", [128, 128])
    tri_d = din("c_tri", [128, 128])
    ecol_d = din("c_ecol", [128, E])
    pfix_d = din("c_poolfix", [128, 64])
    out_d = nc.dram_tensor("out", [S, D], F32, kind="ExternalOutput")
    skind = "ExternalOutput" if debug else "Internal"
    XE = nc.dram_tensor("XE", [XR, D], BF16, kind=skind)
    YE = nc.dram_tensor("YE", [XR, D], F32, kind=skind)
    H1F = nc.dram_tensor("H1F", [S, D], F32, kind=skind)
    if debug:
        DBG_ROW = nc.dram_tensor("DBG_ROW", [128, NT * 4], I32, kind="ExternalOutput")
        DBG_GATE = nc.dram_tensor("DBG_GATE", [128, NT * 4], F32, kind="ExternalOutput")

    with ExitStack() as stack:
        abase = (nc._sbuf_addr_for_side("left") + 31) // 32 * 32
        arena = stack.enter_context(nc.sbuf_tensor("arena", [128, SBUF_BYTES], mybir.dt.uint8))
        assert nc._sbuf_addr_for_side("left") == abase + SBUF_BYTES, (abase, nc._sbuf_addr_for_side("left"))
        k = K(nc, stack)
        k.base = abase

        regs = {}

        def mkreg(eng):
            r = eng.alloc_register("bc")
            eng.reg_mov(r, XR - 1)
            regs["bc"] = r

        k.eng["gpsimd"].prog.append(mkreg)

        pb = [T(nc.alloc_psum_tensor("pb%d" % i, [128, 512], F32).ap()) for i in range(4)]
        pw = T(nc.alloc_psum_tensor("pw", [128, 1024], F32).ap())
        pt = T(nc.alloc_psum_tensor("pt", [128, 1024], BF16).ap())
        pl = T(nc.alloc_psum_tensor("pl", [128, 512], F32).ap())

        ident = k.tile([128, 128], BF16, "ident")
        tri = k.tile([128, 128], BF16, "tri")
        ones = k.tile([128, 128], BF16, "ones")
        ecol = k.tile([128, 1, E], F32, "ecol")
        ROWI = k.tile([128, NT, 4], I32, "rowi")
        GATE = k.tile([128, NT, 4], F32, "gate")
        base_run = k.tile([128, E], F32, "base")
        cstage = k.tile([128, 128], F32, "cstage")
        cstage2 = k.tile([128, 128], F32, "cstage2")
        persist_end = k.off

        def load_cast(dst_t, src_ap, stage_t, q="sync", ce="vector"):
            k.dma(q, lambda g: g.dma_start(out=stage_t.ap, in_=src_ap), stage_t, writes=[stage_t])
            k.op(ce, lambda g: g.tensor_copy(out=dst_t.ap, in_=stage_t.ap), reads=[stage_t], writes=[dst_t])

        load_cast(ident, ident_d.ap(), cstage)
        load_cast(tri, tri_d.ap(), cstage2)
        k.op("vector", lambda g: g.memset(ones.ap, 1.0), writes=[ones])
        k.dma("sync", lambda g: g.dma_start(out=ecol.ap, in_=ecol_d.ap().rearrange("p (o e) -> p o e", o=1)), ecol, writes=[ecol])
        k.op("vector", lambda g: g.memset(base_run.ap, 0.0), writes=[base_run])
        k.op("vector", lambda g: g.memset(ROWI.ap, 0), writes=[ROWI])
        k.op("vector", lambda g: g.memset(GATE.ap, 0.0), writes=[GATE])

        def layer_norm(z, G, Bv, out, st, mv, rs, nm, mul_eng="gpsimd"):
            k.op("vector", lambda g: g.bn_stats(out=st.ap[:, 0:6], in_=z.ap[:, 0:512]), reads=[z], writes=[st])
            k.op("vector", lambda g: g.bn_stats(out=st.ap[:, 6:12], in_=z.ap[:, 512:1024]), reads=[z, st], writes=[st])
            k.op("vector", lambda g: g.bn_aggr(out=mv.ap, in_=st.ap), reads=[st], writes=[mv])
            k.op("scalar", lambda g: g.activation(out=rs.ap, in_=mv.ap[:, 1:2], func=AF.Sqrt, bias=EPS, scale=1.0), reads=[mv], writes=[rs])
            k.op("vector", lambda g: g.reciprocal(out=rs.ap, in_=rs.ap), reads=[rs], writes=[rs])
            k.op("vector", lambda g: g.tensor_scalar(out=nm.ap, in0=mv.ap[:, 0:1], scalar1=rs.ap, scalar2=-1.0,
                                                      op0=ALU.mult, op1=ALU.mult), reads=[mv, rs], writes=[nm])
            k.op("scalar", lambda g: g.activation(out=z.ap, in_=z.ap, func=AF.Identity, bias=nm.ap, scale=rs.ap),
                 reads=[z, nm, rs], writes=[z])
            k.op(mul_eng, lambda g: g.tensor_tensor(out=z.ap, in0=z.ap, in1=G.ap, op=ALU.mult), reads=[z, G], writes=[z])
            k.op("vector", lambda g: g.tensor_tensor(out=out.ap, in0=z.ap, in1=Bv.ap, op=ALU.add), reads=[z, Bv], writes=[out])

        def transpose8(src_bf, dstT, copy_eng, out_ap=None):
            if out_ap is None:
                out_ap = dstT.ap
            pt3 = pt.ap.rearrange("p (k t) -> p k t", k=8)
            for kk in range(8):
                k.op("tensor", lambda g, kk=kk: g.transpose(out=pt.ap[:, kk * 128:(kk + 1) * 128],
                                                            in_=src_bf.ap[:, kk * 128:(kk + 1) * 128], identity=ident.ap),
                     reads=[src_bf, ident], writes=[pt], signal=(kk == 7))
            if copy_eng == "scalar":
                k.op("scalar", lambda g: g.copy(out=out_ap, in_=pt3), reads=[pt], writes=[dstT])
            else:
                k.op(copy_eng, lambda g: g.tensor_copy(out=out_ap, in_=pt3), reads=[pt], writes=[dstT])

        k.off = persist_end
        w_in = [k.tile([128, 2048], BF16, "w_in%d" % i) for i in range(8)]
        w_out = [k.tile([128, 1024], BF16, "w_out%d" % i) for i in range(8)]
        pmix = k.tile([128, 4, 128], BF16, "pmix")
        rw = k.tile([128, 8, E], BF16, "rw")
        rb = k.tile([128, E], F32, "rb")
        pscale = k.tile([128, 4], F32, "pscale")
        convw = k.tile([128, 12], F32, "convw")
        pfix = k.tile([128, 64], F32, "pfix")
        G1 = k.tile([128, D], F32, "G1")
        B1 = k.tile([128, D], F32, "B1")
        wst = [k.tile([128, 1024], F32, "wst%d" % i) for i in range(3)]
        zero = k.tile([128, 2048], BF16, "zero")
        xs = k.tile([128, 8, 512], F32, "xs")
        xb = [k.tile([128, 8, 512], BF16, "xb0")] * 2
        vp = [[k.tile([128, 528], F32, "vp%d" % g_)] * 2 for g_ in range(4)]
        ptmp = [k.tile([128, 528], F32, "ptmp%d" % i) for i in range(2)]
        db = [k.tile([128, 512], BF16, "db%d" % i) for i in range(2)]
        ub = [[k.tile([128, 514], F32, "ub%d" % j)] * 2 for j in range(4)]
        cu = k.tile([128, 512], F32, "cu")
        ctmp = [k.tile([128, 512], F32, "ctmp%d" % i) for i in range(2)]
        mixT = [[k.tile([128, 512], BF16, "mixT%d" % c) for c in range(8)]] * 2
        xt = [k.tile([128, D], F32, "xt%d" % i) for i in range(2)]
        zt = [k.tile([128, D], F32, "zt%d" % i) for i in range(2)]
        h1 = [k.tile([128, D], F32, "h1_%d" % i) for i in range(2)]
        h1b = [[k.tile([128, D], BF16, "h1b%d_%d" % (i, j)) for j in range(4)] for i in range(2)]
        h1T = [k.tile([128, 8, 128], BF16, "h1T%d" % i) for i in range(2)]
        st = k.tile([128, 12], F32, "st")
        mv = k.tile([128, 2], F32, "mv")
        rs = k.tile([128, 1], F32, "rs")
        nm = k.tile([128, 1], F32, "nm")
        Lm = [k.tile([128, 128], F32, "Lm%d" % i) for i in range(2)]
        m8 = k.tile([128, 4, 8], F32, "m8")
        maskb = k.tile([128, 128], BF16, "maskb")
        d4 = k.tile([128, 4, 4], F32, "d4")
        e4 = k.tile([128, 4, 4], F32, "e4")
        s4 = k.tile([128, 4, 1], F32, "s4")
        r4 = k.tile([128, 4, 1], F32, "r4")
        Rk = k.tile([128, 128], F32, "Rk")
        pen = k.tile([128, 128], F32, "pen")
        oh = k.tile([128, 128], F32, "oh")
        rowf = k.tile([128, 4, 4], F32, "rowf")
        print("phase A sbuf bytes", k.off)

        k.op("gpsimd", lambda g: g.memset(zero.ap, 0.0), writes=[zero])
        zf_ev = None
        for n in range(XR // 256):
            zf_ev = k.dma("scalar", lambda g, n=n: g.dma_start(
                out=XE[n * 256:(n + 1) * 256, :].rearrange("(p r) d -> p (r d)", p=128), in_=zero.ap), zero, reads=[zero])

        k.dma("sync", lambda g: g.dma_start(out=pscale.ap, in_=pscale_d.ap()), pscale, writes=[pscale])
        k.dma("sync", lambda g: g.dma_start(out=convw.ap, in_=convw_d.ap()), convw, writes=[convw])
        k.dma("sync", lambda g: g.dma_start(out=pfix.ap, in_=pfix_d.ap()), pfix, writes=[pfix])
        k.dma("sync", lambda g: g.dma_start(out=rb.ap, in_=rb_d.ap().partition_broadcast(128)), rb, writes=[rb])
        k.dma("sync", lambda g: g.dma_start(out=G1.ap, in_=ln_d[0:1, :].partition_broadcast(128)), G1, writes=[G1])
        k.dma("sync", lambda g: g.dma_start(out=B1.ap, in_=ln_d[1:2, :].partition_broadcast(128)), B1, writes=[B1])
        ci = 0
        ces = ["vector", "gpsimd", "scalar"]

        def cast(eng, dst_ap, src_t, dst_t):
            if eng == "scalar":
                k.op("scalar", lambda g: g.copy(out=dst_ap, in_=src_t.ap), reads=[src_t], writes=[dst_t])
            else:
                k.op(eng, lambda g: g.tensor_copy(out=dst_ap, in_=src_t.ap), reads=[src_t], writes=[dst_t])

        for kk in range(8):
            for hf in range(2):
                s_ = wst[ci % 3]
                k.dma("sync", lambda g, kk=kk, hf=hf, s_=s_: g.dma_start(
                    out=s_.ap, in_=w_in_d[kk * 128:(kk + 1) * 128, hf * 1024:(hf + 1) * 1024]), s_, writes=[s_])
                cast(ces[ci % 3], w_in[kk].ap[:, hf * 1024:(hf + 1) * 1024], s_, w_in[kk])
                ci += 1
        for kk in range(8):
            s_ = wst[ci % 3]
            k.dma("sync", lambda g, kk=kk, s_=s_: g.dma_start(out=s_.ap, in_=w_out_d[kk * 128:(kk + 1) * 128, :]), s_, writes=[s_])
            cast(ces[ci % 3], w_out[kk].ap, s_, w_out[kk])
            ci += 1
        s_ = wst[ci % 3]
        k.dma("sync", lambda g, s_=s_: g.dma_start(out=s_.ap[:, 0:512].rearrange("p (g c) -> p g c", g=4),
                                                  in_=pmix_d.ap().rearrange("g p c -> p g c")), s_, writes=[s_])
        k.op("vector", lambda g, s_=s_: g.tensor_copy(out=pmix.ap.rearrange("p g c -> p (g c)"), in_=s_.ap[:, 0:512]), reads=[s_], writes=[pmix])
        ci += 1
        s_ = wst[ci % 3]
        k.dma("sync", lambda g, s_=s_: g.dma_start(out=s_.ap[:, 0:256].rearrange("p (k e) -> p k e", k=8),
                                                  in_=rw_d.ap().rearrange("(k p) e -> p k e", p=128)), s_, writes=[s_])
        k.op("vector", lambda g, s_=s_: g.tensor_copy(out=rw.ap.rearrange("p k e -> p (k e)"), in_=s_.ap[:, 0:256]), reads=[s_], writes=[rw])
        ci += 1
        for g_ in range(4):
            k.op("vector", lambda g, g_=g_: g.memset(vp[g_][0].ap[:, 0:16], 0.0), writes=[vp[g_][0]])
            k.op("vector", lambda g, g_=g_: g.memset(ub[g_][0].ap[:, 0:2], 0.0), writes=[ub[g_][0]])

        xT_v = xT_d.ap().rearrange("(k p) t -> p k t", p=128)
        pbi = [0]

        def next_pb():
            t_ = pb[pbi[0] % 3]
            pbi[0] += 1
            return t_

        def proj_chunk(xbm, fc, pbt):
            for kk in range(8):
                k.op("tensor", lambda g, kk=kk: g.matmul(pbt.ap, lhsT=w_in[kk].ap[:, fc * 128:(fc + 1) * 128],
                                                         rhs=xbm.ap[:, kk, :], start=(kk == 0), stop=(kk == 7)),
                     reads=[w_in[kk], xbm], writes=[pbt], signal=(kk == 7))

        def stage1(m):
            par = m % 2
            nxt = (m + 1) % 2
            t0 = m * 512
            xbm = xb[par]
            k.dma("sync", lambda g: g.dma_start(out=xs.ap, in_=xT_v[:, :, t0:t0 + 512]), xs, writes=[xs])
            for q in range(4):
                eng = ["vector", "scalar", "vector", "scalar"][q]
                if eng == "scalar":
                    k.op("scalar", lambda g, q=q: g.copy(out=xbm.ap[:, 2 * q:2 * q + 2, :], in_=xs.ap[:, 2 * q:2 * q + 2, :]), reads=[xs], writes=[xbm])
                else:
                    k.op(eng, lambda g, q=q: g.tensor_copy(out=xbm.ap[:, 2 * q:2 * q + 2, :], in_=xs.ap[:, 2 * q:2 * q + 2, :]), reads=[xs], writes=[xbm])
            mx = mixT[par]
            for g_ in range(4):
                w = WINS[g_]
                v = vp[g_][par]
                vn = vp[g_][nxt]
                pbt = next_pb()
                proj_chunk(xbm, g_, pbt)
                k.op("scalar", lambda g, v=v, pbt=pbt: g.copy(out=v.ap[:, 16:528], in_=pbt.ap), reads=[pbt], writes=[v])
                cur = v
                sh = 1
                lo = 0
                idx = 0
                while sh < w:
                    lo = lo + sh
                    dst = ptmp[idx % 2]
                    k.op("vector", lambda g, cur=cur, dst=dst, lo=lo, sh=sh: g.tensor_tensor(
                        out=dst.ap[:, lo:528], in0=cur.ap[:, lo:528], in1=cur.ap[:, lo - sh:528 - sh], op=ALU.add),
                        reads=[cur], writes=[dst])
                    cur = dst
                    sh *= 2
                    idx += 1
                if m == 0:
                    k.op("vector", lambda g, cur=cur, g_=g_: g.tensor_tensor(
                        out=cur.ap[:, 16:32], in0=cur.ap[:, 16:32], in1=pfix.ap[:, g_ * 16:(g_ + 1) * 16], op=ALU.mult),
                        reads=[cur, pfix], writes=[cur])
                dbt = db[g_ % 2]
                k.op("vector", lambda g, cur=cur, v=v, dbt=dbt, w=w: g.scalar_tensor_tensor(
                    out=dbt.ap, in0=cur.ap[:, 16:528], scalar=1.0 / w, in1=v.ap[:, 16:528], op0=ALU.mult, op1=ALU.subtract),
                    reads=[cur, v], writes=[dbt])
                k.op("gpsimd", lambda g, v=v, vn=vn: g.tensor_copy(out=vn.ap[:, 0:16], in_=v.ap[:, 512:528]), reads=[v], writes=[vn])
                k.op("tensor", lambda g, g_=g_, dbt=dbt: g.matmul(pb[3].ap, lhsT=pmix.ap[:, g_, :], rhs=dbt.ap, start=True, stop=True),
                     reads=[pmix, dbt], writes=[pb[3]])
                k.op("scalar", lambda g, g_=g_: g.activation(out=mx[g_].ap, in_=pb[3].ap, func=AF.Identity, scale=pscale.ap[:, g_:g_ + 1]),
                     reads=[pb[3], pscale], writes=[mx[g_]])
            for j in range(4):
                u = ub[j][par]
                un = ub[j][nxt]
                pB = next_pb()
                proj_chunk(xbm, 4 + j, pB)
                pC = next_pb()
                proj_chunk(xbm, 8 + j, pC)
                pV = next_pb()
                proj_chunk(xbm, 12 + j, pV)
                k.op("scalar", lambda g, pC=pC: g.copy(out=cu.ap, in_=pC.ap), reads=[pC], writes=[cu])
                k.op("vector", lambda g, u=u, pV=pV: g.tensor_tensor(out=u.ap[:, 2:514], in0=cu.ap, in1=pV.ap, op=ALU.mult),
                     reads=[cu, pV], writes=[u])
                c0, c1 = ctmp
                k.op("scalar", lambda g, u=u, j=j: g.activation(out=c0.ap, in_=u.ap[:, 0:512], func=AF.Identity, scale=convw.ap[:, 3 * j:3 * j + 1]),
                     reads=[u, convw], writes=[c0])
                k.op("vector", lambda g, u=u, j=j: g.scalar_tensor_tensor(out=c1.ap, in0=u.ap[:, 1:513], scalar=convw.ap[:, 3 * j + 1:3 * j + 2],
                                                                            in1=c0.ap, op0=ALU.mult, op1=ALU.add), reads=[u, convw, c0], writes=[c1])
                k.op("vector", lambda g, u=u, j=j: g.scalar_tensor_tensor(out=c0.ap, in0=u.ap[:, 2:514], scalar=convw.ap[:, 3 * j + 2:3 * j + 3],
                                                                            in1=c1.ap, op0=ALU.mult, op1=ALU.add), reads=[u, convw, c1], writes=[c0])
                k.op("gpsimd", lambda g, u=u, un=un: g.tensor_copy(out=un.ap[:, 0:2], in_=u.ap[:, 512:514]), reads=[u], writes=[un])
                k.op("vector", lambda g, j=j, pB=pB: g.tensor_tensor(out=mx[4 + j].ap, in0=c0.ap, in1=pB.ap, op=ALU.mult),
                     reads=[c0, pB], writes=[mx[4 + j]])
            for sub in range(4):
                tl = 4 * m + sub
                xtt = xt[sub % 2]
                ztt = zt[sub % 2]
                h1t = h1[sub % 2]
                k.dma("sync", lambda g, tl=tl, xtt=xtt: g.dma_start(out=xtt.ap, in_=x_d[tl * 128:(tl + 1) * 128, :]), xtt, writes=[xtt])
                for hf in range(2):
                    for kk in range(8):
                        k.op("tensor", lambda g, kk=kk, hf=hf, sub=sub: g.matmul(
                            pw.ap[:, hf * 512:(hf + 1) * 512], lhsT=mx[kk].ap[:, sub * 128:(sub + 1) * 128],
                            rhs=w_out[kk].ap[:, hf * 512:(hf + 1) * 512], start=(kk == 0), stop=(kk == 7)),
                            reads=[mx[kk], w_out[kk]], writes=[pw], signal=(kk == 7))
                k.op("vector", lambda g, xtt=xtt, ztt=ztt: g.scalar_tensor_tensor(out=ztt.ap, in0=xtt.ap, scalar=ALPHA, in1=pw.ap,
                                                                                    op0=ALU.mult, op1=ALU.add), reads=[xtt, pw], writes=[ztt])
                layer_norm(ztt, G1, B1, h1t, st, mv, rs, nm)
                k.dma("gpsimd", lambda g, tl=tl, h1t=h1t: g.dma_start(out=H1F[tl * 128:(tl + 1) * 128, :], in_=h1t.ap), h1t, reads=[h1t])
                hb = h1b[par][sub]
                k.op("scalar", lambda g, h1t=h1t, hb=hb: g.copy(out=hb.ap, in_=h1t.ap), reads=[h1t], writes=[hb])

        def stage2(m):
            par = m % 2
            L = Lm[par]
            L3 = L.ap.rearrange("p (j e) -> p j e", e=E)
            for sub in range(4):
                hb = h1b[par][sub]
                hT = h1T[sub % 2]
                transpose8(hb, hT, "scalar" if sub % 2 else "vector")
                for kk in range(8):
                    k.op("tensor", lambda g, kk=kk, hT=hT: g.matmul(pl.ap[:, 0:E], lhsT=hT.ap[:, kk, :], rhs=rw.ap[:, kk, :],
                                                                      start=(kk == 0), stop=(kk == 7)),
                         reads=[hT, rw], writes=[pl], signal=(kk == 7))
                k.op("vector", lambda g, sub=sub: g.tensor_tensor(out=L.ap[:, sub * E:(sub + 1) * E], in0=pl.ap[:, 0:E], in1=rb.ap, op=ALU.add),
                     reads=[pl, rb], writes=[L])
            for j in range(4):
                k.op("vector", lambda g, j=j: g.max(out=m8.ap[:, j, :], in_=L.ap[:, j * E:(j + 1) * E]), reads=[L], writes=[m8])
            k.op("vector", lambda g: g.tensor_tensor(out=maskb.ap.rearrange("p (j e) -> p j e", e=E), in0=L3,
                                                      in1=m8.ap[:, :, 3:4].to_broadcast([128, 4, E]), op=ALU.is_ge),
                 reads=[L, m8], writes=[maskb])
            k.op("vector", lambda g: g.tensor_tensor(out=d4.ap, in0=m8.ap[:, :, 0:4], in1=m8.ap[:, :, 0:1].to_broadcast([128, 4, 4]),
                                                      op=ALU.subtract), reads=[m8], writes=[d4])
            k.op("scalar", lambda g: g.activation(out=e4.ap, in_=d4.ap, func=AF.Exp), reads=[d4], writes=[e4])
            k.op("vector", lambda g: g.tensor_reduce(out=s4.ap.rearrange("p j o -> p (j o)"), in_=e4.ap, axis=AX.X, op=ALU.add),
                 reads=[e4], writes=[s4])
            k.op("vector", lambda g: g.reciprocal(out=r4.ap, in_=s4.ap), reads=[s4], writes=[r4])
            k.op("vector", lambda g: g.tensor_tensor(out=GATE.ap[:, 4 * m:4 * m + 4, :], in0=e4.ap, in1=r4.ap.to_broadcast([128, 4, 4]),
                                                      op=ALU.mult), reads=[e4, r4, GATE], writes=[GATE])
            k.op("tensor", lambda g: g.matmul(pl.ap[:, 128:256], lhsT=tri.ap, rhs=maskb.ap, start=True, stop=True),
                 reads=[tri, maskb], writes=[pl])
            k.op("tensor", lambda g: g.matmul(pl.ap[:, 256:384], lhsT=ones.ap, rhs=maskb.ap, start=True, stop=True),
                 reads=[ones, maskb], writes=[pl])
            for j in range(4):
                k.op("vector", lambda g, j=j: g.tensor_tensor(out=Rk.ap[:, j * E:(j + 1) * E], in0=pl.ap[:, 128 + j * E:128 + (j + 1) * E],
                                                               in1=base_run.ap, op=ALU.add), reads=[pl, base_run, Rk], writes=[Rk])
                k.op("vector", lambda g, j=j: g.tensor_tensor(out=base_run.ap, in0=base_run.ap, in1=pl.ap[:, 256 + j * E:256 + (j + 1) * E],
                                                               op=ALU.add), reads=[pl, base_run], writes=[base_run])
            k.op("vector", lambda g: g.tensor_scalar(out=pen.ap, in0=Rk.ap, scalar1=CAP + 0.5, scalar2=1.0e6, op0=ALU.is_gt, op1=ALU.mult),
                 reads=[Rk], writes=[pen])
            Rk3 = Rk.ap.rearrange("p (j e) -> p j e", e=E)
            k.op("vector", lambda g: g.tensor_tensor(out=Rk3, in0=Rk3, in1=ecol.ap.to_broadcast([128, 4, E]), op=ALU.add),
                 reads=[Rk, ecol], writes=[Rk])
            k.op("vector", lambda g: g.tensor_tensor(out=Rk.ap, in0=Rk.ap, in1=pen.ap, op=ALU.add), reads=[Rk, pen], writes=[Rk])
            oh3 = oh.ap.rearrange("p (j e) -> p j e", e=E)
            for kq in range(4):
                k.op("vector", lambda g, kq=kq: g.tensor_tensor(out=oh3, in0=L3, in1=m8.ap[:, :, kq:kq + 1].to_broadcast([128, 4, E]),
                                                                 op=ALU.is_equal), reads=[L, m8], writes=[oh])
                k.op("vector", lambda g: g.tensor_tensor(out=oh.ap, in0=oh.ap, in1=Rk.ap, op=ALU.mult), reads=[oh, Rk], writes=[oh])
                k.op("vector", lambda g, kq=kq: g.tensor_reduce(out=rowf.ap[:, :, kq], in_=oh3, axis=AX.X, op=ALU.add),
                     reads=[oh, rowf], writes=[rowf])
            k.op("vector", lambda g: g.tensor_copy(out=ROWI.ap[:, 4 * m:4 * m + 4, :], in_=rowf.ap), reads=[rowf, ROWI], writes=[ROWI])
            for j in range(4):
                hb = h1b[par][j]
                for kq in range(4):
                    k.dma("gpsimd", lambda g, j=j, kq=kq, hb=hb: g.indirect_dma_start(
                        out=XE[:, :], out_offset=bass.IndirectOffsetOnAxis(ap=ROWI.ap[:, 4 * m + j, kq:kq + 1], axis=0),
                        in_=hb.ap, in_offset=None, bounds_check=regs["bc"], oob_is_err=False),
                        hb, reads=[hb, ROWI], extra=[zf_ev])

        import os as _os
        NMR = int(_os.environ.get("K_NM", NM))
        stage1(0)
        for m in range(1, NMR):
            stage1(m)
            stage2(m - 1)
        stage2(NMR - 1)
        if debug:
            k.dma("sync", lambda g: g.dma_start(out=DBG_ROW.ap(), in_=ROWI.ap.rearrange("p t k -> p (t k)")), ROWI, reads=[ROWI])
            k.dma("sync", lambda g: g.dma_start(out=DBG_GATE.ap(), in_=GATE.ap.rearrange("p t k -> p (t k)")), GATE, reads=[GATE])
        k.barrier()

        k.off = persist_end
        import os as _os
        upto = _os.environ.get("K_UPTO", upto)
        wgu = [[k.tile([128, 2048], BF16, "wgu%d_%d" % (i, kk)) for kk in range(8)] for i in range(2)]
        wdn = [[k.tile([128, 1024], BF16, "wdn%d_%d" % (i, kk)) for kk in range(8)] for i in range(2)]
        cst = [k.tile([128, 1024], F32, "cst%d" % i) for i in range(6)]
        bgu = k.tile([128, E * 16], F32, "bgu")
        bdn = [k.tile([128, D], F32, "bdn%d" % i) for i in range(2)]
        xblk = [[k.tile([128, D], BF16, "xblk%d_%d" % (i, j)) for j in range(NBLK)] for i in range(2)]
        XT = k.tile([128, 8, CAP], BF16, "XT")
        actT = [k.tile([128, CAP], BF16, "actT%d" % i) for i in range(8)]
        g1 = [k.tile([128, 512], F32, "g1_%d" % i) for i in range(2)]
        sg = [k.tile([128, 512], F32, "sg_%d" % i) for i in range(2)]
        u0 = [k.tile([128, 512], F32, "u0_%d" % i) for i in range(2)]
        tg = [k.tile([128, 512], F32, "tg_%d" % i) for i in range(2)]
        yb = [k.tile([128, D], F32, "yb%d" % i) for i in range(2)]
        print("phase C sbuf bytes", k.off)

        k.dma("sync", lambda g: g.dma_start(out=bgu.ap, in_=bgu_d.ap()), bgu, writes=[bgu])
        cctr = [0]
        cast_engs = ["scalar", "vector", "scalar", "vector", "scalar"]

        def weight_tasks(e):
            b = e % 2
            tasks = []
            for kk in range(8):
                for hf in range(2):
                    def tk(kk=kk, hf=hf):
                        s_ = cst[cctr[0] % 6]
                        k.dma("sync", lambda g: g.dma_start(
                            out=s_.ap, in_=wgu_d[e, kk * 128:(kk + 1) * 128, hf * 1024:(hf + 1) * 1024]), s_, writes=[s_])
                        cast(cast_engs[cctr[0] % 5], wgu[b][kk].ap[:, hf * 1024:(hf + 1) * 1024], s_, wgu[b][kk])
                        cctr[0] += 1
                    tasks.append(tk)
            for kk in range(8):
                def tk(kk=kk):
                    s_ = cst[cctr[0] % 6]
                    k.dma("sync", lambda g: g.dma_start(out=s_.ap, in_=wdn_d[e, kk * 128:(kk + 1) * 128, :]), s_, writes=[s_])
                    cast(cast_engs[cctr[0] % 5], wdn[b][kk].ap, s_, wdn[b][kk])
                    cctr[0] += 1
                tasks.append(tk)

            def tb():
                k.dma("sync", lambda g: g.dma_start(out=bdn[b].ap, in_=bdn_d[e:e + 1, :].partition_broadcast(128)), bdn[b], writes=[bdn[b]])
            tasks.append(tb)
            return tasks

        pend = []

        def pump(n):
            for _ in range(n):
                if pend:
                    pend.pop(0)()

        def load_X(e):
            for blk in range(NBLK):
                xbk = xblk[e % 2][blk]
                r0 = e * CAP + blk * 128
                k.dma("gpsimd", lambda g, xbk=xbk, r0=r0: g.dma_start(out=xbk.ap, in_=XE[r0:r0 + 128, :]), xbk, writes=[xbk])

        def do_T(e):
            for blk in range(NBLK):
                xbk = xblk[e % 2][blk]
                transpose8(xbk, XT, "scalar" if blk % 2 else "vector", out_ap=XT.ap[:, :, blk * 128:(blk + 1) * 128])

        gctr = [0]

        def do_GU(e):
            b = e % 2
            for pc in range(8):
                for unit in range(2):
                    if unit == 0:
                        pg, pu = (pb[0], pb[1]) if gctr[0] % 2 == 0 else (pb[2], pb[3])
                        pg_ap, pu_ap = pg.ap, pu.ap
                        n0, nn = 0, 512
                        blks = [0, 1, 2, 3]
                    else:
                        pg = pu = pl
                        pg_ap, pu_ap = pl.ap[:, 0:128], pl.ap[:, 128:256]
                        n0, nn = 512, 128
                        blks = [4]
                    bi = gctr[0] % 2
                    gctr[0] += 1
                    for which, (pp_t, pp_ap, cbase) in enumerate(((pg, pg_ap, pc * 128), (pu, pu_ap, 1024 + pc * 128))):
                        for kk in range(8):
                            k.op("tensor", lambda g, kk=kk, pp_ap=pp_ap, cbase=cbase, n0=n0, nn=nn: g.matmul(
                                pp_ap, lhsT=wgu[b][kk].ap[:, cbase:cbase + 128], rhs=XT.ap[:, kk, n0:n0 + nn],
                                start=(kk == 0), stop=(kk == 7)),
                                reads=[wgu[b][kk], XT], writes=[pp_t], signal=(kk == 7))
                    a_g1, a_sg, a_u0, a_tg = g1[bi], sg[bi], u0[bi], tg[bi]
                    bg_ap = bgu.ap[:, e * 16 + pc:e * 16 + pc + 1]
                    bu_ap = bgu.ap[:, e * 16 + 8 + pc:e * 16 + 8 + pc + 1]
                    k.op("vector", lambda g, pg_ap=pg_ap, a_g1=a_g1, bg_ap=bg_ap, nn=nn: g.tensor_scalar(
                        out=a_g1.ap[:, 0:nn], in0=pg_ap, scalar1=bg_ap, scalar2=7.0, op0=ALU.add, op1=ALU.min),
                        reads=[pg, bgu], writes=[a_g1])
                    k.op("scalar", lambda g, a_g1=a_g1, a_sg=a_sg, nn=nn: g.activation(out=a_sg.ap[:, 0:nn], in_=a_g1.ap[:, 0:nn], func=AF.Sigmoid, scale=1.702),
                         reads=[a_g1], writes=[a_sg])
                    k.op("scalar", lambda g, pu_ap=pu_ap, a_u0=a_u0, bu_ap=bu_ap, nn=nn: g.activation(out=a_u0.ap[:, 0:nn], in_=pu_ap, func=AF.Identity, bias=bu_ap),
                         reads=[pu, bgu], writes=[a_u0])
                    k.op("vector", lambda g, a_u0=a_u0, nn=nn: g.tensor_scalar(out=a_u0.ap[:, 0:nn], in0=a_u0.ap[:, 0:nn], scalar1=7.0, scalar2=-7.0,
                                                                              op0=ALU.min, op1=ALU.max), reads=[a_u0], writes=[a_u0])
                    k.op("gpsimd", lambda g, a_g1=a_g1, a_sg=a_sg, a_tg=a_tg, nn=nn: g.tensor_tensor(out=a_tg.ap[:, 0:nn], in0=a_g1.ap[:, 0:nn], in1=a_sg.ap[:, 0:nn], op=ALU.mult),
                         reads=[a_g1, a_sg], writes=[a_tg])
                    k.op("vector", lambda g, a_u0=a_u0, a_tg=a_tg, pc=pc, n0=n0, nn=nn: g.scalar_tensor_tensor(
                        out=actT[pc].ap[:, n0:n0 + nn], in0=a_u0.ap[:, 0:nn], scalar=1.0, in1=a_tg.ap[:, 0:nn], op0=ALU.add, op1=ALU.mult),
                        reads=[a_u0, a_tg, actT[pc]], writes=[actT[pc]])
                    pump(1)

        yctr = [0]

        def do_DN(e):
            b = e % 2
            for blk in range(NBLK):
                for hf in range(2):
                    for fc in range(8):
                        k.op("tensor", lambda g, fc=fc, hf=hf, blk=blk: g.matmul(
                            pw.ap[:, hf * 512:(hf + 1) * 512], lhsT=actT[fc].ap[:, blk * 128:(blk + 1) * 128],
                            rhs=wdn[b][fc].ap[:, hf * 512:(hf + 1) * 512], start=(fc == 0), stop=(fc == 7)),
                            reads=[actT[fc], wdn[b][fc]], writes=[pw], signal=(fc == 7))
                ybt = yb[yctr[0] % 2]
                yctr[0] += 1
                k.op("vector", lambda g, ybt=ybt: g.tensor_tensor(out=ybt.ap, in0=pw.ap, in1=bdn[b].ap, op=ALU.add),
                     reads=[pw, bdn[b]], writes=[ybt])
                r0 = e * CAP + blk * 128
                k.dma("gpsimd", lambda g, ybt=ybt, r0=r0: g.dma_start(out=YE[r0:r0 + 128, :], in_=ybt.ap), ybt, reads=[ybt])
                pump(2)

        if upto != "A":
            pend.extend(weight_tasks(0))
            pump(100)
            load_X(0)
            do_T(0)
        for e in range(E if upto != "A" else 0):
            if e + 1 < E:
                pend.extend(weight_tasks(e + 1))
                load_X(e + 1)
            do_GU(e)
            if e + 1 < E:
                do_T(e + 1)
            do_DN(e)
            pump(100)
        k.barrier()

        k.off = persist_end
        pgw = [k.tile([128, D], BF16, "pgw%d" % i) for i in range(8)]
        pproj = [k.tile([128, D], BF16, "pproj%d" % i) for i in range(2)]
        dst_ = [k.tile([128, 1024], F32, "dst%d" % i) for i in range(3)]
        G2 = k.tile([128, D], F32, "G2")
        B2 = k.tile([128, D], F32, "B2")
        G3 = k.tile([128, D], F32, "G3")
        B3 = k.tile([128, D], F32, "B3")
        pgb = k.tile([128, D], F32, "pgb")
        yk = [[k.tile([128, D], F32, "yk%d_%d" % (i, q)) for q in range(4)] for i in range(3)]
        h1r = [k.tile([128, D], F32, "h1r%d" % i) for i in range(3)]
        acc = [k.tile([128, D], F32, "acc%d" % i) for i in range(2)]
        h2 = [k.tile([128, D], F32, "h2_%d" % i) for i in range(3)]
        h2b = [k.tile([128, D], BF16, "h2b%d" % i) for i in range(2)]
        h2T = [k.tile([128, 8, 128], BF16, "h2T%d" % i) for i in range(2)]
        pts = [k.tile([128, 2, 128], F32, "pts%d" % i) for i in range(3)]
        ptb = [k.tile([128, 2, 128], BF16, "ptb%d" % i) for i in range(3)]
        gs = [k.tile([128, D], F32, "gs%d" % i) for i in range(2)]
        z3 = [k.tile([128, D], F32, "z3_%d" % i) for i in range(2)]
        ot = [k.tile([128, D], F32, "ot%d" % i) for i in range(2)]
        st2 = k.tile([128, 12], F32, "st2")
        mv2 = k.tile([128, 2], F32, "mv2")
        rs2 = k.tile([128, 1], F32, "rs2")
        nm2 = k.tile([128, 1], F32, "nm2")
        st3 = k.tile([128, 12], F32, "st3")
        mv3 = k.tile([128, 2], F32, "mv3")
        rs3 = k.tile([128, 1], F32, "rs3")
        nm3 = k.tile([128, 1], F32, "nm3")
        print("phase D sbuf bytes", k.off)

        for i, (tt, row) in enumerate(((G2, 2), (B2, 3), (G3, 4), (B3, 5))):
            k.dma("sync", lambda g, tt=tt, row=row: g.dma_start(out=tt.ap, in_=ln_d[row:row + 1, :].partition_broadcast(128)), tt, writes=[tt])
        k.dma("sync", lambda g: g.dma_start(out=pgb.ap, in_=pgb_d.ap().partition_broadcast(128)), pgb, writes=[pgb])
        ci = 0
        for kk in range(8):
            s_ = dst_[ci % 3]
            k.dma("sync", lambda g, kk=kk, s_=s_: g.dma_start(out=s_.ap, in_=pgw_d[kk * 128:(kk + 1) * 128, :]), s_, writes=[s_])
            cast(ces[ci % 3], pgw[kk].ap, s_, pgw[kk])
            ci += 1
        for c in range(2):
            s_ = dst_[ci % 3]
            k.dma("sync", lambda g, c=c, s_=s_: g.dma_start(out=s_.ap, in_=pproj_d[c * 128:(c + 1) * 128, :]), s_, writes=[s_])
            cast(ces[ci % 3], pproj[c].ap, s_, pproj[c])
            ci += 1
        pT_v = pT_d.ap().rearrange("(c p) t -> p c t", p=128)

        def gatherD(t):
            b3 = t % 3
            for q in range(4):
                k.dma("gpsimd", lambda g, q=q: g.indirect_dma_start(
                    out=yk[b3][q].ap, out_offset=None, in_=YE[:, :],
                    in_offset=bass.IndirectOffsetOnAxis(ap=ROWI.ap[:, t, q:q + 1], axis=0),
                    bounds_check=regs["bc"], oob_is_err=False), yk[b3][q], reads=[ROWI], writes=[yk[b3][q]])
            k.dma("sync", lambda g: g.dma_start(out=h1r[b3].ap, in_=H1F[t * 128:(t + 1) * 128, :]), h1r[b3], writes=[h1r[b3]])
            k.dma("sync", lambda g: g.dma_start(out=pts[b3].ap, in_=pT_v[:, :, t * 128:(t + 1) * 128]), pts[b3], writes=[pts[b3]])
            k.op("gpsimd", lambda g: g.tensor_copy(out=ptb[b3].ap, in_=pts[b3].ap), reads=[pts[b3]], writes=[ptb[b3]])

        def stageD1(t):
            b = t % 2
            b3 = t % 3
            a = acc[b]
            k.op("scalar", lambda g: g.activation(out=a.ap, in_=yk[b3][0].ap, func=AF.Identity, scale=GATE.ap[:, t, 0:1]),
                 reads=[yk[b3][0], GATE], writes=[a])
            for q in range(1, 4):
                k.op("vector", lambda g, q=q: g.scalar_tensor_tensor(out=a.ap, in0=yk[b3][q].ap, scalar=GATE.ap[:, t, q:q + 1], in1=a.ap,
                                                                      op0=ALU.mult, op1=ALU.add), reads=[yk[b3][q], GATE, a], writes=[a])
            k.op("vector", lambda g: g.scalar_tensor_tensor(out=a.ap, in0=h1r[b3].ap, scalar=ALPHA, in1=a.ap, op0=ALU.mult, op1=ALU.add),
                 reads=[h1r[b3], a], writes=[a])
            h2t = h2[t % 3]
            layer_norm(a, G2, B2, h2t, st2, mv2, rs2, nm2)
            k.op("scalar", lambda g: g.copy(out=h2b[b].ap, in_=h2t.ap), reads=[h2t], writes=[h2b[b]])

        def stageD2(t):
            b = t % 2
            h2t = h2[t % 3]
            transpose8(h2b[b], h2T[b], "scalar")
            for hf in range(2):
                for kk in range(8):
                    k.op("tensor", lambda g, kk=kk, hf=hf: g.matmul(pw.ap[:, hf * 512:(hf + 1) * 512], lhsT=h2T[b].ap[:, kk, :],
                                                                      rhs=pgw[kk].ap[:, hf * 512:(hf + 1) * 512], start=(kk == 0), stop=(kk == 7)),
                         reads=[h2T[b], pgw[kk]], writes=[pw], signal=(kk == 7))
            pp = (pb[0], pb[1]) if b == 0 else (pb[2], pb[3])
            for hf in range(2):
                for c in range(2):
                    k.op("tensor", lambda g, c=c, hf=hf: g.matmul(pp[hf].ap, lhsT=ptb[t % 3].ap[:, c, :], rhs=pproj[c].ap[:, hf * 512:(hf + 1) * 512],
                                                                    start=(c == 0), stop=(c == 1)),
                         reads=[ptb[t % 3], pproj[c]], writes=[pp[hf]], signal=(c == 1))
            g_ = gs[b]
            k.op("vector", lambda g: g.tensor_tensor(out=g_.ap, in0=pw.ap, in1=pgb.ap, op=ALU.add), reads=[pw, pgb], writes=[g_])
            k.op("scalar", lambda g: g.activation(out=g_.ap, in_=g_.ap, func=AF.Sigmoid), reads=[g_], writes=[g_])
            for hf in range(2):
                k.op("vector", lambda g, hf=hf: g.tensor_tensor(out=g_.ap[:, hf * 512:(hf + 1) * 512], in0=g_.ap[:, hf * 512:(hf + 1) * 512],
                                                                 in1=pp[hf].ap, op=ALU.mult), reads=[g_, pp[hf]], writes=[g_])
            z = z3[b]
            k.op("vector", lambda g: g.scalar_tensor_tensor(out=z.ap, in0=h2t.ap, scalar=ALPHA, in1=g_.ap, op0=ALU.mult, op1=ALU.add),
                 reads=[h2t, g_], writes=[z])
            o = ot[b]
            layer_norm(z, G3, B3, o, st3, mv3, rs3, nm3)
            return k.dma("sync", lambda g: g.dma_start(out=out_d[t * 128:(t + 1) * 128, :], in_=o.ap), o, reads=[o])

        if upto == "D":
            gatherD(0)
            gatherD(1)
            stageD1(0)
            for t in range(1, NT):
                if t + 1 < NT:
                    gatherD(t + 1)
                stageD1(t)
                stageD2(t - 1)
            stageD2(NT - 1)
        k.finish(None)

        with nc.Block() as block:
            @block.sync
            def _(eng):
                for f in k.eng["sync"].prog:
                    f(eng)

            @block.scalar
            def _(eng):
                for f in k.eng["scalar"].prog:
                    f(eng)

            @block.vector
            def _(eng):
                for f in k.eng["vector"].prog:
                    f(eng)

            @block.gpsimd
            def _(eng):
                for f in k.eng["gpsimd"].prog:
                    f(eng)

            @block.tensor
            def _(eng):
                for f in k.eng["tensor"].prog:
                    f(eng)
    return nc


def make_consts():
    ident = np.eye(128, dtype=np.float32)
    tri = np.triu(np.ones((128, 128), dtype=np.float32))
    ecol = np.tile((np.arange(E, dtype=np.float32) * CAP - 1.0)[None, :], (128, 1))
    pfix = np.ones((128, 4, 16), dtype=np.float32)
    for g_, w in enumerate(WINS):
        for t in range(16):
            pfix[:, g_, t] = w / min(t + 1, w)
    return ident, tri, ecol, pfix.reshape(128, 64)


def make_in_maps(inp):
    f = lambda a: np.ascontiguousarray(np.asarray(a, dtype=np.float32))
    x = f(inp["x"])
    p = f(inp["p"])[0]
    ident, tri, ecol, pfix = make_consts()
    shared = {
        "w_in": f(inp["w_in"][0]),
        "pool_mix": f(inp["pool_mix"][0]),
        "pool_scale_t": f(np.asarray(inp["pool_scale"][0]).reshape(4, 128).T),
        "conv_w_t": f(np.asarray(inp["conv_w"][0]).reshape(3, 4, 128).transpose(2, 1, 0).reshape(128, 12)),
        "w_out": f(inp["w_out"][0]),
        "ln_all": f(np.stack([np.asarray(inp[n][0]) for n in ("ln1_g", "ln1_b", "ln2_g", "ln2_b", "ln3_g", "ln3_b")], 0)),
        "router_w": f(inp["router_w"][0]),
        "router_b": f(np.asarray(inp["router_b"][0]).reshape(1, E)),
        "w_gate_up": f(inp["w_gate_up"][0]),
        "b_gu_t": f(np.asarray(inp["b_gate_up"][0]).reshape(E, 16, 128).transpose(2, 0, 1).reshape(128, E * 16)),
        "w_down": f(inp["w_down"][0]),
        "b_down": f(inp["b_down"][0]),
        "ple_proj": f(inp["ple_proj"][0]),
        "ple_gate_w": f(inp["ple_gate_w"][0]),
        "ple_gate_b": f(np.asarray(inp["ple_gate_b"][0]).reshape(1, D)),
        "c_ident": ident, "c_tri": tri, "c_ecol": ecol, "c_poolfix": pfix,
    }
    maps = []
    for b in range(8):
        m = dict(shared)
        m["x"] = f(x[b])
        m["xT"] = f(x[b].T)
        m["pT"] = f(p[b].T)
        maps.append(m)
    return maps


def kernel(**inputs):
    nc = build(debug=False)
    in_maps = make_in_maps(inputs)
    res = run_bass_kernel_spmd(nc, in_maps, core_ids=list(range(8)))
    out = np.stack([np.asarray(r["out"], dtype=np.float32).reshape(S, D) for r in res.results], 0)
    return out
```

```python
import numpy as np
import concourse.bass as bass
import concourse.mybir as mybir
from concourse.bass_utils import run_bass_kernel_spmd

F32 = mybir.dt.float32
BF16 = mybir.dt.bfloat16
I32 = mybir.dt.int32
ALU = mybir.AluOpType
AF = mybir.ActivationFunctionType
AX = mybir.AxisListType

S = 4096
D = 1024
NT = S // 128
NM = S // 512
E = 32
CAP = 640
NBLK = CAP // 128
XR = E * CAP
ALPHA = float(2.0 ** 0.25)
EPS = 1e-5
WINS = (2, 4, 8, 16)
SBUF_BYTES = 206 * 1024


class T:
    def __init__(self, ap):
        self.ap = ap
        self.w = None
        self.r = []
        self.dsem = None
        self.dcnt = 0

    def __getitem__(self, k):
        return self.ap[k]


class Eng:
    def __init__(self, name, sem):
        self.name = name
        self.sem = sem
        self.cnt = 0
        self.seen = {}
        self.prog = []


class K:
    def __init__(self, nc, stack):
        self.nc = nc
        self.stack = stack
        self.eng = {}
        for n in ("tensor", "vector", "scalar", "gpsimd", "sync"):
            self.eng[n] = Eng(n, stack.enter_context(nc.semaphore("c_" + n)))
        self.bar = stack.enter_context(nc.semaphore("bar"))
        self.nbar = 0
        self.dtiles = []
        self.off = 0
        self.nalloc = 0
        self.semid = {}

    def sb(self, shape, dt, name=None):
        self.nalloc += 1
        nb = int(np.prod(shape[1:])) * (2 if dt == BF16 else 4)
        self.off = (self.off + 31) // 32 * 32
        assert self.off + nb <= SBUF_BYTES, (self.off, nb, name)
        h = self.nc.alloc_sbuf_tensor_at("t%d_%s" % (self.nalloc, name or ""), list(shape), dt, offset=self.base + self.off)
        self.off += nb
        return h

    def tile(self, shape, dt, name=None):
        h = self.sb(shape, dt, name)
        return T(h.ap())

    def view(self, ap):
        return T(ap)

    def _sid(self, sem):
        k = id(sem)
        if k not in self.semid:
            self.semid[k] = sem
        return k

    def _deps(self, e, reads, writes, extra):
        need = {}

        def add(ev):
            if ev is None:
                return
            k = self._sid(ev[0])
            if need.get(k, 0) < ev[1]:
                need[k] = ev[1]

        for t in reads:
            add(t.w)
        for t in writes:
            add(t.w)
            for ev in t.r:
                add(ev)
        for ev in extra:
            add(ev)
        waits = []
        own = id(e.sem)
        for k, v in need.items():
            if k == own and v > e.cnt:
                continue
            if e.seen.get(k, 0) < v:
                e.seen[k] = v
                waits.append((self.semid[k], v))
        return waits

    @staticmethod
    def _mark(ev, reads, writes):
        for t in reads:
            t.r.append(ev)
        for t in writes:
            t.w = ev
            t.r = []

    def op(self, en, fn, reads=(), writes=(), signal=True, extra=()):
        e = self.eng[en]
        waits = self._deps(e, reads, writes, extra)
        sem = e.sem
        if signal:
            e.cnt += 1
            ev = (sem, e.cnt)
        else:
            ev = (sem, e.cnt + 1)

        def run(eng, waits=waits, fn=fn, sem=sem, signal=signal):
            for s_, v_ in waits:
                eng.wait_ge(s_, v_)
            ins = fn(eng)
            if signal:
                ins.then_inc(sem, 1)

        e.prog.append(run)
        self._mark(ev, reads, writes)
        return ev

    def dma(self, qn, fn, owner, reads=(), writes=(), extra=()):
        e = self.eng[qn]
        waits = self._deps(e, reads, writes, extra)
        if owner.dsem is None:
            owner.dsem = self.stack.enter_context(self.nc.semaphore("d%d" % len(self.dtiles)))
            self.dtiles.append(owner)
        owner.dcnt += 16
        sem = owner.dsem
        ev = (sem, owner.dcnt)

        def run(eng, waits=waits, fn=fn, sem=sem):
            for s_, v_ in waits:
                eng.wait_ge(s_, v_)
            fn(eng).then_inc(sem, 16)

        e.prog.append(run)
        self._mark(ev, reads, writes)
        return ev

    def barrier(self):
        self.nbar += 1
        target = 5 * self.nbar
        bar = self.bar
        for n, e in self.eng.items():
            waits = []
            if n != "sync" and e.cnt > 0:
                waits.append((e.sem, e.cnt))
            if n == "sync":
                for t in self.dtiles:
                    waits.append((t.dsem, t.dcnt))

            def run(eng, waits=waits, target=target):
                for s_, v_ in waits:
                    eng.wait_ge(s_, v_)
                eng.sem_inc(bar, 1)
                eng.wait_ge(bar, target)

            e.prog.append(run)

    def finish(self, final_events):
        for n, e in self.eng.items():
            waits = []
            if n != "sync" and e.cnt > 0:
                waits.append((e.sem, e.cnt))
            if n == "sync":
                for t in self.dtiles:
                    waits.append((t.dsem, t.dcnt))

            def run(eng, waits=waits):
                for s_, v_ in waits:
                    eng.wait_ge(s_, v_)

            e.prog.append(run)


def build(debug=False, upto="D"):
    from contextlib import ExitStack

    nc = bass.Bass("TRN2", target_bir_lowering=False)

    def din(name, shape, dt=F32):
        return nc.dram_tensor(name, list(shape), dt, kind="ExternalInput")

    x_d = din("x", [S, D])
    xT_d = din("xT", [D, S])
    pT_d = din("pT", [256, S])
    w_in_d = din("w_in", [D, 2048])
    pmix_d = din("pool_mix", [4, 128, 128])
    pscale_d = din("pool_scale_t", [128, 4])
    convw_d = din("conv_w_t", [128, 12])
    w_out_d = din("w_out", [D, D])
    ln_d = din("ln_all", [6, D])
    rw_d = din("router_w", [D, E])
    rb_d = din("router_b", [1, E])
    wgu_d = din("w_gate_up", [E, D, 2048])
    bgu_d = din("b_gu_t", [128, E * 16])
    wdn_d = din("w_down", [E, D, D])
    bdn_d = din("b_down", [E, D])
    pproj_d = din("ple_proj", [256, D])
    pgw_d = din("ple_gate_w", [D, D])
    pgb_d = din("ple_gate_b", [1, D])
    ident_d = din("c_ident", [128, 128])
    tri_d = din("c_tri", [128, 128])
    ecol_d = din("c_ecol", [128, E])
    pfix_d = din("c_poolfix", [128, 64])
    out_d = nc.dram_tensor("out", [S, D], F32, kind="ExternalOutput")
    skind = "ExternalOutput" if debug else "Internal"
    XE = nc.dram_tensor("XE", [XR, D], BF16, kind=skind)
    YE = nc.dram_tensor("YE", [XR, D], F32, kind=skind)
    H1F = nc.dram_tensor("H1F", [S, D], F32, kind=skind)
    if debug:
        DBG_ROW = nc.dram_tensor("DBG_ROW", [128, NT * 4], I32, kind="ExternalOutput")
        DBG_GATE = nc.dram_tensor("DBG_GATE", [128, NT * 4], F32, kind="ExternalOutput")

    with ExitStack() as stack:
        abase = (nc._sbuf_addr_for_side("left") + 31) // 32 * 32
        arena = stack.enter_context(nc.sbuf_tensor("arena", [128, SBUF_BYTES], mybir.dt.uint8))
        assert nc._sbuf_addr_for_side("left") == abase + SBUF_BYTES, (abase, nc._sbuf_addr_for_side("left"))
        k = K(nc, stack)
        k.base = abase

        regs = {}

        def mkreg(eng):
            r = eng.alloc_register("bc")
            eng.reg_mov(r, XR - 1)
            regs["bc"] = r

        k.eng["gpsimd"].prog.append(mkreg)

        pb = [T(nc.alloc_psum_tensor("pb%d" % i, [128, 512], F32).ap()) for i in range(4)]
        pw = T(nc.alloc_psum_tensor("pw", [128, 1024], F32).ap())
        pt = T(nc.alloc_psum_tensor("pt", [128, 1024], BF16).ap())
        pl = T(nc.alloc_psum_tensor("pl", [128, 512], F32).ap())

        ident = k.tile([128, 128], BF16, "ident")
        tri = k.tile([128, 128], BF16, "tri")
        ones = k.tile([128, 128], BF16, "ones")
        ecol = k.tile([128, 1, E], F32, "ecol")
        ROWI = k.tile([128, NT, 4], I32, "rowi")
        GATE = k.tile([128, NT, 4], F32, "gate")
        base_run = k.tile([128, E], F32, "base")
        cstage = k.tile([128, 128], F32, "cstage")
        cstage2 = k.tile([128, 128], F32, "cstage2")
        persist_end = k.off

        def load_cast(dst_t, src_ap, stage_t, q="sync", ce="vector"):
            k.dma(q, lambda g: g.dma_start(out=stage_t.ap, in_=src_ap), stage_t, writes=[stage_t])
            k.op(ce, lambda g: g.tensor_copy(out=dst_t.ap, in_=stage_t.ap), reads=[stage_t], writes=[dst_t])

        load_cast(ident, ident_d.ap(), cstage)
        load_cast(tri, tri_d.ap(), cstage2)
        k.op("vector", lambda g: g.memset(ones.ap, 1.0), writes=[ones])
        k.dma("sync", lambda g: g.dma_start(out=ecol.ap, in_=ecol_d.ap().rearrange("p (o e) -> p o e", o=1)), ecol, writes=[ecol])
        k.op("vector", lambda g: g.memset(base_run.ap, 0.0), writes=[base_run])
        k.op("vector", lambda g: g.memset(ROWI.ap, 0), writes=[ROWI])
        k.op("vector", lambda g: g.memset(GATE.ap, 0.0), writes=[GATE])

        def layer_norm(z, G, Bv, out, st, mv, rs, nm, mul_eng="gpsimd"):
            k.op("vector", lambda g: g.bn_stats(out=st.ap[:, 0:6], in_=z.ap[:, 0:512]), reads=[z], writes=[st])
            k.op("vector", lambda g: g.bn_stats(out=st.ap[:, 6:12], in_=z.ap[:, 512:1024]), reads=[z, st], writes=[st])
            k.op("vector", lambda g: g.bn_aggr(out=mv.ap, in_=st.ap), reads=[st], writes=[mv])
            k.op("scalar", lambda g: g.activation(out=rs.ap, in_=mv.ap[:, 1:2], func=AF.Sqrt, bias=EPS, scale=1.0), reads=[mv], writes=[rs])
            k.op("vector", lambda g: g.reciprocal(out=rs.ap, in_=rs.ap), reads=[rs], writes=[rs])
            k.op("vector", lambda g: g.tensor_scalar(out=nm.ap, in0=mv.ap[:, 0:1], scalar1=rs.ap, scalar2=-1.0,
                                                      op0=ALU.mult, op1=ALU.mult), reads=[mv, rs], writes=[nm])
            k.op("scalar", lambda g: g.activation(out=z.ap, in_=z.ap, func=AF.Identity, bias=nm.ap, scale=rs.ap),
                 reads=[z, nm, rs], writes=[z])
            k.op(mul_eng, lambda g: g.tensor_tensor(out=z.ap, in0=z.ap, in1=G.ap, op=ALU.mult), reads=[z, G], writes=[z])
            k.op("vector", lambda g: g.tensor_tensor(out=out.ap, in0=z.ap, in1=Bv.ap, op=ALU.add), reads=[z, Bv], writes=[out])

        def transpose8(src_bf, dstT, copy_eng, out_ap=None):
            if out_ap is None:
                out_ap = dstT.ap
            pt3 = pt.ap.rearrange("p (k t) -> p k t", k=8)
            for kk in range(8):
                k.op("tensor", lambda g, kk=kk: g.transpose(out=pt.ap[:, kk * 128:(kk + 1) * 128],
                                                            in_=src_bf.ap[:, kk * 128:(kk + 1) * 128], identity=ident.ap),
                     reads=[src_bf, ident], writes=[pt], signal=(kk == 7))
            if copy_eng == "scalar":
                k.op("scalar", lambda g: g.copy(out=out_ap, in_=pt3), reads=[pt], writes=[dstT])
            else:
                k.op(copy_eng, lambda g: g.tensor_copy(out=out_ap, in_=pt3), reads=[pt], writes=[dstT])

        k.off = persist_end
        w_in = [k.tile([128, 2048], BF16, "w_in%d" % i) for i in range(8)]
        w_out = [k.tile([128, 1024], BF16, "w_out%d" % i) for i in range(8)]
        pmix = k.tile([128, 4, 128], BF16, "pmix")
        rw = k.tile([128, 8, E], BF16, "rw")
        rb = k.tile([128, E], F32, "rb")
        pscale = k.tile([128, 4], F32, "pscale")
        convw = k.tile([128, 12], F32, "convw")
        pfix = k.tile([128, 64], F32, "pfix")
        G1 = k.tile([128, D], F32, "G1")
        B1 = k.tile([128, D], F32, "B1")
        wst = [k.tile([128, 1024], F32, "wst%d" % i) for i in range(3)]
        zero = k.tile([128, 2048], BF16, "zero")
        xs = k.tile([128, 8, 512], F32, "xs")
        xb = [k.tile([128, 8, 512], BF16, "xb0")] * 2
        vp = [[k.tile([128, 528], F32, "vp%d" % g_)] * 2 for g_ in range(4)]
        ptmp = [k.tile([128, 528], F32, "ptmp%d" % i) for i in range(2)]
        db = [k.tile([128, 512], BF16, "db%d" % i) for i in range(2)]
        ub = [[k.tile([128, 514], F32, "ub%d" % j)] * 2 for j in range(4)]
        cu = k.tile([128, 512], F32, "cu")
        ctmp = [k.tile([128, 512], F32, "ctmp%d" % i) for i in range(2)]
        mixT = [[k.tile([128, 512], BF16, "mixT%d" % c) for c in range(8)]] * 2
        xt = [k.tile([128, D], F32, "xt%d" % i) for i in range(2)]
        zt = [k.tile([128, D], F32, "zt%d" % i) for i in range(2)]
        h1 = [k.tile([128, D], F32, "h1_%d" % i) for i in range(2)]
        h1b = [[k.tile([128, D], BF16, "h1b%d_%d" % (i, j)) for j in range(4)] for i in range(2)]
        h1T = [k.tile([128, 8, 128], BF16, "h1T%d" % i) for i in range(2)]
        st = k.tile([128, 12], F32, "st")
        mv = k.tile([128, 2], F32, "mv")
        rs = k.tile([128, 1], F32, "rs")
        nm = k.tile([128, 1], F32, "nm")
        Lm = [k.tile([128, 128], F32, "Lm%d" % i) for i in range(2)]
        m8 = k.tile([128, 4, 8], F32, "m8")
        maskb = k.tile([128, 128], BF16, "maskb")
        d4 = k.tile([128, 4, 4], F32, "d4")
        e4 = k.tile([128, 4, 4], F32, "e4")
        s4 = k.tile([128, 4, 1], F32, "s4")
        r4 = k.tile([128, 4, 1], F32, "r4")
        Rk = k.tile([128, 128], F32, "Rk")
        pen = k.tile([128, 128], F32, "pen")
        oh = k.tile([128, 128], F32, "oh")
        rowf = k.tile([128, 4, 4], F32, "rowf")
        print("phase A sbuf bytes", k.off)

        k.op("gpsimd", lambda g: g.memset(zero.ap, 0.0), writes=[zero])
        zf_ev = None
        for n in range(XR // 256):
            zf_ev = k.dma("scalar", lambda g, n=n: g.dma_start(
                out=XE[n * 256:(n + 1) * 256, :].rearrange("(p r) d -> p (r d)", p=128), in_=zero.ap), zero, reads=[zero])

        k.dma("sync", lambda g: g.dma_start(out=pscale.ap, in_=pscale_d.ap()), pscale, writes=[pscale])
        k.dma("sync", lambda g: g.dma_start(out=convw.ap, in_=convw_d.ap()), convw, writes=[convw])
        k.dma("sync", lambda g: g.dma_start(out=pfix.ap, in_=pfix_d.ap()), pfix, writes=[pfix])
        k.dma("sync", lambda g: g.dma_start(out=rb.ap, in_=rb_d.ap().partition_broadcast(128)), rb, writes=[rb])
        k.dma("sync", lambda g: g.dma_start(out=G1.ap, in_=ln_d[0:1, :].partition_broadcast(128)), G1, writes=[G1])
        k.dma("sync", lambda g: g.dma_start(out=B1.ap, in_=ln_d[1:2, :].partition_broadcast(128)), B1, writes=[B1])
        ci = 0
        ces = ["vector", "gpsimd", "scalar"]

        def cast(eng, dst_ap, src_t, dst_t):
            if eng == "scalar":
                k.op("scalar", lambda g: g.copy(out=dst_ap, in_=src_t.ap), reads=[src_t], writes=[dst_t])
            else:
                k.op(eng, lambda g: g.tensor_copy(out=dst_ap, in_=src_t.ap), reads=[src_t], writes=[dst_t])

        for kk in range(8):
            for hf in range(2):
                s_ = wst[ci % 3]
                k.dma("sync", lambda g, kk=kk, hf=hf, s_=s_: g.dma_start(
                    out=s_.ap, in_=w_in_d[kk * 128:(kk + 1) * 128, hf * 1024:(hf + 1) * 1024]), s_, writes=[s_])
                cast(ces[ci % 3], w_in[kk].ap[:, hf * 1024:(hf + 1) * 1024], s_, w_in[kk])
                ci += 1
        for kk in range(8):
            s_ = wst[ci % 3]
            k.dma("sync", lambda g, kk=kk, s_=s_: g.dma_start(out=s_.ap, in_=w_out_d[kk * 128:(kk + 1) * 128, :]), s_, writes=[s_])
            cast(ces[ci % 3], w_out[kk].ap, s_, w_out[kk])
            ci += 1
        s_ = wst[ci % 3]
        k.dma("sync", lambda g, s_=s_: g.dma_start(out=s_.ap[:, 0:512].rearrange("p (g c) -> p g c", g=4),
                                                  in_=pmix_d.ap().rearrange("g p c -> p g c")), s_, writes=[s_])
        k.op("vector", lambda g, s_=s_: g.tensor_copy(out=pmix.ap.rearrange("p g c -> p (g c)"), in_=s_.ap[:, 0:512]), reads=[s_], writes=[pmix])
        ci += 1
        s_ = wst[ci % 3]
        k.dma("sync", lambda g, s_=s_: g.dma_start(out=s_.ap[:, 0:256].rearrange("p (k e) -> p k e", k=8),
                                                  in_=rw_d.ap().rearrange("(k p) e -> p k e", p=128)), s_, writes=[s_])
        k.op("vector", lambda g, s_=s_: g.tensor_copy(out=rw.ap.rearrange("p k e -> p (k e)"), in_=s_.ap[:, 0:256]), reads=[s_], writes=[rw])
        ci += 1
        for g_ in range(4):
            k.op("vector", lambda g, g_=g_: g.memset(vp[g_][0].ap[:, 0:16], 0.0), writes=[vp[g_][0]])
            k.op("vector", lambda g, g_=g_: g.memset(ub[g_][0].ap[:, 0:2], 0.0), writes=[ub[g_][0]])

        xT_v = xT_d.ap().rearrange("(k p) t -> p k t", p=128)
        pbi = [0]

        def next_pb():
            t_ = pb[pbi[0] % 3]
            pbi[0] += 1
            return t_

        def proj_chunk(xbm, fc, pbt):
            for kk in range(8):
                k.op("tensor", lambda g, kk=kk: g.matmul(pbt.ap, lhsT=w_in[kk].ap[:, fc * 128:(fc + 1) * 128],
                                                         rhs=xbm.ap[:, kk, :], start=(kk == 0), stop=(kk == 7)),
                     reads=[w_in[kk], xbm], writes=[pbt], signal=(kk == 7))

        def stage1(m):
            par = m % 2
            nxt = (m + 1) % 2
            t0 = m * 512
            xbm = xb[par]
            k.dma("sync", lambda g: g.dma_start(out=xs.ap, in_=xT_v[:, :, t0:t0 + 512]), xs, writes=[xs])
            for q in range(4):
                eng = ["vector", "scalar", "vector", "scalar"][q]
                if eng == "scalar":
                    k.op("scalar", lambda g, q=q: g.copy(out=xbm.ap[:, 2 * q:2 * q + 2, :], in_=xs.ap[:, 2 * q:2 * q + 2, :]), reads=[xs], writes=[xbm])
                else:
                    k.op(eng, lambda g, q=q: g.tensor_copy(out=xbm.ap[:, 2 * q:2 * q + 2, :], in_=xs.ap[:, 2 * q:2 * q + 2, :]), reads=[xs], writes=[xbm])
            mx = mixT[par]
            for g_ in range(4):
                w = WINS[g_]
                v = vp[g_][par]
                vn = vp[g_][nxt]
                pbt = next_pb()
                proj_chunk(xbm, g_, pbt)
                k.op("scalar", lambda g, v=v, pbt=pbt: g.copy(out=v.ap[:, 16:528], in_=pbt.ap), reads=[pbt], writes=[v])
                cur = v
                sh = 1
                lo = 0
                idx = 0
                while sh < w:
                    lo = lo + sh
                    dst = ptmp[idx % 2]
                    k.op("vector", lambda g, cur=cur, dst=dst, lo=lo, sh=sh: g.tensor_tensor(
                        out=dst.ap[:, lo:528], in0=cur.ap[:, lo:528], in1=cur.ap[:, lo - sh:528 - sh], op=ALU.add),
                        reads=[cur], writes=[dst])
                    cur = dst
                    sh *= 2
                    idx += 1
                if m == 0:
                    k.op("vector", lambda g, cur=cur, g_=g_: g.tensor_tensor(
                        out=cur.ap[:, 16:32], in0=cur.ap[:, 16:32], in1=pfix.ap[:, g_ * 16:(g_ + 1) * 16], op=ALU.mult),
                        reads=[cur, pfix], writes=[cur])
                dbt = db[g_ % 2]
                k.op("vector", lambda g, cur=cur, v=v, dbt=dbt, w=w: g.scalar_tensor_tensor(
                    out=dbt.ap, in0=cur.ap[:, 16:528], scalar=1.0 / w, in1=v.ap[:, 16:528], op0=ALU.mult, op1=ALU.subtract),
                    reads=[cur, v], writes=[dbt])
                k.op("gpsimd", lambda g, v=v, vn=vn: g.tensor_copy(out=vn.ap[:, 0:16], in_=v.ap[:, 512:528]), reads=[v], writes=[vn])
                k.op("tensor", lambda g, g_=g_, dbt=dbt: g.matmul(pb[3].ap, lhsT=pmix.ap[:, g_, :], rhs=dbt.ap, start=True, stop=True),
                     reads=[pmix, dbt], writes=[pb[3]])
                k.op("scalar", lambda g, g_=g_: g.activation(out=mx[g_].ap, in_=pb[3].ap, func=AF.Identity, scale=pscale.ap[:, g_:g_ + 1]),
                     reads=[pb[3], pscale], writes=[mx[g_]])
            for j in range(4):
                u = ub[j][par]
                un = ub[j][nxt]
                pB = next_pb()
                proj_chunk(xbm, 4 + j, pB)
                pC = next_pb()
                proj_chunk(xbm, 8 + j, pC)
                pV = next_pb()
                proj_chunk(xbm, 12 + j, pV)
                k.op("scalar", lambda g, pC=pC: g.copy(out=cu.ap, in_=pC.ap), reads=[pC], writes=[cu])
                k.op("vector", lambda g, u=u, pV=pV: g.tensor_tensor(out=u.ap[:, 2:514], in0=cu.ap, in1=pV.ap, op=ALU.mult),
                     reads=[cu, pV], writes=[u])
                c0, c1 = ctmp
                k.op("scalar", lambda g, u=u, j=j: g.activation(out=c0.ap, in_=u.ap[:, 0:512], func=AF.Identity, scale=convw.ap[:, 3 * j:3 * j + 1]),
                     reads=[u, convw], writes=[c0])
                k.op("vector", lambda g, u=u, j=j: g.scalar_tensor_tensor(out=c1.ap, in0=u.ap[:, 1:513], scalar=convw.ap[:, 3 * j + 1:3 * j + 2],
                                                                            in1=c0.ap, op0=ALU.mult, op1=ALU.add), reads=[u, convw, c0], writes=[c1])
                k.op("vector", lambda g, u=u, j=j: g.scalar_tensor_tensor(out=c0.ap, in0=u.ap[:, 2:514], scalar=convw.ap[:, 3 * j + 2:3 * j + 3],
                                                                            in1=c1.ap, op0=ALU.mult, op1=ALU.add), reads=[u, convw, c1], writes=[c0])
                k.op("gpsimd", lambda g, u=u, un=un: g.tensor_copy(out=un.ap[:, 0:2], in_=u.ap[:, 512:514]), reads=[u], writes=[un])
                k.op("vector", lambda g, j=j, pB=pB: g.tensor_tensor(out=mx[4 + j].ap, in0=c0.ap, in1=pB.ap, op=ALU.mult),
                     reads=[c0, pB], writes=[mx[4 + j]])
            for sub in range(4):
                tl = 4 * m + sub
                xtt = xt[sub % 2]
                ztt = zt[sub % 2]
                h1t = h1[sub % 2]
                k.dma("sync", lambda g, tl=tl, xtt=xtt: g.dma_start(out=xtt.ap, in_=x_d[tl * 128:(tl + 1) * 128, :]), xtt, writes=[xtt])
                for hf in range(2):
                    for kk in range(8):
                        k.op("tensor", lambda g, kk=kk, hf=hf, sub=sub: g.matmul(
                            pw.ap[:, hf * 512:(hf + 1) * 512], lhsT=mx[kk].ap[:, sub * 128:(sub + 1) * 128],
                            rhs=w_out[kk].ap[:, hf * 512:(hf + 1) * 512], start=(kk == 0), stop=(kk == 7)),
                            reads=[mx[kk], w_out[kk]], writes=[pw], signal=(kk == 7))
                k.op("vector", lambda g, xtt=xtt, ztt=ztt: g.scalar_tensor_tensor(out=ztt.ap, in0=xtt.ap, scalar=ALPHA, in1=pw.ap,
                                                                                    op0=ALU.mult, op1=ALU.add), reads=[xtt, pw], writes=[ztt])
                layer_norm(ztt, G1, B1, h1t, st, mv, rs, nm)
                k.dma("gpsimd", lambda g, tl=tl, h1t=h1t: g.dma_start(out=H1F[tl * 128:(tl + 1) * 128, :], in_=h1t.ap), h1t, reads=[h1t])
                hb = h1b[par][sub]
                k.op("scalar", lambda g, h1t=h1t, hb=hb: g.copy(out=hb.ap, in_=h1t.ap), reads=[h1t], writes=[hb])

        def stage2(m):
            par = m % 2
            L = Lm[par]
            L3 = L.ap.rearrange("p (j e) -> p j e", e=E)
            for sub in range(4):
                hb = h1b[par][sub]
                hT = h1T[sub % 2]
                transpose8(hb, hT, "scalar" if sub % 2 else "vector")
                for kk in range(8):
                    k.op("tensor", lambda g, kk=kk, hT=hT: g.matmul(pl.ap[:, 0:E], lhsT=hT.ap[:, kk, :], rhs=rw.ap[:, kk, :],
                                                                      start=(kk == 0), stop=(kk == 7)),
                         reads=[hT, rw], writes=[pl], signal=(kk == 7))
                k.op("vector", lambda g, sub=sub: g.tensor_tensor(out=L.ap[:, sub * E:(sub + 1) * E], in0=pl.ap[:, 0:E], in1=rb.ap, op=ALU.add),
                     reads=[pl, rb], writes=[L])
            for j in range(4):
                k.op("vector", lambda g, j=j: g.max(out=m8.ap[:, j, :], in_=L.ap[:, j * E:(j + 1) * E]), reads=[L], writes=[m8])
            k.op("vector", lambda g: g.tensor_tensor(out=maskb.ap.rearrange("p (j e) -> p j e", e=E), in0=L3,
                                                      in1=m8.ap[:, :, 3:4].to_broadcast([128, 4, E]), op=ALU.is_ge),
                 reads=[L, m8], writes=[maskb])
            k.op("vector", lambda g: g.tensor_tensor(out=d4.ap, in0=m8.ap[:, :, 0:4], in1=m8.ap[:, :, 0:1].to_broadcast([128, 4, 4]),
                                                      op=ALU.subtract), reads=[m8], writes=[d4])
            k.op("scalar", lambda g: g.activation(out=e4.ap, in_=d4.ap, func=AF.Exp), reads=[d4], writes=[e4])
            k.op("vector", lambda g: g.tensor_reduce(out=s4.ap.rearrange("p j o -> p (j o)"), in_=e4.ap, axis=AX.X, op=ALU.add),
                 reads=[e4], writes=[s4])
            k.op("vector", lambda g: g.reciprocal(out=r4.ap, in_=s4.ap), reads=[s4], writes=[r4])
            k.op("vector", lambda g: g.tensor_tensor(out=GATE.ap[:, 4 * m:4 * m + 4, :], in0=e4.ap, in1=r4.ap.to_broadcast([128, 4, 4]),
                                                      op=ALU.mult), reads=[e4, r4, GATE], writes=[GATE])
            k.op("tensor", lambda g: g.matmul(pl.ap[:, 128:256], lhsT=tri.ap, rhs=maskb.ap, start=True, stop=True),
                 reads=[tri, maskb], writes=[pl])
            k.op("tensor", lambda g: g.matmul(pl.ap[:, 256:384], lhsT=ones.ap, rhs=maskb.ap, start=True, stop=True),
                 reads=[ones, maskb], writes=[pl])
            for j in range(4):
                k.op("vector", lambda g, j=j: g.tensor_tensor(out=Rk.ap[:, j * E:(j + 1) * E], in0=pl.ap[:, 128 + j * E:128 + (j + 1) * E],
                                                               in1=base_run.ap, op=ALU.add), reads=[pl, base_run, Rk], writes=[Rk])
                k.op("vector", lambda g, j=j: g.tensor_tensor(out=base_run.ap, in0=base_run.ap, in1=pl.ap[:, 256 + j * E:256 + (j + 1) * E],
                                                               op=ALU.add), reads=[pl, base_run], writes=[base_run])
            k.op("vector", lambda g: g.tensor_scalar(out=pen.ap, in0=Rk.ap, scalar1=CAP + 0.5, scalar2=1.0e6, op0=ALU.is_gt, op1=ALU.mult),
                 reads=[Rk], writes=[pen])
            Rk3 = Rk.ap.rearrange("p (j e) -> p j e", e=E)
            k.op("vector", lambda g: g.tensor_tensor(out=Rk3, in0=Rk3, in1=ecol.ap.to_broadcast([128, 4, E]), op=ALU.add),
                 reads=[Rk, ecol], writes=[Rk])
            k.op("vector", lambda g: g.tensor_tensor(out=Rk.ap, in0=Rk.ap, in1=pen.ap, op=ALU.add), reads=[Rk, pen], writes=[Rk])
            oh3 = oh.ap.rearrange("p (j e) -> p j e", e=E)
            for kq in range(4):
                k.op("vector", lambda g, kq=kq: g.tensor_tensor(out=oh3, in0=L3, in1=m8.ap[:, :, kq:kq + 1].to_broadcast([128, 4, E]),
                                                                 op=ALU.is_equal), reads=[L, m8], writes=[oh])
                k.op("vector", lambda g: g.tensor_tensor(out=oh.ap, in0=oh.ap, in1=Rk.ap, op=ALU.mult), reads=[oh, Rk], writes=[oh])
                k.op("vector", lambda g, kq=kq: g.tensor_reduce(out=rowf.ap[:, :, kq], in_=oh3, axis=AX.X, op=ALU.add),
                     reads=[oh, rowf], writes=[rowf])
            k.op("vector", lambda g: g.tensor_copy(out=ROWI.ap[:, 4 * m:4 * m + 4, :], in_=rowf.ap), reads=[rowf, ROWI], writes=[ROWI])
            for j in range(4):
                hb = h1b[par][j]
                for kq in range(4):
                    k.dma("gpsimd", lambda g, j=j, kq=kq, hb=hb: g.indirect_dma_start(
                        out=XE[:, :], out_offset=bass.IndirectOffsetOnAxis(ap=ROWI.ap[:, 4 * m + j, kq:kq + 1], axis=0),
                        in_=hb.ap, in_offset=None, bounds_check=regs["bc"], oob_is_err=False),
                        hb, reads=[hb, ROWI], extra=[zf_ev])

        import os as _os
        NMR = int(_os.environ.get("K_NM", NM))
        stage1(0)
        for m in range(1, NMR):
            stage1(m)
            stage2(m - 1)
        stage2(NMR - 1)
        if debug:
            k.dma("sync", lambda g: g.dma_start(out=DBG_ROW.ap(), in_=ROWI.ap.rearrange("p t k -> p (t k)")), ROWI, reads=[ROWI])
            k.dma("sync", lambda g: g.dma_start(out=DBG_GATE.ap(), in_=GATE.ap.rearrange("p t k -> p (t k)")), GATE, reads=[GATE])
        k.barrier()

        k.off = persist_end
        import os as _os
        upto = _os.environ.get("K_UPTO", upto)
        wgu = [[k.tile([128, 2048], BF16, "wgu%d_%d" % (i, kk)) for kk in range(8)] for i in range(2)]
        wdn = [[k.tile([128, 1024], BF16, "wdn%d_%d" % (i, kk)) for kk in range(8)] for i in range(2)]
        cst = [k.tile([128, 1024], F32, "cst%d" % i) for i in range(6)]
        bgu = k.tile([128, E * 16], F32, "bgu")
        bdn = [k.tile([128, D], F32, "bdn%d" % i) for i in range(2)]
        xblk = [[k.tile([128, D], BF16, "xblk%d_%d" % (i, j)) for j in range(NBLK)] for i in range(2)]
        XT = k.tile([128, 8, CAP], BF16, "XT")
        actT = [k.tile([128, CAP], BF16, "actT%d" % i) for i in range(8)]
        g1 = [k.tile([128, 512], F32, "g1_%d" % i) for i in range(2)]
        sg = [k.tile([128, 512], F32, "sg_%d" % i) for i in range(2)]
        u0 = [k.tile([128, 512], F32, "u0_%d" % i) for i in range(2)]
        tg = [k.tile([128, 512], F32, "tg_%d" % i) for i in range(2)]
        yb = [k.tile([128, D], F32, "yb%d" % i) for i in range(2)]
        print("phase C sbuf bytes", k.off)

        k.dma("sync", lambda g: g.dma_start(out=bgu.ap, in_=bgu_d.ap()), bgu, writes=[bgu])
        cctr = [0]
        cast_engs = ["scalar", "vector", "scalar", "vector", "scalar"]

        def weight_tasks(e):
            b = e % 2
            tasks = []
            for kk in range(8):
                for hf in range(2):
                    def tk(kk=kk, hf=hf):
                        s_ = cst[cctr[0] % 6]
                        k.dma("sync", lambda g: g.dma_start(
                            out=s_.ap, in_=wgu_d[e, kk * 128:(kk + 1) * 128, hf * 1024:(hf + 1) * 1024]), s_, writes=[s_])
                        cast(cast_engs[cctr[0] % 5], wgu[b][kk].ap[:, hf * 1024:(hf + 1) * 1024], s_, wgu[b][kk])
                        cctr[0] += 1
                    tasks.append(tk)
            for kk in range(8):
                def tk(kk=kk):
                    s_ = cst[cctr[0] % 6]
                    k.dma("sync", lambda g: g.dma_start(out=s_.ap, in_=wdn_d[e, kk * 128:(kk + 1) * 128, :]), s_, writes=[s_])
                    cast(cast_engs[cctr[0] % 5], wdn[b][kk].ap, s_, wdn[b][kk])
                    cctr[0] += 1
                tasks.append(tk)

            def tb():
                k.dma("sync", lambda g: g.dma_start(out=bdn[b].ap, in_=bdn_d[e:e + 1, :].partition_broadcast(128)), bdn[b], writes=[bdn[b]])
            tasks.append(tb)
            return tasks

        pend = []

        def pump(n):
            for _ in range(n):
                if pend:
                    pend.pop(0)()

        def load_X(e):
            for blk in range(NBLK):
                xbk = xblk[e % 2][blk]
                r0 = e * CAP + blk * 128
                k.dma("gpsimd", lambda g, xbk=xbk, r0=r0: g.dma_start(out=xbk.ap, in_=XE[r0:r0 + 128, :]), xbk, writes=[xbk])

        def do_T(e):
            for blk in range(NBLK):
                xbk = xblk[e % 2][blk]
                transpose8(xbk, XT, "scalar" if blk % 2 else "vector", out_ap=XT.ap[:, :, blk * 128:(blk + 1) * 128])

        gctr = [0]

        def gu_front(e, b, pc, unit):
            if unit == 0:
                pg, pu = (pb[0], pb[1]) if gctr[0] % 2 == 0 else (pb[2], pb[3])
                pg_ap, pu_ap = pg.ap, pu.ap
                n0, nn = 0, 512
            else:
                pg = pu = pl
                pg_ap, pu_ap = pl.ap[:, 0:128], pl.ap[:, 128:256]
                n0, nn = 512, 128
            bi = gctr[0] % 2
            gctr[0] += 1
            for pp_t, pp_ap, cbase in ((pg, pg_ap, pc * 128), (pu, pu_ap, 1024 + pc * 128)):
                for kk in range(8):
                    k.op("tensor", lambda g, kk=kk, pp_ap=pp_ap, cbase=cbase: g.matmul(
                        pp_ap, lhsT=wgu[b][kk].ap[:, cbase:cbase + 128], rhs=XT.ap[:, kk, n0:n0 + nn],
                        start=(kk == 0), stop=(kk == 7)),
                        reads=[wgu[b][kk], XT], writes=[pp_t], signal=(kk == 7))
            a_g1, a_sg, a_u0, a_tg = g1[bi], sg[bi], u0[bi], tg[bi]
            bg_ap = bgu.ap[:, e * 16 + pc:e * 16 + pc + 1]
            bu_ap = bgu.ap[:, e * 16 + 8 + pc:e * 16 + 8 + pc + 1]
            k.op("vector", lambda g: g.tensor_scalar(out=a_g1.ap[:, 0:nn], in0=pg_ap, scalar1=bg_ap, scalar2=7.0, op0=ALU.add, op1=ALU.min),
                 reads=[pg, bgu], writes=[a_g1])
            if _os.environ.get("K_VAR", "1") == "1":
                k.op("scalar", lambda g: g.activation(out=a_sg.ap[:, 0:nn], in_=a_g1.ap[:, 0:nn], func=AF.Sigmoid, scale=1.702),
                     reads=[a_g1], writes=[a_sg])
                k.op("scalar", lambda g: g.activation(out=a_u0.ap[:, 0:nn], in_=pu_ap, func=AF.Identity, bias=bu_ap),
                     reads=[pu, bgu], writes=[a_u0])
            else:
                k.op("scalar", lambda g: g.activation(out=a_u0.ap[:, 0:nn], in_=pu_ap, func=AF.Identity, bias=bu_ap),
                     reads=[pu, bgu], writes=[a_u0])
                k.op("scalar", lambda g: g.activation(out=a_sg.ap[:, 0:nn], in_=a_g1.ap[:, 0:nn], func=AF.Sigmoid, scale=1.702),
                     reads=[a_g1], writes=[a_sg])
            return (pc, n0, nn, a_g1, a_sg, a_u0, a_tg)

        def gu_back(ctx):
            pc, n0, nn, a_g1, a_sg, a_u0, a_tg = ctx
            k.op("gpsimd", lambda g: g.tensor_tensor(out=a_tg.ap[:, 0:nn], in0=a_g1.ap[:, 0:nn], in1=a_sg.ap[:, 0:nn], op=ALU.mult),
                 reads=[a_g1, a_sg], writes=[a_tg])
            k.op("vector", lambda g: g.tensor_scalar(out=a_u0.ap[:, 0:nn], in0=a_u0.ap[:, 0:nn], scalar1=7.0, scalar2=-7.0,
                                                      op0=ALU.min, op1=ALU.max), reads=[a_u0], writes=[a_u0])
            k.op("vector", lambda g: g.scalar_tensor_tensor(
                out=actT[pc].ap[:, n0:n0 + nn], in0=a_u0.ap[:, 0:nn], scalar=1.0, in1=a_tg.ap[:, 0:nn], op0=ALU.add, op1=ALU.mult),
                reads=[a_u0, a_tg, actT[pc]], writes=[actT[pc]])
            pump(1)

        def do_GU(e):
            b = e % 2
            prev = None
            for pc in range(8):
                for unit in range(2):
                    ctx = gu_front(e, b, pc, unit)
                    if _os.environ.get("K_VAR", "") == "2":
                        gu_back(ctx)
                        continue
                    if prev is not None:
                        gu_back(prev)
                    prev = ctx
            if prev is not None:
                gu_back(prev)

        yctr = [0]

        def do_DN_T(e):
            b = e % 2
            for blk in range(NBLK):
                if e + 1 < E:
                    xbk = xblk[(e + 1) % 2][blk]
                    transpose8(xbk, XT, "scalar", out_ap=XT.ap[:, :, blk * 128:(blk + 1) * 128])
                for hf in range(2):
                    for fc in range(8):
                        k.op("tensor", lambda g, fc=fc, hf=hf, blk=blk: g.matmul(
                            pw.ap[:, hf * 512:(hf + 1) * 512], lhsT=actT[fc].ap[:, blk * 128:(blk + 1) * 128],
                            rhs=wdn[b][fc].ap[:, hf * 512:(hf + 1) * 512], start=(fc == 0), stop=(fc == 7)),
                            reads=[actT[fc], wdn[b][fc]], writes=[pw], signal=(fc == 7))
                ybt = yb[yctr[0] % 2]
                yctr[0] += 1
                k.op("vector", lambda g, ybt=ybt: g.tensor_tensor(out=ybt.ap, in0=pw.ap, in1=bdn[b].ap, op=ALU.add),
                     reads=[pw, bdn[b]], writes=[ybt])
                r0 = e * CAP + blk * 128
                k.dma("gpsimd", lambda g, ybt=ybt, r0=r0: g.dma_start(out=YE[r0:r0 + 128, :], in_=ybt.ap), ybt, reads=[ybt])
                pump(2)

        if upto != "A":
            pend.extend(weight_tasks(0))
            pump(100)
            load_X(0)
            do_T(0)
        NER = int(_os.environ.get("K_NE", E))
        for e in range(NER if upto != "A" else 0):
            if e + 1 < E:
                pend.extend(weight_tasks(e + 1))
                load_X(e + 1)
            do_GU(e)
            do_DN_T(e)
            pump(100)
        k.barrier()

        k.off = persist_end
        pgw = [k.tile([128, D], BF16, "pgw%d" % i) for i in range(8)]
        pproj = [k.tile([128, D], BF16, "pproj%d" % i) for i in range(2)]
        dst_ = [k.tile([128, 1024], F32, "dst%d" % i) for i in range(3)]
        G2 = k.tile([128, D], F32, "G2")
        B2 = k.tile([128, D], F32, "B2")
        G3 = k.tile([128, D], F32, "G3")
        B3 = k.tile([128, D], F32, "B3")
        pgb = k.tile([128, D], F32, "pgb")
        yk = [[k.tile([128, D], F32, "yk%d_%d" % (i, q)) for q in range(4)] for i in range(3)]
        h1r = [k.tile([128, D], F32, "h1r%d" % i) for i in range(3)]
        acc = [k.tile([128, D], F32, "acc%d" % i) for i in range(2)]
        h2 = [k.tile([128, D], F32, "h2_%d" % i) for i in range(3)]
        h2b = [k.tile([128, D], BF16, "h2b%d" % i) for i in range(2)]
        h2T = [k.tile([128, 8, 128], BF16, "h2T%d" % i) for i in range(2)]
        pts = [k.tile([128, 2, 128], F32, "pts%d" % i) for i in range(3)]
        ptb = [k.tile([128, 2, 128], BF16, "ptb%d" % i) for i in range(3)]
        gs = [k.tile([128, D], F32, "gs%d" % i) for i in range(2)]
        z3 = [k.tile([128, D], F32, "z3_%d" % i) for i in range(2)]
        ot = [k.tile([128, D], F32, "ot%d" % i) for i in range(2)]
        st2 = k.tile([128, 12], F32, "st2")
        mv2 = k.tile([128, 2], F32, "mv2")
        rs2 = k.tile([128, 1], F32, "rs2")
        nm2 = k.tile([128, 1], F32, "nm2")
        st3 = k.tile([128, 12], F32, "st3")
        mv3 = k.tile([128, 2], F32, "mv3")
        rs3 = k.tile([128, 1], F32, "rs3")
        nm3 = k.tile([128, 1], F32, "nm3")
        print("phase D sbuf bytes", k.off)

        for i, (tt, row) in enumerate(((G2, 2), (B2, 3), (G3, 4), (B3, 5))):
            k.dma("sync", lambda g, tt=tt, row=row: g.dma_start(out=tt.ap, in_=ln_d[row:row + 1, :].partition_broadcast(128)), tt, writes=[tt])
        k.dma("sync", lambda g: g.dma_start(out=pgb.ap, in_=pgb_d.ap().partition_broadcast(128)), pgb, writes=[pgb])
        ci = 0
        for kk in range(8):
            s_ = dst_[ci % 3]
            k.dma("sync", lambda g, kk=kk, s_=s_: g.dma_start(out=s_.ap, in_=pgw_d[kk * 128:(kk + 1) * 128, :]), s_, writes=[s_])
            cast(ces[ci % 3], pgw[kk].ap, s_, pgw[kk])
            ci += 1
        for c in range(2):
            s_ = dst_[ci % 3]
            k.dma("sync", lambda g, c=c, s_=s_: g.dma_start(out=s_.ap, in_=pproj_d[c * 128:(c + 1) * 128, :]), s_, writes=[s_])
            cast(ces[ci % 3], pproj[c].ap, s_, pproj[c])
            ci += 1
        pT_v = pT_d.ap().rearrange("(c p) t -> p c t", p=128)

        def gatherD(t):
            b3 = t % 3
            for q in range(4):
                k.dma("gpsimd", lambda g, q=q: g.indirect_dma_start(
                    out=yk[b3][q].ap, out_offset=None, in_=YE[:, :],
                    in_offset=bass.IndirectOffsetOnAxis(ap=ROWI.ap[:, t, q:q + 1], axis=0),
                    bounds_check=regs["bc"], oob_is_err=False), yk[b3][q], reads=[ROWI], writes=[yk[b3][q]])
            k.dma("sync", lambda g: g.dma_start(out=h1r[b3].ap, in_=H1F[t * 128:(t + 1) * 128, :]), h1r[b3], writes=[h1r[b3]])
            k.dma("sync", lambda g: g.dma_start(out=pts[b3].ap, in_=pT_v[:, :, t * 128:(t + 1) * 128]), pts[b3], writes=[pts[b3]])
            k.op("gpsimd", lambda g: g.tensor_copy(out=ptb[b3].ap, in_=pts[b3].ap), reads=[pts[b3]], writes=[ptb[b3]])

        def stageD1(t):
            b = t % 2
            b3 = t % 3
            a = acc[b]
            k.op("scalar", lambda g: g.activation(out=a.ap, in_=yk[b3][0].ap, func=AF.Identity, scale=GATE.ap[:, t, 0:1]),
                 reads=[yk[b3][0], GATE], writes=[a])
            for q in range(1, 4):
                k.op("vector", lambda g, q=q: g.scalar_tensor_tensor(out=a.ap, in0=yk[b3][q].ap, scalar=GATE.ap[:, t, q:q + 1], in1=a.ap,
                                                                      op0=ALU.mult, op1=ALU.add), reads=[yk[b3][q], GATE, a], writes=[a])
            k.op("vector", lambda g: g.scalar_tensor_tensor(out=a.ap, in0=h1r[b3].ap, scalar=ALPHA, in1=a.ap, op0=ALU.mult, op1=ALU.add),
                 reads=[h1r[b3], a], writes=[a])
            h2t = h2[t % 3]
            layer_norm(a, G2, B2, h2t, st2, mv2, rs2, nm2)
            k.op("scalar", lambda g: g.copy(out=h2b[b].ap, in_=h2t.ap), reads=[h2t], writes=[h2b[b]])

        def stageD2(t):
            b = t % 2
            h2t = h2[t % 3]
            transpose8(h2b[b], h2T[b], "scalar")
            for hf in range(2):
                for kk in range(8):
                    k.op("tensor", lambda g, kk=kk, hf=hf: g.matmul(pw.ap[:, hf * 512:(hf + 1) * 512], lhsT=h2T[b].ap[:, kk, :],
                                                                      rhs=pgw[kk].ap[:, hf * 512:(hf + 1) * 512], start=(kk == 0), stop=(kk == 7)),
                         reads=[h2T[b], pgw[kk]], writes=[pw], signal=(kk == 7))
            pp = (pb[0], pb[1]) if b == 0 else (pb[2], pb[3])
            for hf in range(2):
                for c in range(2):
                    k.op("tensor", lambda g, c=c, hf=hf: g.matmul(pp[hf].ap, lhsT=ptb[t % 3].ap[:, c, :], rhs=pproj[c].ap[:, hf * 512:(hf + 1) * 512],
                                                                    start=(c == 0), stop=(c == 1)),
                         reads=[ptb[t % 3], pproj[c]], writes=[pp[hf]], signal=(c == 1))
            g_ = gs[b]
            k.op("vector", lambda g: g.tensor_tensor(out=g_.ap, in0=pw.ap, in1=pgb.ap, op=ALU.add), reads=[pw, pgb], writes=[g_])
            k.op("scalar", lambda g: g.activation(out=g_.ap, in_=g_.ap, func=AF.Sigmoid), reads=[g_], writes=[g_])
            for hf in range(2):
                k.op("vector", lambda g, hf=hf: g.tensor_tensor(out=g_.ap[:, hf * 512:(hf + 1) * 512], in0=g_.ap[:, hf * 512:(hf + 1) * 512],
                                                                 in1=pp[hf].ap, op=ALU.mult), reads=[g_, pp[hf]], writes=[g_])
            z = z3[b]
            k.op("vector", lambda g: g.scalar_tensor_tensor(out=z.ap, in0=h2t.ap, scalar=ALPHA, in1=g_.ap, op0=ALU.mult, op1=ALU.add),
                 reads=[h2t, g_], writes=[z])
            o = ot[b]
            layer_norm(z, G3, B3, o, st3, mv3, rs3, nm3)
            return k.dma("sync", lambda g: g.dma_start(out=out_d[t * 128:(t + 1) * 128, :], in_=o.ap), o, reads=[o])

        if upto == "D":
            gatherD(0)
            gatherD(1)
            stageD1(0)
            for t in range(1, NT):
                if t + 1 < NT:
                    gatherD(t + 1)
                stageD1(t)
                stageD2(t - 1)
            stageD2(NT - 1)
        k.finish(None)

        with nc.Block() as block:
            @block.sync
            def _(eng):
                for f in k.eng["sync"].prog:
                    f(eng)

            @block.scalar
            def _(eng):
                for f in k.eng["scalar"].prog:
                    f(eng)

            @block.vector
            def _(eng):
                for f in k.eng["vector"].prog:
                    f(eng)

            @block.gpsimd
            def _(eng):
                for f in k.eng["gpsimd"].prog:
                    f(eng)

            @block.tensor
            def _(eng):
                for f in k.eng["tensor"].prog:
                    f(eng)
    return nc


def make_consts():
    ident = np.eye(128, dtype=np.float32)
    tri = np.triu(np.ones((128, 128), dtype=np.float32))
    ecol = np.tile((np.arange(E, dtype=np.float32) * CAP - 1.0)[None, :], (128, 1))
    pfix = np.ones((128, 4, 16), dtype=np.float32)
    for g_, w in enumerate(WINS):
        for t in range(16):
            pfix[:, g_, t] = w / min(t + 1, w)
    return ident, tri, ecol, pfix.reshape(128, 64)


def make_in_maps(inp):
    f = lambda a: np.ascontiguousarray(np.asarray(a, dtype=np.float32))
    x = f(inp["x"])
    p = f(inp["p"])[0]
    ident, tri, ecol, pfix = make_consts()
    shared = {
        "w_in": f(inp["w_in"][0]),
        "pool_mix": f(inp["pool_mix"][0]),
        "pool_scale_t": f(np.asarray(inp["pool_scale"][0]).reshape(4, 128).T),
        "conv_w_t": f(np.asarray(inp["conv_w"][0]).reshape(3, 4, 128).transpose(2, 1, 0).reshape(128, 12)),
        "w_out": f(inp["w_out"][0]),
        "ln_all": f(np.stack([np.asarray(inp[n][0]) for n in ("ln1_g", "ln1_b", "ln2_g", "ln2_b", "ln3_g", "ln3_b")], 0)),
        "router_w": f(inp["router_w"][0]),
        "router_b": f(np.asarray(inp["router_b"][0]).reshape(1, E)),
        "w_gate_up": f(inp["w_gate_up"][0]),
        "b_gu_t": f(np.asarray(inp["b_gate_up"][0]).reshape(E, 16, 128).transpose(2, 0, 1).reshape(128, E * 16)),
        "w_down": f(inp["w_down"][0]),
        "b_down": f(inp["b_down"][0]),
        "ple_proj": f(inp["ple_proj"][0]),
        "ple_gate_w": f(inp["ple_gate_w"][0]),
        "ple_gate_b": f(np.asarray(inp["ple_gate_b"][0]).reshape(1, D)),
        "c_ident": ident, "c_tri": tri, "c_ecol": ecol, "c_poolfix": pfix,
    }
    maps = []
    for b in range(8):
        m = dict(shared)
        m["x"] = f(x[b])
        m["xT"] = f(x[b].T)
        m["pT"] = f(p[b].T)
        maps.append(m)
    return maps


def kernel(**inputs):
    nc = build(debug=False)
    in_maps = make_in_maps(inputs)
    res = run_bass_kernel_spmd(nc, in_maps, core_ids=list(range(8)))
    out = np.stack([np.asarray(r["out"], dtype=np.float32).reshape(S, D) for r in res.results], 0)
    return out
```

```python
import numpy as np
import concourse.bass as bass
import concourse.mybir as mybir
from concourse.bass_utils import run_bass_kernel_spmd

F32 = mybir.dt.float32
BF16 = mybir.dt.bfloat16
I32 = mybir.dt.int32
ALU = mybir.AluOpType
AF = mybir.ActivationFunctionType
AX = mybir.AxisListType

S = 4096
D = 1024
NT = S // 128
NM = S // 512
E = 32
CAP = 640
NBLK = CAP // 128
XR = E * CAP
ALPHA = float(2.0 ** 0.25)
EPS = 1e-5
WINS = (2, 4, 8, 16)
SBUF_BYTES = 206 * 1024


class T:
    def __init__(self, ap):
        self.ap = ap
        self.w = None
        self.r = []
        self.dsem = None
        self.dcnt = 0

    def __getitem__(self, k):
        return self.ap[k]


class Eng:
    def __init__(self, name, sem):
        self.name = name
        self.sem = sem
        self.cnt = 0
        self.seen = {}
        self.prog = []


class K:
    def __init__(self, nc, stack):
        self.nc = nc
        self.stack = stack
        self.eng = {}
        for n in ("tensor", "vector", "scalar", "gpsimd", "sync"):
            self.eng[n] = Eng(n, stack.enter_context(nc.semaphore("c_" + n)))
        self.bar = stack.enter_context(nc.semaphore("bar"))
        self.nbar = 0
        self.dtiles = []
        self.off = 0
        self.nalloc = 0
        self.semid = {}

    def sb(self, shape, dt, name=None):
        self.nalloc += 1
        nb = int(np.prod(shape[1:])) * (2 if dt == BF16 else 4)
        self.off = (self.off + 31) // 32 * 32
        assert self.off + nb <= SBUF_BYTES, (self.off, nb, name)
        h = self.nc.alloc_sbuf_tensor_at("t%d_%s" % (self.nalloc, name or ""), list(shape), dt, offset=self.base + self.off)
        self.off += nb
        return h

    def tile(self, shape, dt, name=None):
        h = self.sb(shape, dt, name)
        return T(h.ap())

    def view(self, ap):
        return T(ap)

    def _sid(self, sem):
        k = id(sem)
        if k not in self.semid:
            self.semid[k] = sem
        return k

    def _deps(self, e, reads, writes, extra):
        need = {}

        def add(ev):
            if ev is None:
                return
            k = self._sid(ev[0])
            if need.get(k, 0) < ev[1]:
                need[k] = ev[1]

        for t in reads:
            add(t.w)
        for t in writes:
            add(t.w)
            for ev in t.r:
                add(ev)
        for ev in extra:
            add(ev)
        waits = []
        own = id(e.sem)
        for k, v in need.items():
            if k == own and v > e.cnt:
                continue
            if e.seen.get(k, 0) < v:
                e.seen[k] = v
                waits.append((self.semid[k], v))
        return waits

    @staticmethod
    def _mark(ev, reads, writes):
        for t in reads:
            t.r.append(ev)
        for t in writes:
            t.w = ev
            t.r = []

    def op(self, en, fn, reads=(), writes=(), signal=True, extra=()):
        e = self.eng[en]
        waits = self._deps(e, reads, writes, extra)
        sem = e.sem
        if signal:
            e.cnt += 1
            ev = (sem, e.cnt)
        else:
            ev = (sem, e.cnt + 1)

        def run(eng, waits=waits, fn=fn, sem=sem, signal=signal):
            for s_, v_ in waits:
                eng.wait_ge(s_, v_)
            ins = fn(eng)
            if signal:
                ins.then_inc(sem, 1)

        e.prog.append(run)
        self._mark(ev, reads, writes)
        return ev

    def dma(self, qn, fn, owner, reads=(), writes=(), extra=()):
        e = self.eng[qn]
        waits = self._deps(e, reads, writes, extra)
        if owner.dsem is None:
            owner.dsem = self.stack.enter_context(self.nc.semaphore("d%d" % len(self.dtiles)))
            self.dtiles.append(owner)
        owner.dcnt += 16
        sem = owner.dsem
        ev = (sem, owner.dcnt)

        def run(eng, waits=waits, fn=fn, sem=sem):
            for s_, v_ in waits:
                eng.wait_ge(s_, v_)
            fn(eng).then_inc(sem, 16)

        e.prog.append(run)
        self._mark(ev, reads, writes)
        return ev

    def barrier(self):
        self.nbar += 1
        target = 5 * self.nbar
        bar = self.bar
        for n, e in self.eng.items():
            waits = []
            if n != "sync" and e.cnt > 0:
                waits.append((e.sem, e.cnt))
            if n == "sync":
                for t in self.dtiles:
                    waits.append((t.dsem, t.dcnt))

            def run(eng, waits=waits, target=target):
                for s_, v_ in waits:
                    eng.wait_ge(s_, v_)
                eng.sem_inc(bar, 1)
                eng.wait_ge(bar, target)

            e.prog.append(run)

    def finish(self, final_events):
        for n, e in self.eng.items():
            waits = []
            if n != "sync" and e.cnt > 0:
                waits.append((e.sem, e.cnt))
            if n == "sync":
                for t in self.dtiles:
                    waits.append((t.dsem, t.dcnt))

            def run(eng, waits=waits):
                for s_, v_ in waits:
                    eng.wait_ge(s_, v_)

            e.prog.append(run)


def build(debug=False, upto="D"):
    import os as _os
    from contextlib import ExitStack

    nc = bass.Bass("TRN2", target_bir_lowering=False)

    def din(name, shape, dt=F32):
        return nc.dram_tensor(name, list(shape), dt, kind="ExternalInput")

    x_d = din("x", [S, D])
    xT_d = din("xT", [D, S])
    pT_d = din("pT", [256, S])
    w_in_d = din("w_in", [D, 2048])
    pmix_d = din("pool_mix", [4, 128, 128])
    pscale_d = din("pool_scale_t", [128, 4])
    convw_d = din("conv_w_t", [128, 12])
    w_out_d = din("w_out", [D, D])
    ln_d = din("ln_all", [6, D])
    rw_d = din("router_w", [D, E])
    rb_d = din("router_b", [1, E])
    wgu_d = din("w_gate_up", [E, D, 2048])
    bgu_d = din("b_gu_t", [128, E * 16])
    wdn_d = din("w_down", [E, D, D])
    bdn_d = din("b_down", [E, D])
    pproj_d = din("ple_proj", [256, D])
    pgw_d = din("ple_gate_w", [D, D])
    pgb_d = din("ple_gate_b", [1, D])
    ident_d = din("c_ident", [128, 128])
    tri_d = din("c_tri", [128, 128])
    ecol_d = din("c_ecol", [128, E])
    pfix_d = din("c_poolfix", [128, 64])
    out_d = nc.dram_tensor("out", [S, D], F32, kind="ExternalOutput")
    skind = "ExternalOutput" if debug else "Internal"
    XE = nc.dram_tensor("XE", [XR, D], BF16, kind=skind)
    YE = nc.dram_tensor("YE", [XR, D], F32, kind=skind)
    H1F = nc.dram_tensor("H1F", [S, D], F32, kind=skind)
    if debug:
        DBG_ROW = nc.dram_tensor("DBG_ROW", [128, NT * 4], I32, kind="ExternalOutput")
        DBG_GATE = nc.dram_tensor("DBG_GATE", [128, NT * 4], F32, kind="ExternalOutput")

    with ExitStack() as stack:
        abase = (nc._sbuf_addr_for_side("left") + 31) // 32 * 32
        arena = stack.enter_context(nc.sbuf_tensor("arena", [128, SBUF_BYTES], mybir.dt.uint8))
        assert nc._sbuf_addr_for_side("left") == abase + SBUF_BYTES, (abase, nc._sbuf_addr_for_side("left"))
        k = K(nc, stack)
        k.base = abase

        regs = {}

        def mkreg(eng):
            r = eng.alloc_register("bc")
            eng.reg_mov(r, XR - 1)
            regs["bc"] = r

        k.eng["gpsimd"].prog.append(mkreg)

        pb = [T(nc.alloc_psum_tensor("pb%d" % i, [128, 512], F32).ap()) for i in range(4)]
        pw = T(nc.alloc_psum_tensor("pw", [128, 1024], F32).ap())
        pt = T(nc.alloc_psum_tensor("pt", [128, 1024], BF16).ap())
        pl = T(nc.alloc_psum_tensor("pl", [128, 512], F32).ap())

        ident = k.tile([128, 128], BF16, "ident")
        tri = k.tile([128, 128], BF16, "tri")
        ones = k.tile([128, 128], BF16, "ones")
        ecol = k.tile([128, 1, E], F32, "ecol")
        ROWI = k.tile([128, NT, 4], I32, "rowi")
        GATE = k.tile([128, NT, 4], F32, "gate")
        base_run = k.tile([128, E], F32, "base")
        cstage = k.tile([128, 128], F32, "cstage")
        cstage2 = k.tile([128, 128], F32, "cstage2")
        persist_end = k.off

        def load_cast(dst_t, src_ap, stage_t, q="sync", ce="vector"):
            k.dma(q, lambda g: g.dma_start(out=stage_t.ap, in_=src_ap), stage_t, writes=[stage_t])
            k.op(ce, lambda g: g.tensor_copy(out=dst_t.ap, in_=stage_t.ap), reads=[stage_t], writes=[dst_t])

        load_cast(ident, ident_d.ap(), cstage)
        load_cast(tri, tri_d.ap(), cstage2)
        k.op("vector", lambda g: g.memset(ones.ap, 1.0), writes=[ones])
        k.dma("sync", lambda g: g.dma_start(out=ecol.ap, in_=ecol_d.ap().rearrange("p (o e) -> p o e", o=1)), ecol, writes=[ecol])
        k.op("vector", lambda g: g.memset(base_run.ap, 0.0), writes=[base_run])
        k.op("vector", lambda g: g.memset(ROWI.ap, 0), writes=[ROWI])
        k.op("vector", lambda g: g.memset(GATE.ap, 0.0), writes=[GATE])

        def interleave(*gens):
            gens = list(gens)
            if _os.environ.get("K_SEQ", "") == "1":
                for g_ in gens:
                    for _ in g_:
                        pass
                return
            while gens:
                for g_ in list(gens):
                    try:
                        next(g_)
                    except StopIteration:
                        gens.remove(g_)

        def layer_norm(z, G, Bv, out, st, mv, rs, nm, mul_eng="gpsimd"):
            k.op("vector", lambda g: g.bn_stats(out=st.ap[:, 0:6], in_=z.ap[:, 0:512]), reads=[z], writes=[st])
            k.op("vector", lambda g: g.bn_stats(out=st.ap[:, 6:12], in_=z.ap[:, 512:1024]), reads=[z, st], writes=[st])
            k.op("vector", lambda g: g.bn_aggr(out=mv.ap, in_=st.ap), reads=[st], writes=[mv])
            yield
            k.op("scalar", lambda g: g.activation(out=rs.ap, in_=mv.ap[:, 1:2], func=AF.Sqrt, bias=EPS, scale=1.0), reads=[mv], writes=[rs])
            yield
            k.op("vector", lambda g: g.reciprocal(out=rs.ap, in_=rs.ap), reads=[rs], writes=[rs])
            k.op("vector", lambda g: g.tensor_scalar(out=nm.ap, in0=mv.ap[:, 0:1], scalar1=rs.ap, scalar2=-1.0,
                                                      op0=ALU.mult, op1=ALU.mult), reads=[mv, rs], writes=[nm])
            yield
            k.op("scalar", lambda g: g.activation(out=z.ap, in_=z.ap, func=AF.Identity, bias=nm.ap, scale=rs.ap),
                 reads=[z, nm, rs], writes=[z])
            yield
            k.op(mul_eng, lambda g: g.tensor_tensor(out=z.ap, in0=z.ap, in1=G.ap, op=ALU.mult), reads=[z, G], writes=[z])
            yield
            k.op("vector", lambda g: g.tensor_tensor(out=out.ap, in0=z.ap, in1=Bv.ap, op=ALU.add), reads=[z, Bv], writes=[out])
            yield

        def transpose8(src_bf, dstT, copy_eng, out_ap=None):
            if out_ap is None:
                out_ap = dstT.ap
            pt3 = pt.ap.rearrange("p (k t) -> p k t", k=8)
            for kk in range(8):
                k.op("tensor", lambda g, kk=kk: g.transpose(out=pt.ap[:, kk * 128:(kk + 1) * 128],
                                                            in_=src_bf.ap[:, kk * 128:(kk + 1) * 128], identity=ident.ap),
                     reads=[src_bf, ident], writes=[pt], signal=(kk == 7))
            if copy_eng == "scalar":
                k.op("scalar", lambda g: g.copy(out=out_ap, in_=pt3), reads=[pt], writes=[dstT])
            else:
                k.op(copy_eng, lambda g: g.tensor_copy(out=out_ap, in_=pt3), reads=[pt], writes=[dstT])

        k.off = persist_end
        w_in = [k.tile([128, 2048], BF16, "w_in%d" % i) for i in range(8)]
        w_out = [k.tile([128, 1024], BF16, "w_out%d" % i) for i in range(8)]
        pmix = k.tile([128, 4, 128], BF16, "pmix")
        rw = k.tile([128, 8, E], BF16, "rw")
        rb = k.tile([128, E], F32, "rb")
        pscale = k.tile([128, 4], F32, "pscale")
        convw = k.tile([128, 12], F32, "convw")
        pfix = k.tile([128, 64], F32, "pfix")
        G1 = k.tile([128, D], F32, "G1")
        B1 = k.tile([128, D], F32, "B1")
        wst = [k.tile([128, 1024], F32, "wst%d" % i) for i in range(3)]
        zero = k.tile([128, 2048], BF16, "zero")
        xs = k.tile([128, 8, 512], F32, "xs")
        xb = [k.tile([128, 8, 512], BF16, "xb0")] * 2
        vp = [[k.tile([128, 528], F32, "vp%d" % g_)] * 2 for g_ in range(4)]
        ptmp = [k.tile([128, 528], F32, "ptmp%d" % i) for i in range(2)]
        db = [k.tile([128, 512], BF16, "db%d" % i) for i in range(2)]
        ub = [[k.tile([128, 514], F32, "ub%d" % j)] * 2 for j in range(4)]
        cu = k.tile([128, 512], F32, "cu")
        ctmp = [k.tile([128, 512], F32, "ctmp%d" % i) for i in range(2)]
        mixT = [[k.tile([128, 512], BF16, "mixT%d" % c) for c in range(8)]] * 2
        xt = [k.tile([128, D], F32, "xt%d" % i) for i in range(2)]
        zt = [k.tile([128, D], F32, "zt%d" % i) for i in range(2)]
        h1 = [k.tile([128, D], F32, "h1_%d" % i) for i in range(2)]
        h1b = [[k.tile([128, D], BF16, "h1b%d_%d" % (i, j)) for j in range(4)] for i in range(2)]
        h1T = [k.tile([128, 8, 128], BF16, "h1T%d" % i) for i in range(2)]
        st = [k.tile([128, 12], F32, "st%d" % i) for i in range(2)]
        mv = [k.tile([128, 2], F32, "mv%d" % i) for i in range(2)]
        rs = [k.tile([128, 1], F32, "rs%d" % i) for i in range(2)]
        nm = [k.tile([128, 1], F32, "nm%d" % i) for i in range(2)]
        Lm = [k.tile([128, 128], F32, "Lm%d" % i) for i in range(2)]
        m8 = k.tile([128, 4, 8], F32, "m8")
        maskb = k.tile([128, 128], BF16, "maskb")
        d4 = k.tile([128, 4, 4], F32, "d4")
        e4 = k.tile([128, 4, 4], F32, "e4")
        s4 = k.tile([128, 4, 1], F32, "s4")
        r4 = k.tile([128, 4, 1], F32, "r4")
        Rk = k.tile([128, 128], F32, "Rk")
        pen = k.tile([128, 128], F32, "pen")
        oh = k.tile([128, 128], F32, "oh")
        rowf = k.tile([128, 4, 4], F32, "rowf")
        print("phase A sbuf bytes", k.off)

        k.op("gpsimd", lambda g: g.memset(zero.ap, 0.0), writes=[zero])
        zf_ev = None
        for n in range(XR // 256):
            zf_ev = k.dma("scalar", lambda g, n=n: g.dma_start(
                out=XE[n * 256:(n + 1) * 256, :].rearrange("(p r) d -> p (r d)", p=128), in_=zero.ap), zero, reads=[zero])

        k.dma("sync", lambda g: g.dma_start(out=pscale.ap, in_=pscale_d.ap()), pscale, writes=[pscale])
        k.dma("sync", lambda g: g.dma_start(out=convw.ap, in_=convw_d.ap()), convw, writes=[convw])
        k.dma("sync", lambda g: g.dma_start(out=pfix.ap, in_=pfix_d.ap()), pfix, writes=[pfix])
        k.dma("sync", lambda g: g.dma_start(out=rb.ap, in_=rb_d.ap().partition_broadcast(128)), rb, writes=[rb])
        k.dma("sync", lambda g: g.dma_start(out=G1.ap, in_=ln_d[0:1, :].partition_broadcast(128)), G1, writes=[G1])
        k.dma("sync", lambda g: g.dma_start(out=B1.ap, in_=ln_d[1:2, :].partition_broadcast(128)), B1, writes=[B1])
        ci = 0
        ces = ["vector", "gpsimd", "scalar"]

        def cast(eng, dst_ap, src_t, dst_t):
            if eng == "scalar":
                k.op("scalar", lambda g: g.copy(out=dst_ap, in_=src_t.ap), reads=[src_t], writes=[dst_t])
            else:
                k.op(eng, lambda g: g.tensor_copy(out=dst_ap, in_=src_t.ap), reads=[src_t], writes=[dst_t])

        for kk in range(8):
            for hf in range(2):
                s_ = wst[ci % 3]
                k.dma("sync", lambda g, kk=kk, hf=hf, s_=s_: g.dma_start(
                    out=s_.ap, in_=w_in_d[kk * 128:(kk + 1) * 128, hf * 1024:(hf + 1) * 1024]), s_, writes=[s_])
                cast(ces[ci % 3], w_in[kk].ap[:, hf * 1024:(hf + 1) * 1024], s_, w_in[kk])
                ci += 1
        for kk in range(8):
            s_ = wst[ci % 3]
            k.dma("sync", lambda g, kk=kk, s_=s_: g.dma_start(out=s_.ap, in_=w_out_d[kk * 128:(kk + 1) * 128, :]), s_, writes=[s_])
            cast(ces[ci % 3], w_out[kk].ap, s_, w_out[kk])
            ci += 1
        s_ = wst[ci % 3]
        k.dma("sync", lambda g, s_=s_: g.dma_start(out=s_.ap[:, 0:512].rearrange("p (g c) -> p g c", g=4),
                                                  in_=pmix_d.ap().rearrange("g p c -> p g c")), s_, writes=[s_])
        k.op("vector", lambda g, s_=s_: g.tensor_copy(out=pmix.ap.rearrange("p g c -> p (g c)"), in_=s_.ap[:, 0:512]), reads=[s_], writes=[pmix])
        ci += 1
        s_ = wst[ci % 3]
        k.dma("sync", lambda g, s_=s_: g.dma_start(out=s_.ap[:, 0:256].rearrange("p (k e) -> p k e", k=8),
                                                  in_=rw_d.ap().rearrange("(k p) e -> p k e", p=128)), s_, writes=[s_])
        k.op("vector", lambda g, s_=s_: g.tensor_copy(out=rw.ap.rearrange("p k e -> p (k e)"), in_=s_.ap[:, 0:256]), reads=[s_], writes=[rw])
        ci += 1
        for g_ in range(4):
            k.op("vector", lambda g, g_=g_: g.memset(vp[g_][0].ap[:, 0:16], 0.0), writes=[vp[g_][0]])
            k.op("vector", lambda g, g_=g_: g.memset(ub[g_][0].ap[:, 0:2], 0.0), writes=[ub[g_][0]])

        xT_v = xT_d.ap().rearrange("(k p) t -> p k t", p=128)
        pbi = [0]

        def next_pb():
            t_ = pb[pbi[0] % 3]
            pbi[0] += 1
            return t_

        def proj_chunk(xbm, fc, pbt):
            for kk in range(8):
                k.op("tensor", lambda g, kk=kk: g.matmul(pbt.ap, lhsT=w_in[kk].ap[:, fc * 128:(fc + 1) * 128],
                                                         rhs=xbm.ap[:, kk, :], start=(kk == 0), stop=(kk == 7)),
                     reads=[w_in[kk], xbm], writes=[pbt], signal=(kk == 7))

        def stage1(m):
            par = m % 2
            nxt = (m + 1) % 2
            t0 = m * 512
            xbm = xb[par]
            k.dma("sync", lambda g: g.dma_start(out=xs.ap, in_=xT_v[:, :, t0:t0 + 512]), xs, writes=[xs])
            for q in range(4):
                eng = ["vector", "scalar", "vector", "scalar"][q]
                if eng == "scalar":
                    k.op("scalar", lambda g, q=q: g.copy(out=xbm.ap[:, 2 * q:2 * q + 2, :], in_=xs.ap[:, 2 * q:2 * q + 2, :]), reads=[xs], writes=[xbm])
                else:
                    k.op(eng, lambda g, q=q: g.tensor_copy(out=xbm.ap[:, 2 * q:2 * q + 2, :], in_=xs.ap[:, 2 * q:2 * q + 2, :]), reads=[xs], writes=[xbm])
            mx = mixT[par]
            pend_pm = [None]
            for g_ in range(4):
                w = WINS[g_]
                v = vp[g_][par]
                vn = vp[g_][nxt]
                pbt = next_pb()
                proj_chunk(xbm, g_, pbt)
                k.op("scalar", lambda g, v=v, pbt=pbt: g.copy(out=v.ap[:, 16:528], in_=pbt.ap), reads=[pbt], writes=[v])
                cur = v
                sh = 1
                lo = 0
                idx = 0
                while sh < w:
                    lo = lo + sh
                    dst = ptmp[idx % 2]
                    k.op("vector", lambda g, cur=cur, dst=dst, lo=lo, sh=sh: g.tensor_tensor(
                        out=dst.ap[:, lo:528], in0=cur.ap[:, lo:528], in1=cur.ap[:, lo - sh:528 - sh], op=ALU.add),
                        reads=[cur], writes=[dst])
                    cur = dst
                    sh *= 2
                    idx += 1
                if m == 0:
                    k.op("vector", lambda g, cur=cur, g_=g_: g.tensor_tensor(
                        out=cur.ap[:, 16:32], in0=cur.ap[:, 16:32], in1=pfix.ap[:, g_ * 16:(g_ + 1) * 16], op=ALU.mult),
                        reads=[cur, pfix], writes=[cur])
                dbt = db[g_ % 2]
                k.op("vector", lambda g, cur=cur, v=v, dbt=dbt, w=w: g.scalar_tensor_tensor(
                    out=dbt.ap, in0=cur.ap[:, 16:528], scalar=1.0 / w, in1=v.ap[:, 16:528], op0=ALU.mult, op1=ALU.subtract),
                    reads=[cur, v], writes=[dbt])
                k.op("gpsimd", lambda g, v=v, vn=vn: g.tensor_copy(out=vn.ap[:, 0:16], in_=v.ap[:, 512:528]), reads=[v], writes=[vn])
                def pm(g_=g_, dbt=dbt):
                    k.op("tensor", lambda g: g.matmul(pb[3].ap, lhsT=pmix.ap[:, g_, :], rhs=dbt.ap, start=True, stop=True),
                         reads=[pmix, dbt], writes=[pb[3]])
                    k.op("scalar", lambda g: g.activation(out=mx[g_].ap, in_=pb[3].ap, func=AF.Identity, scale=pscale.ap[:, g_:g_ + 1]),
                         reads=[pb[3], pscale], writes=[mx[g_]])
                if pend_pm[0] is not None:
                    pend_pm[0]()
                pend_pm[0] = pm
            for j in range(4):
                u = ub[j][par]
                un = ub[j][nxt]
                pC = next_pb()
                proj_chunk(xbm, 8 + j, pC)
                if j == 0 and pend_pm[0] is not None:
                    pend_pm[0]()
                    pend_pm[0] = None
                pV = next_pb()
                proj_chunk(xbm, 12 + j, pV)
                pB = next_pb()
                proj_chunk(xbm, 4 + j, pB)
                k.op("scalar", lambda g, pC=pC: g.copy(out=cu.ap, in_=pC.ap), reads=[pC], writes=[cu])
                k.op("vector", lambda g, u=u, pV=pV: g.tensor_tensor(out=u.ap[:, 2:514], in0=cu.ap, in1=pV.ap, op=ALU.mult),
                     reads=[cu, pV], writes=[u])
                c0, c1 = ctmp
                k.op("scalar", lambda g, u=u, j=j: g.activation(out=c0.ap, in_=u.ap[:, 0:512], func=AF.Identity, scale=convw.ap[:, 3 * j:3 * j + 1]),
                     reads=[u, convw], writes=[c0])
                k.op("vector", lambda g, u=u, j=j: g.scalar_tensor_tensor(out=c1.ap, in0=u.ap[:, 1:513], scalar=convw.ap[:, 3 * j + 1:3 * j + 2],
                                                                            in1=c0.ap, op0=ALU.mult, op1=ALU.add), reads=[u, convw, c0], writes=[c1])
                k.op("vector", lambda g, u=u, j=j: g.scalar_tensor_tensor(out=c0.ap, in0=u.ap[:, 2:514], scalar=convw.ap[:, 3 * j + 2:3 * j + 3],
                                                                            in1=c1.ap, op0=ALU.mult, op1=ALU.add), reads=[u, convw, c1], writes=[c0])
                k.op("gpsimd", lambda g, u=u, un=un: g.tensor_copy(out=un.ap[:, 0:2], in_=u.ap[:, 512:514]), reads=[u], writes=[un])
                k.op("vector", lambda g, j=j, pB=pB: g.tensor_tensor(out=mx[4 + j].ap, in0=c0.ap, in1=pB.ap, op=ALU.mult),
                     reads=[c0, pB], writes=[mx[4 + j]])
            def sub_gen(sub):
                tl = 4 * m + sub
                xtt = xt[sub % 2]
                ztt = zt[sub % 2]
                h1t = h1[sub % 2]
                k.dma("sync", lambda g: g.dma_start(out=xtt.ap, in_=x_d[tl * 128:(tl + 1) * 128, :]), xtt, writes=[xtt])
                for hf in range(2):
                    for kk in range(8):
                        k.op("tensor", lambda g, kk=kk, hf=hf: g.matmul(
                            pw.ap[:, hf * 512:(hf + 1) * 512], lhsT=mx[kk].ap[:, sub * 128:(sub + 1) * 128],
                            rhs=w_out[kk].ap[:, hf * 512:(hf + 1) * 512], start=(kk == 0), stop=(kk == 7)),
                            reads=[mx[kk], w_out[kk]], writes=[pw], signal=(kk == 7))
                k.op("vector", lambda g: g.scalar_tensor_tensor(out=ztt.ap, in0=xtt.ap, scalar=ALPHA, in1=pw.ap,
                                                                 op0=ALU.mult, op1=ALU.add), reads=[xtt, pw], writes=[ztt])
                yield
                yield from layer_norm(ztt, G1, B1, h1t, st[sub % 2], mv[sub % 2], rs[sub % 2], nm[sub % 2])
                k.dma("gpsimd", lambda g: g.dma_start(out=H1F[tl * 128:(tl + 1) * 128, :], in_=h1t.ap), h1t, reads=[h1t])
                hb = h1b[par][sub]
                k.op("scalar", lambda g: g.copy(out=hb.ap, in_=h1t.ap), reads=[h1t], writes=[hb])
                yield

            interleave(sub_gen(0), sub_gen(1))
            interleave(sub_gen(2), sub_gen(3))

        def stage2(m):
            par = m % 2
            L = Lm[par]
            L3 = L.ap.rearrange("p (j e) -> p j e", e=E)
            for sub in range(4):
                hb = h1b[par][sub]
                hT = h1T[sub % 2]
                transpose8(hb, hT, "scalar" if sub % 2 else "vector")
                for kk in range(8):
                    k.op("tensor", lambda g, kk=kk, hT=hT: g.matmul(pl.ap[:, 0:E], lhsT=hT.ap[:, kk, :], rhs=rw.ap[:, kk, :],
                                                                      start=(kk == 0), stop=(kk == 7)),
                         reads=[hT, rw], writes=[pl], signal=(kk == 7))
                k.op("vector", lambda g, sub=sub: g.tensor_tensor(out=L.ap[:, sub * E:(sub + 1) * E], in0=pl.ap[:, 0:E], in1=rb.ap, op=ALU.add),
                     reads=[pl, rb], writes=[L])
            for j in range(4):
                k.op("vector", lambda g, j=j: g.max(out=m8.ap[:, j, :], in_=L.ap[:, j * E:(j + 1) * E]), reads=[L], writes=[m8])
            k.op("vector", lambda g: g.tensor_tensor(out=maskb.ap.rearrange("p (j e) -> p j e", e=E), in0=L3,
                                                      in1=m8.ap[:, :, 3:4].to_broadcast([128, 4, E]), op=ALU.is_ge),
                 reads=[L, m8], writes=[maskb])
            k.op("vector", lambda g: g.tensor_tensor(out=d4.ap, in0=m8.ap[:, :, 0:4], in1=m8.ap[:, :, 0:1].to_broadcast([128, 4, 4]),
                                                      op=ALU.subtract), reads=[m8], writes=[d4])
            k.op("scalar", lambda g: g.activation(out=e4.ap, in_=d4.ap, func=AF.Exp), reads=[d4], writes=[e4])
            k.op("vector", lambda g: g.tensor_reduce(out=s4.ap.rearrange("p j o -> p (j o)"), in_=e4.ap, axis=AX.X, op=ALU.add),
                 reads=[e4], writes=[s4])
            k.op("vector", lambda g: g.reciprocal(out=r4.ap, in_=s4.ap), reads=[s4], writes=[r4])
            k.op("vector", lambda g: g.tensor_tensor(out=GATE.ap[:, 4 * m:4 * m + 4, :], in0=e4.ap, in1=r4.ap.to_broadcast([128, 4, 4]),
                                                      op=ALU.mult), reads=[e4, r4, GATE], writes=[GATE])
            k.op("tensor", lambda g: g.matmul(pl.ap[:, 128:256], lhsT=tri.ap, rhs=maskb.ap, start=True, stop=True),
                 reads=[tri, maskb], writes=[pl])
            k.op("tensor", lambda g: g.matmul(pl.ap[:, 256:384], lhsT=ones.ap, rhs=maskb.ap, start=True, stop=True),
                 reads=[ones, maskb], writes=[pl])
            for j in range(4):
                k.op("vector", lambda g, j=j: g.tensor_tensor(out=Rk.ap[:, j * E:(j + 1) * E], in0=pl.ap[:, 128 + j * E:128 + (j + 1) * E],
                                                               in1=base_run.ap, op=ALU.add), reads=[pl, base_run, Rk], writes=[Rk])
                k.op("vector", lambda g, j=j: g.tensor_tensor(out=base_run.ap, in0=base_run.ap, in1=pl.ap[:, 256 + j * E:256 + (j + 1) * E],
                                                               op=ALU.add), reads=[pl, base_run], writes=[base_run])
            k.op("vector", lambda g: g.tensor_scalar(out=pen.ap, in0=Rk.ap, scalar1=CAP + 0.5, scalar2=1.0e6, op0=ALU.is_gt, op1=ALU.mult),
                 reads=[Rk], writes=[pen])
            Rk3 = Rk.ap.rearrange("p (j e) -> p j e", e=E)
            k.op("vector", lambda g: g.tensor_tensor(out=Rk3, in0=Rk3, in1=ecol.ap.to_broadcast([128, 4, E]), op=ALU.add),
                 reads=[Rk, ecol], writes=[Rk])
            k.op("vector", lambda g: g.tensor_tensor(out=Rk.ap, in0=Rk.ap, in1=pen.ap, op=ALU.add), reads=[Rk, pen], writes=[Rk])
            oh3 = oh.ap.rearrange("p (j e) -> p j e", e=E)
            for kq in range(4):
                k.op("vector", lambda g, kq=kq: g.tensor_tensor(out=oh3, in0=L3, in1=m8.ap[:, :, kq:kq + 1].to_broadcast([128, 4, E]),
                                                                 op=ALU.is_equal), reads=[L, m8], writes=[oh])
                k.op("vector", lambda g: g.tensor_tensor(out=oh.ap, in0=oh.ap, in1=Rk.ap, op=ALU.mult), reads=[oh, Rk], writes=[oh])
                k.op("vector", lambda g, kq=kq: g.tensor_reduce(out=rowf.ap[:, :, kq], in_=oh3, axis=AX.X, op=ALU.add),
                     reads=[oh, rowf], writes=[rowf])
            k.op("vector", lambda g: g.tensor_copy(out=ROWI.ap[:, 4 * m:4 * m + 4, :], in_=rowf.ap), reads=[rowf, ROWI], writes=[ROWI])
            for j in range(4):
                hb = h1b[par][j]
                for kq in range(4):
                    k.dma("gpsimd", lambda g, j=j, kq=kq, hb=hb: g.indirect_dma_start(
                        out=XE[:, :], out_offset=bass.IndirectOffsetOnAxis(ap=ROWI.ap[:, 4 * m + j, kq:kq + 1], axis=0),
                        in_=hb.ap, in_offset=None, bounds_check=regs["bc"], oob_is_err=False),
                        hb, reads=[hb, ROWI], extra=[zf_ev])

        import os as _os
        NMR = int(_os.environ.get("K_NM", NM))
        stage1(0)
        for m in range(1, NMR):
            stage1(m)
            stage2(m - 1)
        stage2(NMR - 1)
        if debug:
            k.dma("sync", lambda g: g.dma_start(out=DBG_ROW.ap(), in_=ROWI.ap.rearrange("p t k -> p (t k)")), ROWI, reads=[ROWI])
            k.dma("sync", lambda g: g.dma_start(out=DBG_GATE.ap(), in_=GATE.ap.rearrange("p t k -> p (t k)")), GATE, reads=[GATE])
        k.barrier()

        k.off = persist_end
        import os as _os
        upto = _os.environ.get("K_UPTO", upto)
        wgu = [[k.tile([128, 2048], BF16, "wgu%d_%d" % (i, kk)) for kk in range(8)] for i in range(2)]
        wdn = [[k.tile([128, 1024], BF16, "wdn%d_%d" % (i, kk)) for kk in range(8)] for i in range(2)]
        cst = [k.tile([128, 1024], F32, "cst%d" % i) for i in range(6)]
        bgu = k.tile([128, E * 16], F32, "bgu")
        bdn = [k.tile([128, D], F32, "bdn%d" % i) for i in range(2)]
        xblk = [[k.tile([128, D], BF16, "xblk%d_%d" % (i, j)) for j in range(NBLK)] for i in range(2)]
        XT = k.tile([128, 8, CAP], BF16, "XT")
        actT = [k.tile([128, CAP], BF16, "actT%d" % i) for i in range(8)]
        g1 = [k.tile([128, 512], F32, "g1_%d" % i) for i in range(2)]
        sg = [k.tile([128, 512], F32, "sg_%d" % i) for i in range(2)]
        u0 = [k.tile([128, 512], F32, "u0_%d" % i) for i in range(2)]
        tg = [k.tile([128, 512], F32, "tg_%d" % i) for i in range(2)]
        yb = [k.tile([128, D], F32, "yb%d" % i) for i in range(2)]
        print("phase C sbuf bytes", k.off)

        k.dma("sync", lambda g: g.dma_start(out=bgu.ap, in_=bgu_d.ap()), bgu, writes=[bgu])
        cctr = [0]
        cast_engs = ["scalar", "vector", "scalar", "vector", "scalar"]

        def weight_tasks(e):
            b = e % 2
            tasks = []
            for kk in range(8):
                for hf in range(2):
                    def tk(kk=kk, hf=hf):
                        s_ = cst[cctr[0] % 6]
                        k.dma("sync", lambda g: g.dma_start(
                            out=s_.ap, in_=wgu_d[e, kk * 128:(kk + 1) * 128, hf * 1024:(hf + 1) * 1024]), s_, writes=[s_])
                        cast(cast_engs[cctr[0] % 5], wgu[b][kk].ap[:, hf * 1024:(hf + 1) * 1024], s_, wgu[b][kk])
                        cctr[0] += 1
                    tasks.append(tk)
            for kk in range(8):
                def tk(kk=kk):
                    s_ = cst[cctr[0] % 6]
                    k.dma("sync", lambda g: g.dma_start(out=s_.ap, in_=wdn_d[e, kk * 128:(kk + 1) * 128, :]), s_, writes=[s_])
                    cast(cast_engs[cctr[0] % 5], wdn[b][kk].ap, s_, wdn[b][kk])
                    cctr[0] += 1
                tasks.append(tk)

            def tb():
                k.dma("sync", lambda g: g.dma_start(out=bdn[b].ap, in_=bdn_d[e:e + 1, :].partition_broadcast(128)), bdn[b], writes=[bdn[b]])
            tasks.append(tb)
            return tasks

        pend = []

        def pump(n):
            for _ in range(n):
                if pend:
                    pend.pop(0)()

        def load_X(e):
            for blk in range(NBLK):
                xbk = xblk[e % 2][blk]
                r0 = e * CAP + blk * 128
                k.dma("gpsimd", lambda g, xbk=xbk, r0=r0: g.dma_start(out=xbk.ap, in_=XE[r0:r0 + 128, :]), xbk, writes=[xbk])

        def do_T(e):
            for blk in range(NBLK):
                xbk = xblk[e % 2][blk]
                transpose8(xbk, XT, "scalar" if blk % 2 else "vector", out_ap=XT.ap[:, :, blk * 128:(blk + 1) * 128])

        gctr = [0]

        def gu_front(e, b, pc, unit):
            if unit == 0:
                pg, pu = (pb[0], pb[1]) if gctr[0] % 2 == 0 else (pb[2], pb[3])
                pg_ap, pu_ap = pg.ap, pu.ap
                n0, nn = 0, 512
            else:
                pg = pu = pl
                pg_ap, pu_ap = pl.ap[:, 0:128], pl.ap[:, 128:256]
                n0, nn = 512, 128
            bi = gctr[0] % 2
            gctr[0] += 1
            for pp_t, pp_ap, cbase in ((pg, pg_ap, pc * 128), (pu, pu_ap, 1024 + pc * 128)):
                for kk in range(8):
                    k.op("tensor", lambda g, kk=kk, pp_ap=pp_ap, cbase=cbase: g.matmul(
                        pp_ap, lhsT=wgu[b][kk].ap[:, cbase:cbase + 128], rhs=XT.ap[:, kk, n0:n0 + nn],
                        start=(kk == 0), stop=(kk == 7)),
                        reads=[wgu[b][kk], XT], writes=[pp_t], signal=(kk == 7))
            a_g1, a_sg, a_u0, a_tg = g1[bi], sg[bi], u0[bi], tg[bi]
            bg_ap = bgu.ap[:, e * 16 + pc:e * 16 + pc + 1]
            bu_ap = bgu.ap[:, e * 16 + 8 + pc:e * 16 + 8 + pc + 1]
            k.op("vector", lambda g: g.tensor_scalar(out=a_g1.ap[:, 0:nn], in0=pg_ap, scalar1=bg_ap, scalar2=7.0, op0=ALU.add, op1=ALU.min),
                 reads=[pg, bgu], writes=[a_g1])
            if _os.environ.get("K_VAR", "1") == "1":
                k.op("scalar", lambda g: g.activation(out=a_sg.ap[:, 0:nn], in_=a_g1.ap[:, 0:nn], func=AF.Sigmoid, scale=1.702),
                     reads=[a_g1], writes=[a_sg])
                k.op("scalar", lambda g: g.activation(out=a_u0.ap[:, 0:nn], in_=pu_ap, func=AF.Identity, bias=bu_ap),
                     reads=[pu, bgu], writes=[a_u0])
            else:
                k.op("scalar", lambda g: g.activation(out=a_u0.ap[:, 0:nn], in_=pu_ap, func=AF.Identity, bias=bu_ap),
                     reads=[pu, bgu], writes=[a_u0])
                k.op("scalar", lambda g: g.activation(out=a_sg.ap[:, 0:nn], in_=a_g1.ap[:, 0:nn], func=AF.Sigmoid, scale=1.702),
                     reads=[a_g1], writes=[a_sg])
            return (pc, n0, nn, a_g1, a_sg, a_u0, a_tg)

        def gu_back(ctx):
            pc, n0, nn, a_g1, a_sg, a_u0, a_tg = ctx
            k.op("gpsimd", lambda g: g.tensor_tensor(out=a_tg.ap[:, 0:nn], in0=a_g1.ap[:, 0:nn], in1=a_sg.ap[:, 0:nn], op=ALU.mult),
                 reads=[a_g1, a_sg], writes=[a_tg])
            k.op("vector", lambda g: g.tensor_scalar(out=a_u0.ap[:, 0:nn], in0=a_u0.ap[:, 0:nn], scalar1=7.0, scalar2=-7.0,
                                                      op0=ALU.min, op1=ALU.max), reads=[a_u0], writes=[a_u0])
            k.op("vector", lambda g: g.scalar_tensor_tensor(
                out=actT[pc].ap[:, n0:n0 + nn], in0=a_u0.ap[:, 0:nn], scalar=1.0, in1=a_tg.ap[:, 0:nn], op0=ALU.add, op1=ALU.mult),
                reads=[a_u0, a_tg, actT[pc]], writes=[actT[pc]])
            pump(1)

        def do_GU(e):
            b = e % 2
            prev = None
            for pc in range(8):
                for unit in range(2):
                    ctx = gu_front(e, b, pc, unit)
                    if _os.environ.get("K_VAR", "") == "2":
                        gu_back(ctx)
                        continue
                    if prev is not None:
                        gu_back(prev)
                    prev = ctx
            if prev is not None:
                gu_back(prev)

        yctr = [0]

        def do_DN_T(e):
            b = e % 2
            for blk in range(NBLK):
                if e + 1 < E:
                    xbk = xblk[(e + 1) % 2][blk]
                    transpose8(xbk, XT, "scalar", out_ap=XT.ap[:, :, blk * 128:(blk + 1) * 128])
                for hf in range(2):
                    for fc in range(8):
                        k.op("tensor", lambda g, fc=fc, hf=hf, blk=blk: g.matmul(
                            pw.ap[:, hf * 512:(hf + 1) * 512], lhsT=actT[fc].ap[:, blk * 128:(blk + 1) * 128],
                            rhs=wdn[b][fc].ap[:, hf * 512:(hf + 1) * 512], start=(fc == 0), stop=(fc == 7)),
                            reads=[actT[fc], wdn[b][fc]], writes=[pw], signal=(fc == 7))
                ybt = yb[yctr[0] % 2]
                yctr[0] += 1
                k.op("vector", lambda g, ybt=ybt: g.tensor_tensor(out=ybt.ap, in0=pw.ap, in1=bdn[b].ap, op=ALU.add),
                     reads=[pw, bdn[b]], writes=[ybt])
                r0 = e * CAP + blk * 128
                k.dma("gpsimd", lambda g, ybt=ybt, r0=r0: g.dma_start(out=YE[r0:r0 + 128, :], in_=ybt.ap), ybt, reads=[ybt])
                pump(2)

        if upto != "A":
            pend.extend(weight_tasks(0))
            pump(100)
            load_X(0)
            do_T(0)
        NER = int(_os.environ.get("K_NE", E))
        for e in range(NER if upto != "A" else 0):
            if e + 1 < E:
                pend.extend(weight_tasks(e + 1))
                load_X(e + 1)
            do_GU(e)
            do_DN_T(e)
            pump(100)
        k.barrier()

        k.off = persist_end
        pgw = [k.tile([128, D], BF16, "pgw%d" % i) for i in range(8)]
        pproj = [k.tile([128, D], BF16, "pproj%d" % i) for i in range(2)]
        dst_ = [k.tile([128, 1024], F32, "dst%d" % i) for i in range(2)]
        G2 = k.tile([128, D], F32, "G2")
        B2 = k.tile([128, D], F32, "B2")
        G3 = k.tile([128, D], F32, "G3")
        B3 = k.tile([128, D], F32, "B3")
        pgb = k.tile([128, D], F32, "pgb")
        yk = [[k.tile([128, D], F32, "yk%d_%d" % (i, q)) for q in range(4)] for i in range(4)]
        h1r = [k.tile([128, D], F32, "h1r%d" % i) for i in range(4)]
        acc = [k.tile([128, D], F32, "acc%d" % i) for i in range(2)]
        h2 = [k.tile([128, D], F32, "h2_%d" % i) for i in range(4)]
        h2b = [k.tile([128, D], BF16, "h2b%d" % i) for i in range(4)]
        h2T = [k.tile([128, 8, 128], BF16, "h2T%d" % i) for i in range(2)]
        pts = [k.tile([128, 2, 128], F32, "pts%d" % i) for i in range(4)]
        ptb = [k.tile([128, 2, 128], BF16, "ptb%d" % i) for i in range(6)]
        gs = [k.tile([128, D], F32, "gs%d" % i) for i in range(2)]
        z3 = [k.tile([128, D], F32, "z3_%d" % i) for i in range(2)]
        ot = [k.tile([128, D], F32, "ot%d" % i) for i in range(2)]
        st2 = [k.tile([128, 12], F32, "st2_%d" % i) for i in range(2)]
        mv2 = [k.tile([128, 2], F32, "mv2_%d" % i) for i in range(2)]
        rs2 = [k.tile([128, 1], F32, "rs2_%d" % i) for i in range(2)]
        nm2 = [k.tile([128, 1], F32, "nm2_%d" % i) for i in range(2)]
        st3 = [k.tile([128, 12], F32, "st3_%d" % i) for i in range(2)]
        mv3 = [k.tile([128, 2], F32, "mv3_%d" % i) for i in range(2)]
        rs3 = [k.tile([128, 1], F32, "rs3_%d" % i) for i in range(2)]
        nm3 = [k.tile([128, 1], F32, "nm3_%d" % i) for i in range(2)]
        print("phase D sbuf bytes", k.off)

        for i, (tt, row) in enumerate(((G2, 2), (B2, 3), (G3, 4), (B3, 5))):
            k.dma("sync", lambda g, tt=tt, row=row: g.dma_start(out=tt.ap, in_=ln_d[row:row + 1, :].partition_broadcast(128)), tt, writes=[tt])
        k.dma("sync", lambda g: g.dma_start(out=pgb.ap, in_=pgb_d.ap().partition_broadcast(128)), pgb, writes=[pgb])
        ci = 0
        for kk in range(8):
            s_ = dst_[ci % 2]
            k.dma("sync", lambda g, kk=kk, s_=s_: g.dma_start(out=s_.ap, in_=pgw_d[kk * 128:(kk + 1) * 128, :]), s_, writes=[s_])
            cast(ces[ci % 3], pgw[kk].ap, s_, pgw[kk])
            ci += 1
        for c in range(2):
            s_ = dst_[ci % 2]
            k.dma("sync", lambda g, c=c, s_=s_: g.dma_start(out=s_.ap, in_=pproj_d[c * 128:(c + 1) * 128, :]), s_, writes=[s_])
            cast(ces[ci % 3], pproj[c].ap, s_, pproj[c])
            ci += 1
        pT_v = pT_d.ap().rearrange("(c p) t -> p c t", p=128)

        def gatherD(t):
            b4 = t % 4
            for q in range(4):
                k.dma("gpsimd", lambda g, q=q: g.indirect_dma_start(
                    out=yk[b4][q].ap, out_offset=None, in_=YE[:, :],
                    in_offset=bass.IndirectOffsetOnAxis(ap=ROWI.ap[:, t, q:q + 1], axis=0),
                    bounds_check=regs["bc"], oob_is_err=False), yk[b4][q], reads=[ROWI], writes=[yk[b4][q]])
            k.dma("sync", lambda g: g.dma_start(out=h1r[b4].ap, in_=H1F[t * 128:(t + 1) * 128, :]), h1r[b4], writes=[h1r[b4]])
            k.dma("sync", lambda g: g.dma_start(out=pts[b4].ap, in_=pT_v[:, :, t * 128:(t + 1) * 128]), pts[b4], writes=[pts[b4]])
            k.op("gpsimd", lambda g: g.tensor_copy(out=ptb[t % 6].ap, in_=pts[b4].ap), reads=[pts[b4]], writes=[ptb[t % 6]])

        def stageD1(t):
            b = t % 2
            b4 = t % 4
            a = acc[b]
            k.op("scalar", lambda g: g.activation(out=a.ap, in_=yk[b4][0].ap, func=AF.Identity, scale=GATE.ap[:, t, 0:1]),
                 reads=[yk[b4][0], GATE], writes=[a])
            yield
            for q in range(1, 4):
                k.op("vector", lambda g, q=q: g.scalar_tensor_tensor(out=a.ap, in0=yk[b4][q].ap, scalar=GATE.ap[:, t, q:q + 1], in1=a.ap,
                                                                      op0=ALU.mult, op1=ALU.add), reads=[yk[b4][q], GATE, a], writes=[a])
            k.op("vector", lambda g: g.scalar_tensor_tensor(out=a.ap, in0=h1r[b4].ap, scalar=ALPHA, in1=a.ap, op0=ALU.mult, op1=ALU.add),
                 reads=[h1r[b4], a], writes=[a])
            yield
            h2t = h2[b4]
            yield from layer_norm(a, G2, B2, h2t, st2[b], mv2[b], rs2[b], nm2[b])
            k.op("scalar", lambda g: g.copy(out=h2b[b4].ap, in_=h2t.ap), reads=[h2t], writes=[h2b[b4]])
            yield

        def stageD2(t):
            b = t % 2
            b4 = t % 4
            h2t = h2[b4]
            transpose8(h2b[b4], h2T[b], "scalar")
            yield
            for hf in range(2):
                for kk in range(8):
                    k.op("tensor", lambda g, kk=kk, hf=hf: g.matmul(pw.ap[:, hf * 512:(hf + 1) * 512], lhsT=h2T[b].ap[:, kk, :],
                                                                      rhs=pgw[kk].ap[:, hf * 512:(hf + 1) * 512], start=(kk == 0), stop=(kk == 7)),
                         reads=[h2T[b], pgw[kk]], writes=[pw], signal=(kk == 7))
            pp = (pb[0], pb[1]) if b == 0 else (pb[2], pb[3])
            for hf in range(2):
                for c in range(2):
                    k.op("tensor", lambda g, c=c, hf=hf: g.matmul(pp[hf].ap, lhsT=ptb[t % 6].ap[:, c, :], rhs=pproj[c].ap[:, hf * 512:(hf + 1) * 512],
                                                                    start=(c == 0), stop=(c == 1)),
                         reads=[ptb[t % 6], pproj[c]], writes=[pp[hf]], signal=(c == 1))
            g_ = gs[b]
            k.op("vector", lambda g: g.tensor_tensor(out=g_.ap, in0=pw.ap, in1=pgb.ap, op=ALU.add), reads=[pw, pgb], writes=[g_])
            yield
            k.op("scalar", lambda g: g.activation(out=g_.ap, in_=g_.ap, func=AF.Sigmoid), reads=[g_], writes=[g_])
            yield
            for hf in range(2):
                k.op("vector", lambda g, hf=hf: g.tensor_tensor(out=g_.ap[:, hf * 512:(hf + 1) * 512], in0=g_.ap[:, hf * 512:(hf + 1) * 512],
                                                                 in1=pp[hf].ap, op=ALU.mult), reads=[g_, pp[hf]], writes=[g_])
            z = z3[b]
            k.op("vector", lambda g: g.scalar_tensor_tensor(out=z.ap, in0=h2t.ap, scalar=ALPHA, in1=g_.ap, op0=ALU.mult, op1=ALU.add),
                 reads=[h2t, g_], writes=[z])
            yield
            o = ot[b]
            yield from layer_norm(z, G3, B3, o, st3[b], mv3[b], rs3[b], nm3[b])
            k.dma("sync", lambda g: g.dma_start(out=out_d[t * 128:(t + 1) * 128, :], in_=o.ap), o, reads=[o])
            yield

        if upto == "D":
            gatherD(0)
            gatherD(1)
            for pr in range(NT // 2):
                t0 = 2 * pr
                if t0 + 2 < NT:
                    gatherD(t0 + 2)
                    gatherD(t0 + 3)
                gens = [stageD1(t0), stageD1(t0 + 1)]
                if pr >= 1:
                    gens += [stageD2(t0 - 2), stageD2(t0 - 1)]
                interleave(*gens)
            interleave(stageD2(NT - 2), stageD2(NT - 1))
        k.finish(None)

        with nc.Block() as block:
            @block.sync
            def _(eng):
                for f in k.eng["sync"].prog:
                    f(eng)

            @block.scalar
            def _(eng):
                for f in k.eng["scalar"].prog:
                    f(eng)

            @block.vector
            def _(eng):
                for f in k.eng["vector"].prog:
                    f(eng)

            @block.gpsimd
            def _(eng):
                for f in k.eng["gpsimd"].prog:
                    f(eng)

            @block.tensor
            def _(eng):
                for f in k.eng["tensor"].prog:
                    f(eng)
    return nc


def make_consts():
    ident = np.eye(128, dtype=np.float32)
    tri = np.triu(np.ones((128, 128), dtype=np.float32))
    ecol = np.tile((np.arange(E, dtype=np.float32) * CAP - 1.0)[None, :], (128, 1))
    pfix = np.ones((128, 4, 16), dtype=np.float32)
    for g_, w in enumerate(WINS):
        for t in range(16):
            pfix[:, g_, t] = w / min(t + 1, w)
    return ident, tri, ecol, pfix.reshape(128, 64)


def make_in_maps(inp):
    f = lambda a: np.ascontiguousarray(np.asarray(a, dtype=np.float32))
    x = f(inp["x"])
    p = f(inp["p"])[0]
    ident, tri, ecol, pfix = make_consts()
    shared = {
        "w_in": f(inp["w_in"][0]),
        "pool_mix": f(inp["pool_mix"][0]),
        "pool_scale_t": f(np.asarray(inp["pool_scale"][0]).reshape(4, 128).T),
        "conv_w_t": f(np.asarray(inp["conv_w"][0]).reshape(3, 4, 128).transpose(2, 1, 0).reshape(128, 12)),
        "w_out": f(inp["w_out"][0]),
        "ln_all": f(np.stack([np.asarray(inp[n][0]) for n in ("ln1_g", "ln1_b", "ln2_g", "ln2_b", "ln3_g", "ln3_b")], 0)),
        "router_w": f(inp["router_w"][0]),
        "router_b": f(np.asarray(inp["router_b"][0]).reshape(1, E)),
        "w_gate_up": f(inp["w_gate_up"][0]),
        "b_gu_t": f(np.asarray(inp["b_gate_up"][0]).reshape(E, 16, 128).transpose(2, 0, 1).reshape(128, E * 16)),
        "w_down": f(inp["w_down"][0]),
        "b_down": f(inp["b_down"][0]),
        "ple_proj": f(inp["ple_proj"][0]),
        "ple_gate_w": f(inp["ple_gate_w"][0]),
        "ple_gate_b": f(np.asarray(inp["ple_gate_b"][0]).reshape(1, D)),
        "c_ident": ident, "c_tri": tri, "c_ecol": ecol, "c_poolfix": pfix,
    }
    maps = []
    for b in range(8):
        m = dict(shared)
        m["x"] = f(x[b])
        m["xT"] = f(x[b].T)
        m["pT"] = f(p[b].T)
        maps.append(m)
    return maps


def kernel(**inputs):
    nc = build(debug=False)
    in_maps = make_in_maps(inputs)
    res = run_bass_kernel_spmd(nc, in_maps, core_ids=list(range(8)))
    out = np.stack([np.asarray(r["out"], dtype=np.float32).reshape(S, D) for r in res.results], 0)
    return out
```
